# Optimizing a Trainium2 kernel written in Bass

```python
import math
import jax, jax.numpy as jnp
from jax import lax
import numpy as np

D_MODEL = 2048
BATCH = 8
SEQ = 4096
DEPTH = 2

N_META = 16
MLA_HEADS = 16
MLA_NOPE_DIM = 128
MLA_ROPE_DIM = 64
MLA_V_DIM = 128
MLA_Q_RANK = 768
MLA_KV_RANK = 512
SWA_HEADS = 16
SWA_KV_HEADS = 4
SWA_GROUP = SWA_HEADS // SWA_KV_HEADS
SWA_HEAD_DIM = 128
WINDOW = 128
BLOCK = 128
N_BUCKETS = 32
MAX_DISTANCE = 128
D_FF = 5632
N_EXPERTS = 8
TOP_K = 2
MOE_BLOCK = 512
N_DENSE = (DEPTH + 1) // 2
N_MOE = DEPTH // 2
ROPE_THETA = 10000.0
LN_EPS = 1e-5
RMS_EPS = 1e-6
ALPHA = (2 * DEPTH) ** 0.25
BETA = (8 * DEPTH) ** -0.25
NEG = -1e30
SPLITS = (MLA_Q_RANK, MLA_KV_RANK, MLA_ROPE_DIM, SWA_HEADS * SWA_HEAD_DIM,
          SWA_KV_HEADS * SWA_HEAD_DIM, SWA_KV_HEADS * SWA_HEAD_DIM, D_MODEL, D_MODEL)
IN_DIM = sum(SPLITS)

kernel_name = 'hybrid_mla_swa_gated_moe_encoder'


def layer_norm(x, g, b):
    xf = x.astype(jnp.float32)
    mu = xf.mean(-1, keepdims=True)
    var = jnp.square(xf - mu).mean(-1, keepdims=True)
    y = (xf - mu) * lax.rsqrt(var + LN_EPS) * g.astype(jnp.float32) + b.astype(jnp.float32)
    return y.astype(x.dtype)


def rms_norm(x, g):
    xf = x.astype(jnp.float32)
    y = xf * lax.rsqrt(jnp.square(xf).mean(-1, keepdims=True) + RMS_EPS) * g.astype(jnp.float32)
    return y.astype(x.dtype)


def rope_tables(n):
    pos = jnp.arange(n, dtype=jnp.float32)
    inv = ROPE_THETA ** (-jnp.arange(0, MLA_ROPE_DIM, 2, dtype=jnp.float32) / MLA_ROPE_DIM)
    ang = pos[:, None] * inv[None, :]
    return jnp.cos(ang), jnp.sin(ang)


def apply_rope(x, cos, sin):
    half = x.shape[-1] // 2
    x1 = x[..., :half].astype(jnp.float32)
    x2 = x[..., half:].astype(jnp.float32)
    return jnp.concatenate([x1 * cos - x2 * sin, x1 * sin + x2 * cos], -1).astype(x.dtype)


def rel_bucket(rel):
    nb = N_BUCKETS // 2
    max_exact = nb // 2
    n = jnp.abs(rel)
    large = max_exact + (jnp.log(jnp.maximum(n, 1).astype(jnp.float32) / max_exact)
                         / math.log(MAX_DISTANCE / max_exact) * (nb - max_exact)).astype(jnp.int32)
    large = jnp.minimum(large, nb - 1)
    return jnp.where(rel > 0, nb, 0) + jnp.where(n < max_exact, n, large)


def mla_attend(q_nope, q_rope, k_nope, k_rope, v):
    scale = (MLA_NOPE_DIM + MLA_ROPE_DIM) ** -0.5
    s = (jnp.einsum('bqhd,bkhd->bhqk', q_nope, k_nope)
         + jnp.einsum('bqhd,bkd->bhqk', q_rope, k_rope)).astype(jnp.float32) * scale
    p = jax.nn.softmax(s, axis=-1).astype(v.dtype)
    return jnp.einsum('bhqk,bkhd->bqhd', p, v)


def mla_branch(c_q, c_kv, k_rope_raw, q_norm_g, kv_norm_g, w_uq, w_ukv, cos, sin):
    bsz, n_tok, _ = c_q.shape
    q = (rms_norm(c_q, q_norm_g) @ w_uq).reshape(bsz, n_tok, MLA_HEADS, MLA_NOPE_DIM + MLA_ROPE_DIM)
    q_nope = q[..., :MLA_NOPE_DIM]
    q_rope = apply_rope(q[..., MLA_NOPE_DIM:], cos[None, :, None], sin[None, :, None])
    kv = (rms_norm(c_kv, kv_norm_g) @ w_ukv).reshape(bsz, n_tok, MLA_HEADS, MLA_NOPE_DIM + MLA_V_DIM)
    k_nope = kv[..., :MLA_NOPE_DIM]
    v = kv[..., MLA_NOPE_DIM:]
    k_rope = apply_rope(k_rope_raw, cos[None], sin[None])
    n_blk = (n_tok - N_META) // BLOCK

    def to_blocks(t):
        return jnp.moveaxis(t[:, N_META:].reshape(bsz, n_blk, BLOCK, *t.shape[2:]), 1, 0)

    o_meta = mla_attend(q_nope[:, :N_META], q_rope[:, :N_META], k_nope, k_rope, v)
    o_real = lax.map(lambda a: mla_attend(a[0], a[1], k_nope, k_rope, v),
                     (to_blocks(q_nope), to_blocks(q_rope)))
    o_real = jnp.moveaxis(o_real, 0, 1).reshape(bsz, n_tok - N_META, MLA_HEADS, MLA_V_DIM)
    return jnp.concatenate([o_meta, o_real], 1).reshape(bsz, n_tok, MLA_HEADS * MLA_V_DIM)


def swa_attend(q, k, v, q_pos, k_pos, always, in_range, rel_bias, sink):
    rel = k_pos[None, :] - q_pos[:, None]
    visible = always[None, :] | (in_range[None, :] & (jnp.abs(rel) <= WINDOW))
    n_q, n_k = rel.shape
    bias = jnp.transpose(rel_bias[rel_bucket(rel)], (2, 0, 1)).reshape(
        SWA_KV_HEADS, SWA_GROUP, n_q, n_k).astype(jnp.float32)
    s = jnp.einsum('bqhgd,bnhd->bhgqn', q, k).astype(jnp.float32) * SWA_HEAD_DIM ** -0.5 + bias
    s = jnp.where(visible, s, NEG)
    sink_col = jnp.broadcast_to(sink.astype(jnp.float32).reshape(SWA_KV_HEADS, SWA_GROUP, 1, 1),
                                s.shape[:-1] + (1,))
    p = jax.nn.softmax(jnp.concatenate([s, sink_col], -1), axis=-1)[..., :-1].astype(v.dtype)
    return jnp.einsum('bhgqn,bnhd->bqhgd', p, v)


def swa_branch(q, k, v, rel_bias, sink):
    bsz, n_tok, _ = q.shape
    n_real = n_tok - N_META
    n_blk = n_real // BLOCK
    q = q.reshape(bsz, n_tok, SWA_KV_HEADS, SWA_GROUP, SWA_HEAD_DIM)
    k = k.reshape(bsz, n_tok, SWA_KV_HEADS, SWA_HEAD_DIM)
    v = v.reshape(bsz, n_tok, SWA_KV_HEADS, SWA_HEAD_DIM)
    k_meta, v_meta = k[:, :N_META], v[:, :N_META]
    meta_pos = jnp.arange(N_META)
    mk_pos = jnp.arange(N_META + BLOCK)
    o_meta = swa_attend(q[:, :N_META], k[:, :N_META + BLOCK], v[:, :N_META + BLOCK], meta_pos, mk_pos,
                        mk_pos < N_META, jnp.ones((N_META + BLOCK,), bool), rel_bias, sink)
    pad = ((0, 0), (BLOCK, BLOCK), (0, 0), (0, 0))
    k_pad = jnp.pad(k[:, N_META:], pad)
    v_pad = jnp.pad(v[:, N_META:], pad)
    band = jnp.arange(3 * BLOCK)
    always = jnp.concatenate([jnp.ones((N_META,), bool), jnp.zeros((3 * BLOCK,), bool)])
    q_blocks = jnp.moveaxis(q[:, N_META:].reshape(bsz, n_blk, BLOCK, SWA_KV_HEADS, SWA_GROUP,
                                                  SWA_HEAD_DIM), 1, 0)

    def block(args):
        qb, i = args
        start = i * BLOCK
        kb = lax.dynamic_slice_in_dim(k_pad, start, 3 * BLOCK, axis=1)
        vb = lax.dynamic_slice_in_dim(v_pad, start, 3 * BLOCK, axis=1)
        r_key = start - BLOCK + band
        k_pos = jnp.concatenate([meta_pos, N_META + r_key])
        in_range = jnp.concatenate([jnp.ones((N_META,), bool), (r_key >= 0) & (r_key < n_real)])
        q_pos = N_META + start + jnp.arange(BLOCK)
        return swa_attend(qb, jnp.concatenate([k_meta, kb], 1), jnp.concatenate([v_meta, vb], 1),
                          q_pos, k_pos, always, in_range, rel_bias, sink)

    o_real = lax.map(block, (q_blocks, jnp.arange(n_blk)))
    o_real = jnp.moveaxis(o_real, 0, 1).reshape(bsz, n_real, SWA_KV_HEADS, SWA_GROUP, SWA_HEAD_DIM)
    return jnp.concatenate([o_meta, o_real], 1).reshape(bsz, n_tok, SWA_HEADS * SWA_HEAD_DIM)


def mixer(h, w_in, q_norm_g, kv_norm_g, w_uq, w_ukv, sink, w_proj_a, w_proj_b, w_out,
          rel_bias, cos, sin):
    z = h @ w_in
    points = [int(p) for p in np.cumsum(SPLITS)[:-1]]
    c_q, c_kv, k_r, q_b, k_b, v_b, g_a, g_b = jnp.split(z, points, axis=-1)
    o_a = mla_branch(c_q, c_kv, k_r, q_norm_g, kv_norm_g, w_uq, w_ukv, cos, sin)
    o_b = swa_branch(q_b, k_b, v_b, rel_bias, sink)
    merged = jax.nn.sigmoid(g_a) * (o_a @ w_proj_a) + jax.nn.sigmoid(g_b) * (o_b @ w_proj_b)
    return merged @ w_out


def dense_swiglu(h, w1, w3, w2):
    def per_seq(hb):
        return (jax.nn.silu(hb @ w1) * (hb @ w3)) @ w2
    return lax.map(per_seq, h)


def moe_swiglu(h, router_w, router_b, w1, w3, w2):
    bsz, n_tok, dm = h.shape
    n = bsz * n_tok
    xt = h.reshape(n, dm)
    logits = (xt @ router_w).astype(jnp.float32) + router_b.astype(jnp.float32)
    top_val, top_idx = lax.top_k(logits, TOP_K)
    gate = jax.nn.softmax(top_val, axis=-1)
    n_assign = n * TOP_K
    e_flat = top_idx.reshape(n_assign)
    tok_flat = jnp.arange(n_assign) // TOP_K
    g_flat = gate.reshape(n_assign)
    order = jnp.argsort(e_flat)
    e_sorted = e_flat[order]
    counts = jnp.bincount(e_flat, length=N_EXPERTS)
    padded = (counts + MOE_BLOCK - 1) // MOE_BLOCK * MOE_BLOCK
    pad_end = jnp.cumsum(padded)
    pad_start = pad_end - padded
    start = jnp.cumsum(counts) - counts
    dest = pad_start[e_sorted] + jnp.arange(n_assign) - start[e_sorted]
    n_blocks = -(-n_assign // MOE_BLOCK) + N_EXPERTS
    n_slots = n_blocks * MOE_BLOCK
    slot_tok = jnp.full((n_slots,), n, jnp.int32).at[dest].set(tok_flat[order].astype(jnp.int32))
    slot_gate = jnp.zeros((n_slots,), jnp.float32).at[dest].set(g_flat[order])
    block_expert = jnp.minimum(jnp.searchsorted(pad_end, jnp.arange(n_blocks) * MOE_BLOCK, side='right'),
                               N_EXPERTS - 1)
    x_pad = jnp.concatenate([xt, jnp.zeros((1, dm), xt.dtype)], 0)

    def expert_block(args):
        toks, e = args
        xb = x_pad[toks]
        return (jax.nn.silu(xb @ w1[e]) * (xb @ w3[e])) @ w2[e]

    y = lax.map(expert_block, (slot_tok.reshape(n_blocks, MOE_BLOCK), block_expert)).reshape(n_slots, dm)
    out = jax.ops.segment_sum(y * slot_gate[:, None].astype(y.dtype), slot_tok, num_segments=n + 1)[:n]
    return out.reshape(bsz, n_tok, dm)


def setup_inputs(seed: int = 0) -> dict:
    key = jax.random.key(seed)
    ks = jax.random.split(key, 26)

    def nrm(k, shape, scale):
        return jax.random.normal(k, shape, jnp.float32) * scale

    d = D_MODEL
    return {
        'x': nrm(ks[0], (BATCH, SEQ, d), 1.0),
        'meta_tokens': nrm(ks[1], (N_META, d), 1.0),
        'emb_ln_g': 1.0 + nrm(ks[2], (d,), 0.1),
        'emb_ln_b': nrm(ks[3], (d,), 0.02),
        'rel_bias': nrm(ks[4], (N_BUCKETS, SWA_HEADS), 0.5),
        'w_in': nrm(ks[5], (DEPTH, d, IN_DIM), d ** -0.5),
        'q_norm_g': 1.0 + nrm(ks[6], (DEPTH, MLA_Q_RANK), 0.1),
        'kv_norm_g': 1.0 + nrm(ks[7], (DEPTH, MLA_KV_RANK), 0.1),
        'w_uq': nrm(ks[8], (DEPTH, MLA_Q_RANK, MLA_HEADS * (MLA_NOPE_DIM + MLA_ROPE_DIM)), MLA_Q_RANK ** -0.5),
        'w_ukv': nrm(ks[9], (DEPTH, MLA_KV_RANK, MLA_HEADS * (MLA_NOPE_DIM + MLA_V_DIM)), MLA_KV_RANK ** -0.5),
        'sink_logits': nrm(ks[10], (DEPTH, SWA_HEADS), 0.5),
        'w_proj_a': nrm(ks[11], (DEPTH, MLA_HEADS * MLA_V_DIM, d), BETA * (MLA_HEADS * MLA_V_DIM) ** -0.5),
        'w_proj_b': nrm(ks[12], (DEPTH, SWA_HEADS * SWA_HEAD_DIM, d), BETA * (SWA_HEADS * SWA_HEAD_DIM) ** -0.5),
        'w_out': nrm(ks[13], (DEPTH, d, d), BETA * d ** -0.5),
        'ln_mix_g': 1.0 + nrm(ks[14], (DEPTH, d), 0.1),
        'ln_mix_b': nrm(ks[15], (DEPTH, d), 0.02),
        'ln_ffn_g': 1.0 + nrm(ks[16], (DEPTH, d), 0.1),
        'ln_ffn_b': nrm(ks[17], (DEPTH, d), 0.02),
        'ffn_w1': nrm(ks[18], (N_DENSE, d, D_FF), d ** -0.5),
        'ffn_w3': nrm(ks[19], (N_DENSE, d, D_FF), d ** -0.5),
        'ffn_w2': nrm(ks[20], (N_DENSE, D_FF, d), BETA * D_FF ** -0.5),
        'router_w': nrm(ks[21], (N_MOE, d, N_EXPERTS), d ** -0.5),
        'router_b': nrm(ks[22], (N_MOE, N_EXPERTS), 0.01),
        'moe_w1': nrm(ks[23], (N_MOE, N_EXPERTS, d, D_FF), d ** -0.5),
        'moe_w3': nrm(ks[24], (N_MOE, N_EXPERTS, d, D_FF), d ** -0.5),
        'moe_w2': nrm(ks[25], (N_MOE, N_EXPERTS, D_FF, d), BETA * D_FF ** -0.5),
    }


def reference(x, meta_tokens, emb_ln_g, emb_ln_b, rel_bias, w_in, q_norm_g, kv_norm_g, w_uq, w_ukv,
              sink_logits, w_proj_a, w_proj_b, w_out, ln_mix_g, ln_mix_b, ln_ffn_g, ln_ffn_b,
              ffn_w1, ffn_w3, ffn_w2, router_w, router_b, moe_w1, moe_w3, moe_w2):
    bsz = x.shape[0]
    meta = jnp.broadcast_to(meta_tokens[None].astype(x.dtype), (bsz, N_META, x.shape[-1]))
    h = layer_norm(jnp.concatenate([meta, x], 1), emb_ln_g, emb_ln_b)
    cos, sin = rope_tables(h.shape[1])
    for l in range(DEPTH):
        m = mixer(h, w_in[l], q_norm_g[l], kv_norm_g[l], w_uq[l], w_ukv[l], sink_logits[l],
                  w_proj_a[l], w_proj_b[l], w_out[l], rel_bias, cos, sin)
        h = layer_norm(ALPHA * h + m, ln_mix_g[l], ln_mix_b[l])
        if l % 2 == 0:
            f = dense_swiglu(h, ffn_w1[l // 2], ffn_w3[l // 2], ffn_w2[l // 2])
        else:
            f = moe_swiglu(h, router_w[l // 2], router_b[l // 2], moe_w1[l // 2], moe_w3[l // 2],
                           moe_w2[l // 2])
        h = layer_norm(ALPHA * h + f, ln_ffn_g[l], ln_ffn_b[l])
    return h[:, N_META:]
```

```python
import math
import os
from contextlib import ExitStack
import numpy as np
import concourse.bass as bass
import concourse.mybir as mybir
from concourse.bass_utils import run_bass_kernel_spmd

F32 = mybir.dt.float32
BF16 = mybir.dt.bfloat16
ALU = mybir.AluOpType
AF = mybir.ActivationFunctionType
AX = mybir.AxisListType

NEGV = -1.0e30


class Cfg:
    def __init__(self, **kw):
        self.D = 2048
        self.SEQ = 4096
        self.DEPTH = 2
        self.NMETA = 16
        self.MH = 16
        self.NOPE = 128
        self.ROPE = 64
        self.VD = 128
        self.QR = 768
        self.KVR = 512
        self.SH = 16
        self.SKV = 4
        self.HD = 128
        self.DFF = 5632
        self.NE = 8
        self.NCORES = 8
        self.SUPER = 1024
        self.FFPART = 11
        self.CAP = 1536
        for k, v in kw.items():
            setattr(self, k, v)
        self.T = self.SEQ + self.NMETA
        self.NB = self.SEQ // 128
        self.KC = self.D // 128
        self.GRP = self.SH // self.SKV
        self.chunks = [(c0, 512) for c0 in range(0, self.SEQ, 512)] + [(self.SEQ, self.NMETA)]
        self.IN_DIM = self.QR + self.KVR + self.ROPE + self.SH * self.HD + 2 * self.SKV * self.HD + 2 * self.D
        self.LN_EPS = 1e-5
        self.RMS_EPS = 1e-6
        self.ALPHA = (2 * self.DEPTH) ** 0.25


class Buf:
    __slots__ = ("name", "w", "r", "sem", "cnt")

    def __init__(self, name):
        self.name = name
        self.w = {}
        self.r = {}
        self.sem = None
        self.cnt = 0


class Tl:
    __slots__ = ("b", "h")

    def __init__(self, b, h):
        self.b = b
        self.h = h


ENGS = ("pe", "act", "dve", "pool", "sp")


class KB:
    def __init__(self, nc, stack, sb_limit):
        self.nc = nc
        self.stack = stack
        self.q = {e: [] for e in ENGS}
        self.cnt = {e: 0 for e in ENGS}
        self.clock = {e: {} for e in ENGS}
        self.semh = {}
        self.esem = {}
        for e in ENGS:
            self.esem[e] = self.newsem("E_" + e)
        self.dfree = []
        self.dtotal = {}
        self.dall = []
        self.phase_bufs = []
        self.sb_base = 16512
        self.sb_off = 16512
        self.sb_limit = sb_limit
        self.uid = 0
        self.ninst = 0

    def newsem(self, name):
        h = self.stack.enter_context(self.nc.semaphore(name))
        self.semh[name] = h
        return name

    def sb(self, name, shape, dtype, persistent=False):
        esz = 4 if dtype == F32 else 2
        n = 1
        for s in shape[1:]:
            n *= s
        nbytes = (n * esz + 63) // 64 * 64
        self.uid += 1
        off = self.sb_off
        assert off + nbytes <= self.sb_limit, f"SBUF overflow allocating {name}: {off}+{nbytes}"
        h = self.nc.alloc_sbuf_tensor_at(f"{name}_{self.uid}", list(shape), dtype, offset=off)
        self.sb_off = off + nbytes
        if persistent:
            assert self.sb_base == off, "persistent allocs must come first"
            self.sb_base = self.sb_off
        b = Buf(name)
        if not persistent:
            self.phase_bufs.append(b)
        return Tl(b, h)

    def _deps(self, eng, reads, writes):
        mysem = self.esem[eng]
        ck = self.clock[eng]
        waits = {}

        def need(sem, val):
            if ck.get(sem, 0) >= val:
                return
            ck[sem] = val
            waits[sem] = val

        for t in reads:
            for sem, val in t.b.w.items():
                if sem == mysem and eng == "pe":
                    continue
                need(sem, val)
        for t in writes:
            for sem, val in t.b.w.items():
                if sem == mysem:
                    continue
                need(sem, val)
            for sem, val in t.b.r.items():
                if sem == mysem:
                    continue
                need(sem, val)
        return list(waits.items())

    def _mark(self, ev, reads, writes):
        sem, val = ev
        for t in reads:
            if t.b.r.get(sem, 0) < val:
                t.b.r[sem] = val
        for t in writes:
            t.b.w = {sem: val}
            t.b.r = {}

    def op(self, eng, fn, reads=(), writes=(), inc=True):
        waits = self._deps(eng, reads, writes)
        idx = self.cnt[eng] + 1
        if inc:
            self.cnt[eng] = idx
        ev = (self.esem[eng], idx)
        self.q[eng].append((waits, fn, (self.esem[eng], 1) if inc else None))
        self._mark(ev, reads, writes)
        self.ninst += 1
        return ev

    def dma(self, eng, out, in_, owner, reads=(), writes=(), **kw):
        b = owner.b
        if b.sem is None:
            if self.dfree:
                b.sem = self.dfree.pop()
            else:
                b.sem = self.newsem(f"D{len(self.dall)}")
                self.dall.append(b.sem)
                self.dtotal[b.sem] = 0
        waits = self._deps(eng, reads, writes)
        self.dtotal[b.sem] += 16
        ev = (b.sem, self.dtotal[b.sem])
        self.q[eng].append((waits, (lambda e: e.dma_start(out=out, in_=in_, **kw)), (b.sem, 16)))
        self._mark(ev, reads, writes)
        self.ninst += 1
        return ev

    def barrier(self):
        for e in ENGS:
            waits = []
            ck = self.clock[e]
            for e2 in ENGS:
                if e2 == e or self.cnt[e2] == 0:
                    continue
                s = self.esem[e2]
                if ck.get(s, 0) < self.cnt[e2]:
                    ck[s] = self.cnt[e2]
                    waits.append((s, self.cnt[e2]))
            for s in self.dall:
                v = self.dtotal[s]
                if v > 0 and ck.get(s, 0) < v:
                    ck[s] = v
                    waits.append((s, v))
            if waits:
                self.q[e].append((waits, None, None))
        for b in self.phase_bufs:
            if b.sem is not None:
                self.dfree.append(b.sem)
                b.sem = None
        self.phase_bufs = []
        self.sb_off = self.sb_base

    def emit(self):
        nc = self.nc
        engmap = {"pe": "tensor", "act": "scalar", "dve": "vector", "pool": "gpsimd", "sp": "sync"}
        with nc.Block() as block:
            for e in ENGS:
                q = self.q[e]
                semh = self.semh

                def body(eh, q=q):
                    for waits, fn, inc in q:
                        for s, v in waits:
                            eh.wait_ge(semh[s], v)
                        if fn is not None:
                            ins = fn(eh)
                            if inc is not None:
                                ins.then_inc(semh[inc[0]], inc[1])

                getattr(block, engmap[e])(body)


class Prog:
    def __init__(self, cfg, debug=None, stop_after=None):
        self.cfg = cfg
        self.debug = debug or []
        self.stop_after = stop_after
        self.nc = bass.Bass("TRN2", target_bir_lowering=False)
        self.stack = ExitStack()
        self.k = KB(self.nc, self.stack, sb_limit=229376 - 256)
        self.dr = {}

    def din(self, name, shape, dtype=F32):
        t = self.nc.dram_tensor(name, list(shape), dtype, kind="ExternalInput").ap()
        self.dr[name] = t
        return t

    def dscr(self, name, shape, dtype):
        kind = "ExternalOutput" if name in self.debug else "Internal"
        t = self.nc.dram_tensor(name, list(shape), dtype, kind=kind).ap()
        self.dr[name] = t
        return t

    def fm(self, ap):
        return ap.rearrange("(kc p) t -> p kc t", p=128)

    def mm(self, ps, ps_ap, lhsT_ap, rhs_ap, reads, start, stop, inc=None):
        self.k.op("pe", lambda e: e.matmul(ps_ap, lhsT_ap, rhs_ap, start=start, stop=stop),
                  reads=reads, writes=[ps], inc=(stop if inc is None else inc))

    def act(self, out_t, out_ap, in_t, in_ap, func, extra_reads=(), **kw):
        self.k.op("act", lambda e: e.activation(out_ap, in_ap, func, **kw),
                  reads=[in_t, *extra_reads], writes=[out_t])

    def tt(self, out_t, out_ap, a_t, a_ap, b_t, b_ap, op, eng="dve"):
        self.k.op(eng, lambda e: e.tensor_tensor(out_ap, a_ap, b_ap, op), reads=[a_t, b_t], writes=[out_t])

    def ts(self, out_t, out_ap, a_t, a_ap, s1, s2, op0, op1=None, extra_reads=(), eng="dve"):
        if op1 is None:
            self.k.op(eng, lambda e: e.tensor_scalar(out_ap, a_ap, s1, None, op0),
                      reads=[a_t, *extra_reads], writes=[out_t])
        else:
            self.k.op(eng, lambda e: e.tensor_scalar(out_ap, a_ap, s1, s2, op0, op1),
                      reads=[a_t, *extra_reads], writes=[out_t])

    def cp(self, out_t, out_ap, in_t, in_ap, eng="dve"):
        if eng == "act":
            self.k.op("act", lambda e: e.copy(out_ap, in_ap), reads=[in_t], writes=[out_t])
        else:
            self.k.op(eng, lambda e: e.tensor_copy(out_ap, in_ap), reads=[in_t], writes=[out_t])

    def load(self, dst_t, dst_ap, src_ap, eng="sp"):
        self.k.dma(eng, dst_ap, src_ap, owner=dst_t, writes=[dst_t])

    def store(self, dst_ap, src_t, src_ap, eng="sp"):
        self.k.dma(eng, dst_ap, src_ap, owner=src_t, reads=[src_t])

    def build(self):
        cfg = self.cfg
        k = self.k
        nc = self.nc
        D, T, KC = cfg.D, cfg.T, cfg.KC
        L = cfg.DEPTH
        x = self.din("x", [cfg.SEQ, D])
        meta = self.din("meta", [cfg.NMETA, D])
        consts = self.din("consts", [128, 3 * 128])
        self.iotac = self.din("iotac", [128, cfg.CAP])
        self.slotid = self.din("slotid", [128, cfg.CAP // 128])
        pvec = self.din("pvec", [128, self.npvec()])
        ropet = self.din("ropet", [2, 64, T])
        biasd = self.din("biasd", [cfg.SKV, 3, 128, cfg.GRP * 128])
        biasmk = self.din("biasmk", [cfg.SKV, 2, 16, cfg.GRP * 128])
        biasqm = self.din("biasqm", [cfg.SKV, 144, cfg.GRP * 16])
        sinkb = self.din("sinkb", [L, 128, cfg.SH])
        w_in = self.din("w_in", [L, D, cfg.IN_DIM + cfg.ROPE])
        w_uq = self.din("w_uq", [L, cfg.QR, cfg.MH * (cfg.NOPE + 2 * cfg.ROPE)])
        w_ukv = self.din("w_ukv", [L, cfg.KVR, cfg.MH * (cfg.NOPE + cfg.VD)])
        w_pa = self.din("w_pa", [L, cfg.MH * cfg.VD, D])
        w_pb = self.din("w_pb", [L, cfg.SH * cfg.HD, D])
        w_out = self.din("w_out", [L, D, D])
        n_dense = (L + 1) // 2
        n_moe = L // 2
        ffn_w1 = self.din("ffn_w1", [n_dense, D, cfg.DFF])
        ffn_w3 = self.din("ffn_w3", [n_dense, D, cfg.DFF])
        ffn_w2 = self.din("ffn_w2", [n_dense, cfg.DFF, D])
        if n_moe:
            router_w = self.din("router_w", [n_moe, D, cfg.NE])
            router_bb = self.din("router_bb", [n_moe, 128, cfg.NE])
            moe_w1 = self.din("moe_w1", [n_moe * cfg.NE, D, cfg.DFF])
            moe_w3 = self.din("moe_w3", [n_moe * cfg.NE, D, cfg.DFF])
            moe_w2 = self.din("moe_w2", [n_moe * cfg.NE, cfg.DFF, D])
        out = self.nc.dram_tensor("out", [cfg.SEQ, D], F32, kind="ExternalOutput").ap()
        s = self.s = {}
        s["hres"] = self.dscr("hres", [D, T], F32)
        s["hb"] = self.dscr("hb", [D, T], BF16)
        s["yT"] = self.dscr("yT", [D, T], F32)
        s["cqT"] = self.dscr("cqT", [cfg.QR, T], BF16)
        s["ckvT"] = self.dscr("ckvT", [cfg.KVR, T], BF16)
        s["krT"] = self.dscr("krT", [cfg.ROPE, T], BF16)
        s["qsT"] = self.dscr("qsT", [cfg.SH * cfg.HD, T], BF16)
        s["ksT"] = self.dscr("ksT", [cfg.SKV * cfg.HD, T], BF16)
        s["vs"] = self.dscr("vs", [T, cfg.SKV * cfg.HD], BF16)
        s["gaT"] = self.dscr("gaT", [D, T], BF16)
        s["gbT"] = self.dscr("gbT", [D, T], BF16)
        s["qnT"] = self.dscr("qnT", [cfg.MH * cfg.NOPE, T], BF16)
        s["qrT"] = self.dscr("qrT", [cfg.MH * cfg.ROPE, T], BF16)
        s["knT"] = self.dscr("knT", [cfg.MH * cfg.NOPE, T], BF16)
        s["vm"] = self.dscr("vm", [T, cfg.MH * cfg.VD], BF16)
        s["oaT"] = self.dscr("oaT", [cfg.MH * cfg.VD, T], BF16)
        s["obT"] = self.dscr("obT", [cfg.SH * cfg.HD, T], BF16)
        s["tAT"] = self.dscr("tAT", [D, T], F32)
        s["mgT"] = self.dscr("mgT", [D, T], BF16)
        s["gtT"] = self.dscr("gtT", [max(cfg.NE, 1), T], F32)
        s["pmT"] = self.dscr("pmT", [cfg.NE, T], F32)
        s["pmtm"] = self.dscr("pmtm", [T, cfg.NE], F32)
        s["htm"] = self.dscr("htm", [T, D], BF16)
        s["XeT"] = self.dscr("XeT", [cfg.NE * D, cfg.CAP], BF16)
        s["Yd"] = self.dscr("Yd", [cfg.NE * cfg.CAP, D], BF16)

        P = self.P = {}
        P["ident"] = k.sb("ident", [128, 128], F32, persistent=True)
        P["ones"] = k.sb("ones", [128, 128], F32, persistent=True)
        P["identb"] = k.sb("identb", [128, 128], BF16, persistent=True)
        P["onesb"] = k.sb("onesb", [128, 128], BF16, persistent=True)
        P["pvec"] = k.sb("pvec", [128, self.npvec()], F32, persistent=True)
        P["upper"] = k.sb("upper", [128, 128], F32, persistent=True)
        self.ps = [Tl(Buf(f"ps{i}"), nc.alloc_psum_tensor(f"ps{i}", [128, 512], F32)) for i in range(8)]
        self.load(P["ident"], P["ident"].h[:, :], consts[:, 0:128])
        self.load(P["ones"], P["ones"].h[:, :], consts[:, 128:256])
        self.load(P["identb"], P["identb"].h[:, :], consts[:, 0:128], eng="pool")
        self.load(P["onesb"], P["onesb"].h[:, :], consts[:, 128:256], eng="pool")
        self.load(P["pvec"], P["pvec"].h[:, :], pvec[:, :])
        self.load(P["upper"], P["upper"].h[:, :], consts[:, 256:384])
        self.ropet = ropet

        pv = self.pvoff()
        self.phase_embed(x, meta, pv["emb_g"], pv["emb_b"])
        k.barrier()
        done = self.stop_after == "embed"
        for l in range(L):
            if done:
                break
            self.phase_inproj(l, w_in[l])
            k.barrier()
            if self.stop_after == f"inproj{l}":
                break
            self.phase_mla_up(l, w_uq[l], w_ukv[l], pv[f"qn_g{l}"], pv[f"kvn_g{l}"])
            k.barrier()
            if self.stop_after == f"mlaup{l}":
                break
            self.phase_mla_attn(l)
            k.barrier()
            if self.stop_after == f"mlaattn{l}":
                break
            self.phase_swa(l, biasd, biasmk, biasqm, sinkb[l])
            k.barrier()
            if self.stop_after == f"swa{l}":
                break
            self.phase_proj(l, w_pa[l], w_pb[l], w_out[l])
            k.barrier()
            self.phase_ln(pv[f"mix_g{l}"], pv[f"mix_b{l}"])
            k.barrier()
            if self.stop_after == f"mix{l}":
                break
            if l % 2 == 0:
                self.phase_ffn([(ffn_w1[l // 2], ffn_w3[l // 2], ffn_w2[l // 2])], gated=False)
            else:
                self.phase_router(router_w[l // 2], router_bb[l // 2])
                k.barrier()
                if self.stop_after == f"router{l}":
                    break
                m = l // 2
                self.phase_gather()
                k.barrier()
                if self.stop_after == f"gather{l}":
                    break
                self.phase_moe_ffn([(moe_w1[m * cfg.NE + e], moe_w3[m * cfg.NE + e], moe_w2[m * cfg.NE + e])
                                    for e in range(cfg.NE)])
                k.barrier()
                if self.stop_after == f"moeffn{l}":
                    break
                self.phase_combine()
                if self.stop_after == f"combine{l}":
                    k.barrier()
                    break
            k.barrier()
            self.phase_ln(pv[f"ffn_g{l}"], pv[f"ffn_b{l}"])
            k.barrier()
        self.phase_output(out)
        k.barrier()
        k.emit()
        self.stack.close()
        return nc

    def pv_layout(self):
        cfg = self.cfg
        items = [("emb_g", cfg.KC), ("emb_b", cfg.KC)]
        for l in range(cfg.DEPTH):
            items += [(f"mix_g{l}", cfg.KC), (f"mix_b{l}", cfg.KC), (f"ffn_g{l}", cfg.KC), (f"ffn_b{l}", cfg.KC),
                      (f"qn_g{l}", cfg.QR // 128), (f"kvn_g{l}", cfg.KVR // 128)]
        return items

    def npvec(self):
        return sum(n for _, n in self.pv_layout())

    def pvoff(self):
        o = {}
        off = 0
        for name, n in self.pv_layout():
            o[name] = (off, n)
            off += n
        return o

    def ln_fm(self, y, n, gsl, bsl, c0, tmp):
        cfg, k, P = self.cfg, self.k, self.P
        KC, D = cfg.KC, cfg.D
        sq, mean, rstd, mr, msq, ob = tmp
        yv = y.h[:, :, 0:n]
        self.act(sq, sq.h[:, :, 0:n], y, yv, AF.Square)
        ps_s, ps_q = self.ps[6], self.ps[7]
        for kc in range(KC):
            self.mm(ps_s, ps_s.h[:, 0:n], P["ones"].h[:, :], y.h[:, kc, 0:n], [P["ones"], y], kc == 0, kc == KC - 1)
        for kc in range(KC):
            self.mm(ps_q, ps_q.h[:, 0:n], P["ones"].h[:, :], sq.h[:, kc, 0:n], [P["ones"], sq], kc == 0, kc == KC - 1)
        self.ts(mean, mean.h[:, 0:n], ps_s, ps_s.h[:, 0:n], 1.0 / D, None, ALU.mult)
        self.tt(msq, msq.h[:, 0:n], mean, mean.h[:, 0:n], mean, mean.h[:, 0:n], ALU.mult)
        k.op("dve", lambda e: e.scalar_tensor_tensor(rstd.h[:, 0:n], ps_q.h[:, 0:n], 1.0 / D, msq.h[:, 0:n],
                                                     ALU.mult, ALU.subtract),
             reads=[ps_q, msq], writes=[rstd])
        self.ts(rstd, rstd.h[:, 0:n], rstd, rstd.h[:, 0:n], cfg.LN_EPS, None, ALU.add)
        self.act(rstd, rstd.h[:, 0:n], rstd, rstd.h[:, 0:n], AF.Sqrt)
        k.op('dve', lambda e: e.reciprocal(rstd.h[:, 0:n], rstd.h[:, 0:n]), reads=[rstd], writes=[rstd])
        self.tt(mr, mr.h[:, 0:n], mean, mean.h[:, 0:n], rstd, rstd.h[:, 0:n], ALU.mult)
        rb = rstd.h[:, 0:n].unsqueeze(1).to_broadcast([128, KC, n])
        mb = mr.h[:, 0:n].unsqueeze(1).to_broadcast([128, KC, n])
        self.tt(y, yv, y, yv, rstd, rb, ALU.mult)
        self.tt(y, yv, y, yv, mr, mb, ALU.subtract)
        pvh = P["pvec"]
        gb = pvh.h[:, gsl[0]:gsl[0] + KC].unsqueeze(2).to_broadcast([128, KC, n])
        bb = pvh.h[:, bsl[0]:bsl[0] + KC].unsqueeze(2).to_broadcast([128, KC, n])
        self.tt(y, yv, y, yv, pvh, gb, ALU.mult)
        self.tt(y, yv, y, yv, pvh, bb, ALU.add)
        self.cp(ob, ob.h[:, :, 0:n], y, yv, eng="act")
        self.store(self.fm(self.s["hres"])[:, :, c0:c0 + n], y, yv)
        self.store(self.fm(self.s["hb"])[:, :, c0:c0 + n], ob, ob.h[:, :, 0:n])

    def ln_tmps(self):
        k, KC = self.k, self.cfg.KC
        return (k.sb("sq", [128, KC, 512], F32), k.sb("mean", [128, 512], F32), k.sb("rstd", [128, 512], F32),
                k.sb("mr", [128, 512], F32), k.sb("msq", [128, 512], F32), k.sb("ob", [128, KC, 512], BF16))

    def phase_embed(self, x, meta, gsl, bsl):
        cfg, k, P = self.cfg, self.k, self.P
        KC = cfg.KC
        xin = [k.sb(f"xin{i}", [128, 4, cfg.D], F32) for i in range(1)]
        ys = [k.sb(f"y{i}", [128, KC, 512], F32) for i in range(2)]
        tmp = self.ln_tmps()
        for ci, (c0, n) in enumerate(cfg.chunks):
            xt = xin[0]
            y = ys[ci % 2]
            if n == 512:
                self.load(xt, xt.h[:, :, :], x[c0:c0 + 512, :].rearrange("(a p) d -> p a d", p=128))
                nt, rows = 4, 128
            else:
                self.load(xt, xt.h[0:n, 0, :], meta[:, :])
                nt, rows = 1, n
            for kc in range(KC):
                ps = self.ps[kc % 4]
                for a in range(nt):
                    self.mm(ps, ps.h[:, a * 128:a * 128 + rows], xt.h[0:rows, a, kc * 128:(kc + 1) * 128],
                            P["ident"].h[0:rows, 0:rows], [xt, P["ident"]], True, True)
                self.cp(y, y.h[:, kc, 0:n], ps, ps.h[:, 0:n], eng=("act" if kc % 2 else "dve"))
            self.ln_fm(y, n, gsl, bsl, c0, tmp)

    def phase_ln(self, gsl, bsl):
        cfg, k = self.cfg, self.k
        KC = cfg.KC
        ys = [k.sb(f"y{i}", [128, KC, 512], F32) for i in range(2)]
        tmp = self.ln_tmps()
        for ci, (c0, n) in enumerate(cfg.chunks):
            y = ys[ci % 2]
            self.load(y, y.h[:, :, 0:n], self.fm(self.s["yT"])[:, :, c0:c0 + n])
            self.ln_fm(y, n, gsl, bsl, c0, tmp)

    def phase_output(self, out):
        cfg, k, P = self.cfg, self.k, self.P
        KC = cfg.KC
        hs = [k.sb(f"h{i}", [128, KC, 512], F32) for i in range(2)]
        os_ = [k.sb(f"o{i}", [128, cfg.D], F32) for i in range(2)]
        cnt = 0
        for ci, (c0, n) in enumerate(cfg.chunks):
            if n != 512:
                continue
            h = hs[ci % 2]
            self.load(h, h.h[:, :, :], self.fm(self.s["hres"])[:, :, c0:c0 + n])
            for a in range(4):
                o = os_[cnt % 2]
                cnt += 1
                for g in range(0, KC, 4):
                    ps = self.ps[(g // 4) % 4]
                    for j in range(min(4, KC - g)):
                        self.mm(ps, ps.h[:, j * 128:(j + 1) * 128], h.h[:, g + j, a * 128:(a + 1) * 128],
                                P["ident"].h[:, :], [h, P["ident"]], True, True)
                    w = min(4, KC - g) * 128
                    self.cp(o, o.h[:, g * 128:g * 128 + w], ps, ps.h[:, 0:w], eng=("act" if (g // 4) % 2 else "dve"))
                self.store(out[c0 + a * 128:c0 + (a + 1) * 128, :], o, o.h[:, :])

    def load_x(self, name, dram, K):
        cfg, k = self.cfg, self.k
        kc = K // 128
        X = k.sb(name, [128, kc, cfg.T], BF16)
        v = self.fm(dram)
        step = 1024
        for c0 in range(0, cfg.T, step):
            n = min(step, cfg.T - c0)
            k.dma("sp", X.h[:, :, c0:c0 + n], v[:, :, c0:c0 + n], owner=X, writes=[])
        X.b.w = {X.b.sem: k.dtotal[X.b.sem]}
        return X

    def linear(self, X, KCn, W, ftiles, epi, wbufs, gw=512, chunks=None, xoff=0):
        cfg, k = self.cfg, self.k
        chunks = chunks if chunks is not None else cfg.chunks
        Wv = W.rearrange("(kc p) n -> p kc n", p=128)
        groups = []
        cur = []
        for fi, (col0, M) in enumerate(ftiles):
            if cur and (col0 != cur[-1][1] + cur[-1][2] or (col0 + M - cur[0][1]) > gw):
                groups.append(cur)
                cur = []
            cur.append((fi, col0, M))
        if cur:
            groups.append(cur)
        pi = 0
        for gi, grp in enumerate(groups):
            wt = wbufs[gi % 2]
            g0 = grp[0][1]
            gwid = grp[-1][1] + grp[-1][2] - g0
            self.load(wt, wt.h[:, :, 0:gwid], Wv[:, :, g0:g0 + gwid], eng="pool")
            for (fi, col0, M) in grp:
                for ci, (c0, n) in enumerate(chunks):
                    ps = self.ps[pi % 4]
                    pi += 1
                    for kc in range(KCn):
                        self.mm(ps, ps.h[0:M, 0:n], wt.h[:, kc, col0 - g0:col0 - g0 + M],
                                X.h[:, kc, c0 - xoff:c0 - xoff + n], [wt, X], kc == 0, kc == KCn - 1)
                    epi(fi, ci, c0, n, ps)

    def linear_tm(self, X, KCn, W, col0, ncols, epi, wt):
        cfg = self.cfg
        Wv = W.rearrange("(kc p) n -> p kc n", p=128)
        self.load(wt, wt.h[:, :, 0:ncols], Wv[:, :, col0:col0 + ncols], eng="pool")
        ntile = (cfg.T + 127) // 128
        for ti in range(ntile):
            t0 = ti * 128
            rows = min(128, cfg.T - t0)
            ps = self.ps[ti % 4]
            for kc in range(KCn):
                self.mm(ps, ps.h[0:rows, 0:ncols], X.h[:, kc, t0:t0 + rows], wt.h[:, kc, 0:ncols], [wt, X],
                        kc == 0, kc == KCn - 1)
            epi(ti, t0, rows, ps)

    def load_rope(self):
        k, P = self.k, self.P
        P["cos"] = k.sb("cos", [64, self.cfg.T], F32)
        P["sin"] = k.sb("sin", [64, self.cfg.T], F32)
        self.load(P["cos"], P["cos"].h[:, :], self.ropet[0, :, :])
        self.load(P["sin"], P["sin"].h[:, :], self.ropet[1, :, :])

    def phase_inproj(self, l, W):
        cfg, k, P, s = self.cfg, self.k, self.P, self.s
        KC = cfg.KC
        X = self.load_x("X", s["hb"], cfg.D)
        self.load_rope()
        wb = [k.sb(f"w{i}", [128, KC, 256], BF16) for i in range(2)]
        st = [k.sb(f"st{i}", [128, 512], BF16) for i in range(4)]
        t2 = k.sb("t2", [64, 512], F32)
        sti = [0]

        def stage():
            t = st[sti[0] % 4]
            sti[0] += 1
            return t

        def mk_copy(dst, row0):
            def epi(fi, ci, c0, n, ps, dst=dst, row0=row0):
                t = stage()
                r = row0 + fi * 128
                self.cp(t, t.h[:, 0:n], ps, ps.h[:, 0:n], eng=("act" if sti[0] % 2 else "dve"))
                self.store(dst[r:r + 128, c0:c0 + n], t, t.h[:, 0:n])
            return epi

        def mk_sig(dst):
            def epi(fi, ci, c0, n, ps, dst=dst):
                t = stage()
                self.act(t, t.h[:, 0:n], ps, ps.h[:, 0:n], AF.Sigmoid)
                self.store(dst[fi * 128:(fi + 1) * 128, c0:c0 + n], t, t.h[:, 0:n])
            return epi

        o = 0
        self.linear(X, KC, W, [(o + i * 128, 128) for i in range(cfg.QR // 128)], mk_copy(s["cqT"], 0), wb, gw=256)
        o += cfg.QR
        self.linear(X, KC, W, [(o + i * 128, 128) for i in range(cfg.KVR // 128)], mk_copy(s["ckvT"], 0), wb, gw=256)
        o += cfg.KVR
        t1all = k.sb("t1all", [64, cfg.T], F32)

        def epi_r1(fi, ci, c0, n, ps):
            self.tt(t1all, t1all.h[:, c0:c0 + n], ps, ps.h[0:64, 0:n], P["cos"], P["cos"].h[:, c0:c0 + n], ALU.mult)

        def epi_r2(fi, ci, c0, n, ps):
            self.tt(t2, t2.h[:, 0:n], ps, ps.h[0:64, 0:n], P["sin"], P["sin"].h[:, c0:c0 + n], ALU.mult)
            t = stage()
            self.tt(t, t.h[0:64, 0:n], t1all, t1all.h[:, c0:c0 + n], t2, t2.h[:, 0:n], ALU.add)
            self.store(s["krT"][:, c0:c0 + n], t, t.h[0:64, 0:n])

        self.linear(X, KC, W, [(o, 64)], epi_r1, wb, gw=256)
        self.linear(X, KC, W, [(cfg.IN_DIM, 64)], epi_r2, wb, gw=256)
        o += cfg.ROPE
        self.linear(X, KC, W, [(o + i * 128, 128) for i in range(cfg.SH)], mk_copy(s["qsT"], 0), wb, gw=256)
        o += cfg.SH * cfg.HD
        self.linear(X, KC, W, [(o + i * 128, 128) for i in range(cfg.SKV)], mk_copy(s["ksT"], 0), wb, gw=256)
        o += cfg.SKV * cfg.HD
        ncv = cfg.SKV * cfg.HD

        def epi_v(ti, t0, rows, ps):
            t = stage()
            self.cp(t, t.h[0:rows, 0:ncv], ps, ps.h[0:rows, 0:ncv], eng=("act" if ti % 2 else "dve"))
            self.store(s["vs"][t0:t0 + rows, :], t, t.h[0:rows, 0:ncv])

        for v0 in range(0, ncv, 256):
            vn = min(256, ncv - v0)

            def epi_v(ti, t0, rows, ps, v0=v0, vn=vn):
                t = stage()
                self.cp(t, t.h[0:rows, 0:vn], ps, ps.h[0:rows, 0:vn], eng=('act' if ti % 2 else 'dve'))
                self.store(s['vs'][t0:t0 + rows, v0:v0 + vn], t, t.h[0:rows, 0:vn])

            self.linear_tm(X, KC, W, o + v0, vn, epi_v, wb[(v0 // 256) % 2])
        o += ncv
        self.linear(X, KC, W, [(o + i * 128, 128) for i in range(KC)], mk_sig(s["gaT"]), wb, gw=256)
        o += cfg.D
        self.linear(X, KC, W, [(o + i * 128, 128) for i in range(KC)], mk_sig(s["gbT"]), wb, gw=256)

    def rms_stats(self, X, KCn, gsl, rdim, rstd_bc, rstd_tm):
        cfg, k, P = self.cfg, self.k, self.P
        sqs = [k.sb(f"rsq{i}", [128, KCn, 512], BF16) for i in range(2)]
        for ci, (c0, n) in enumerate(cfg.chunks):
            sq = sqs[ci % 2]
            self.act(sq, sq.h[:, :, 0:n], X, X.h[:, :, c0:c0 + n], AF.Square)
            ps = self.ps[4 + ci % 2]
            for kc in range(KCn):
                self.mm(ps, ps.h[:, 0:n], P["onesb"].h[:, :], sq.h[:, kc, 0:n], [P["onesb"], sq], kc == 0, kc == KCn - 1)
            self.ts(rstd_bc, rstd_bc.h[:, c0:c0 + n], ps, ps.h[:, 0:n], 1.0 / rdim, cfg.RMS_EPS, ALU.mult, ALU.add)
            self.act(rstd_bc, rstd_bc.h[:, c0:c0 + n], rstd_bc, rstd_bc.h[:, c0:c0 + n], AF.Sqrt)
            k.op('dve', lambda e, c0=c0, n=n: e.reciprocal(rstd_bc.h[:, c0:c0 + n], rstd_bc.h[:, c0:c0 + n]), reads=[rstd_bc], writes=[rstd_bc])
            if rstd_tm is not None:
                for a in range((n + 127) // 128):
                    rows = min(128, n - a * 128)
                    ti = (c0 + a * 128) // 128
                    pt = self.ps[6 + a % 2]
                    for kc in range(KCn):
                        self.mm(pt, pt.h[0:rows, 0:1], sq.h[:, kc, a * 128:a * 128 + rows], P["onesb"].h[:, 0:1],
                                [P["onesb"], sq], kc == 0, kc == KCn - 1)
                    self.ts(rstd_tm, rstd_tm.h[0:rows, ti:ti + 1], pt, pt.h[0:rows, 0:1], 1.0 / rdim, cfg.RMS_EPS,
                            ALU.mult, ALU.add)
                    self.act(rstd_tm, rstd_tm.h[0:rows, ti:ti + 1], rstd_tm, rstd_tm.h[0:rows, ti:ti + 1], AF.Sqrt)
                    k.op('dve', lambda e, rows=rows, ti=ti: e.reciprocal(rstd_tm.h[0:rows, ti:ti + 1], rstd_tm.h[0:rows, ti:ti + 1]), reads=[rstd_tm], writes=[rstd_tm])
        pvh = P["pvec"]
        for kc in range(KCn):
            self.ts(X, X.h[:, kc, :], X, X.h[:, kc, :], pvh.h[:, gsl[0] + kc:gsl[0] + kc + 1], None, ALU.mult,
                    extra_reads=[pvh])

    def phase_mla_up(self, l, Wq, Wkv, qg, kvg):
        cfg, k, P, s = self.cfg, self.k, self.P, self.s
        T = cfg.T
        MH = cfg.MH
        ntile = (T + 127) // 128
        KQ = cfg.QR // 128
        X = self.load_x("Xq", s["cqT"], cfg.QR)
        rq = k.sb("rq", [128, T], F32)
        self.load_rope()
        self.rms_stats(X, KQ, qg, cfg.QR, rq, None)
        wb = [k.sb(f"w{i}", [128, max(KQ, cfg.KVR // 128), 512], BF16) for i in range(2)]
        st = [k.sb(f"st{i}", [128, 512], BF16) for i in range(4)]
        t1 = k.sb("t1", [64, 512], F32)
        t2 = k.sb("t2", [64, 512], F32)
        sti = [0]

        def stage():
            t = st[sti[0] % 4]
            sti[0] += 1
            return t

        def epi_qn(fi, ci, c0, n, ps):
            t = stage()
            self.tt(t, t.h[:, 0:n], ps, ps.h[:, 0:n], rq, rq.h[:, c0:c0 + n], ALU.mult)
            self.store(s["qnT"][fi * 128:(fi + 1) * 128, c0:c0 + n], t, t.h[:, 0:n])

        self.linear(X, KQ, Wq, [(h * 128, 128) for h in range(MH)], epi_qn, wb)
        ro = MH * cfg.NOPE
        t1all = k.sb("t1all", [64, T], F32)

        def epi_qr(fi, ci, c0, n, ps):
            h_ = fi // 2
            if fi % 2 == 0:
                self.tt(t1all, t1all.h[:, c0:c0 + n], ps, ps.h[0:64, 0:n], P["cos"], P["cos"].h[:, c0:c0 + n],
                        ALU.mult)
            else:
                self.tt(t2, t2.h[:, 0:n], ps, ps.h[0:64, 0:n], P["sin"], P["sin"].h[:, c0:c0 + n], ALU.mult)
                self.tt(t2, t2.h[:, 0:n], t2, t2.h[:, 0:n], t1all, t1all.h[:, c0:c0 + n], ALU.add)
                t = stage()
                self.tt(t, t.h[0:64, 0:n], t2, t2.h[:, 0:n], rq, rq.h[0:64, c0:c0 + n], ALU.mult)
                self.store(s["qrT"][h_ * 64:(h_ + 1) * 64, c0:c0 + n], t, t.h[0:64, 0:n])

        fts = []
        for h in range(MH):
            fts += [(ro + h * 128, 64), (ro + h * 128 + 64, 64)]
        self.linear(X, KQ, Wq, fts, epi_qr, wb)
        self.k.barrier()
        KV = cfg.KVR // 128
        X = self.load_x("Xkv", s["ckvT"], cfg.KVR)
        rk = k.sb("rk", [128, T], F32)
        rkt = k.sb("rkt", [128, ntile], F32)
        self.rms_stats(X, KV, kvg, cfg.KVR, rk, rkt)
        wb = [k.sb(f"w{i}", [128, KV, 512], BF16) for i in range(2)]
        st = [k.sb(f"st{i}", [128, 512], BF16) for i in range(4)]

        def epi_kn(fi, ci, c0, n, ps):
            t = stage2()
            self.tt(t, t.h[:, 0:n], ps, ps.h[:, 0:n], rk, rk.h[:, c0:c0 + n], ALU.mult)
            self.store(s["knT"][fi * 128:(fi + 1) * 128, c0:c0 + n], t, t.h[:, 0:n])

        sti2 = [0]

        def stage2():
            t = st[sti2[0] % 4]
            sti2[0] += 1
            return t

        self.linear(X, KV, Wkv, [(h * 128, 128) for h in range(MH)], epi_kn, wb)
        vo = MH * cfg.NOPE
        for g0 in range(0, MH * cfg.VD, 512):
            ncv = min(512, MH * cfg.VD - g0)

            def epi_v(ti, t0, rows, ps, g0=g0, ncv=ncv):
                t = stage2()
                self.ts(t, t.h[0:rows, 0:ncv], ps, ps.h[0:rows, 0:ncv], rkt.h[0:rows, ti:ti + 1], None, ALU.mult,
                        extra_reads=[rkt])
                self.store(s["vm"][t0:t0 + rows, g0:g0 + ncv], t, t.h[0:rows, 0:ncv])

            self.linear_tm(X, KV, Wkv, vo + g0, ncv, epi_v, wb[(g0 // 512) % 2])

    def phase_mla_attn(self, l):
        cfg, k, P, s = self.cfg, self.k, self.P, self.s
        T = cfg.T
        ntile = (T + 127) // 128
        scale = (cfg.NOPE + cfg.ROPE) ** -0.5
        kr = k.sb("kr", [64, T], BF16)
        self.load(kr, kr.h[:, :], s["krT"][:, :])
        kns = [k.sb(f"kn{i}", [128, T], BF16) for i in range(2)]
        vs_ = [k.sb(f"v{i}", [128, ntile, cfg.VD], BF16) for i in range(2)]
        qns = [k.sb(f"qn{i}", [128, 512], BF16) for i in range(2)]
        qrs = [k.sb(f"qr{i}", [64, 512], BF16) for i in range(2)]
        pts = [k.sb(f"pt{i}", [128, 512], BF16) for i in range(3)]
        rcs = [k.sb(f"rc{i}", [128, 512], F32) for i in range(2)]
        obs = [k.sb(f"ob{i}", [128, 512], BF16) for i in range(2)]
        nfull = T // 128
        rem = T - nfull * 128
        qi = 0
        for h in range(cfg.MH):
            kn = kns[h % 2]
            v = vs_[h % 2]
            self.load(kn, kn.h[:, :], s["knT"][h * 128:(h + 1) * 128, :])
            k.dma("sp", v.h[:, 0:nfull, :],
                  s["vm"][0:nfull * 128, h * cfg.VD:(h + 1) * cfg.VD].rearrange("(a p) d -> p a d", p=128),
                  owner=v, writes=[v])
            if rem:
                k.dma("sp", v.h[0:rem, nfull, :], s["vm"][nfull * 128:T, h * cfg.VD:(h + 1) * cfg.VD], owner=v,
                      writes=[])
                v.b.w = {v.b.sem: k.dtotal[v.b.sem]}
            for ci, (c0, n) in enumerate(cfg.chunks):
                qn = qns[qi % 2]
                qr = qrs[qi % 2]
                rc = rcs[qi % 2]
                ob = obs[qi % 2]
                qi += 1
                self.load(qn, qn.h[:, 0:n], s["qnT"][h * 128:(h + 1) * 128, c0:c0 + n])
                self.load(qr, qr.h[:, 0:n], s["qrT"][h * 64:(h + 1) * 64, c0:c0 + n])
                po, pd = self.ps[3], self.ps[4]

                def smm(kt):
                    kk = 128 if kt < nfull else rem
                    S = self.ps[kt % 3]
                    self.mm(S, S.h[0:kk, 0:n], kn.h[:, kt * 128:kt * 128 + kk], qn.h[:, 0:n], [kn, qn], True, False)
                    self.mm(S, S.h[0:kk, 0:n], kr.h[:, kt * 128:kt * 128 + kk], qr.h[:, 0:n], [kr, qr], False, True)

                smm(0)
                for kt in range(ntile):
                    kk = 128 if kt < nfull else rem
                    if kt + 1 < ntile:
                        smm(kt + 1)
                    S = self.ps[kt % 3]
                    pt = pts[kt % 3]
                    self.act(pt, pt.h[0:kk, 0:n], S, S.h[0:kk, 0:n], AF.Exp, scale=scale)
                    self.mm(po, po.h[:, 0:n], v.h[0:kk, kt, :], pt.h[0:kk, 0:n], [v, pt], kt == 0, kt == ntile - 1)
                    self.mm(pd, pd.h[:, 0:n], P["onesb"].h[0:kk, :], pt.h[0:kk, 0:n], [P["onesb"], pt], kt == 0,
                            kt == ntile - 1)
                k.op("dve", lambda e, rc=rc, pd=pd, n=n: e.reciprocal(rc.h[:, 0:n], pd.h[:, 0:n]), reads=[pd],
                     writes=[rc])
                self.tt(ob, ob.h[:, 0:n], po, po.h[:, 0:n], rc, rc.h[:, 0:n], ALU.mult)
                self.store(s["oaT"][h * 128:(h + 1) * 128, c0:c0 + n], ob, ob.h[:, 0:n])

    def phase_swa(self, l, biasd, biasmk, biasqm, sinkb):
        cfg, k, P, s = self.cfg, self.k, self.P, self.s
        T, NB, G = cfg.T, cfg.NB, cfg.GRP
        ntile = (T + 127) // 128
        scale = cfg.HD ** -0.5
        inv = 1.0 / scale
        GW = G * 128
        sk = k.sb("sk", [128, cfg.SH], F32)
        self.load(sk, sk.h[:, :], sinkb[:, :])
        self.act(sk, sk.h[:, :], sk, sk.h[:, :], AF.Exp)
        ks_ = [k.sb(f"ks{i}", [128, T], BF16) for i in range(2)]
        vs_ = [k.sb(f"v{i}", [128, ntile, cfg.HD], BF16) for i in range(2)]
        qs_ = [k.sb(f"q{i}", [128, G, T], BF16) for i in range(2)]
        bd = [k.sb(f"bd{i}", [128, 3, GW], BF16) for i in range(2)]
        bmk = [k.sb(f"bmk{i}", [16, 2, GW], BF16) for i in range(2)]
        bqm = [k.sb(f"bqm{i}", [128, 2, G * 16], BF16) for i in range(2)]
        ske = [k.sb(f"ske{i}", [128, G, 128], F32) for i in range(2)]
        pts = [k.sb(f"pt{i}", [128, GW], BF16) for i in range(3)]
        rcs = [k.sb(f"rc{i}", [128, GW], F32) for i in range(2)]
        obs = [k.sb(f"ob{i}", [128, G, 128], BF16) for i in range(2)]
        si = 0
        bi = 0
        for hk in range(cfg.SKV):
            ks, v, q = ks_[hk % 2], vs_[hk % 2], qs_[hk % 2]
            b_d, b_mk, b_qm, sk_e = bd[hk % 2], bmk[hk % 2], bqm[hk % 2], ske[hk % 2]
            self.load(ks, ks.h[:, :], s["ksT"][hk * 128:(hk + 1) * 128, :])
            k.dma("sp", v.h[:, 0:NB, :],
                  s["vs"][0:NB * 128, hk * cfg.HD:(hk + 1) * cfg.HD].rearrange("(a p) d -> p a d", p=128),
                  owner=v, writes=[v])
            k.dma("sp", v.h[0:16, NB, :], s["vs"][NB * 128:T, hk * cfg.HD:(hk + 1) * cfg.HD], owner=v, writes=[])
            v.b.w = {v.b.sem: k.dtotal[v.b.sem]}
            self.load(q, q.h[:, :, :],
                      s["qsT"][hk * G * 128:(hk + 1) * G * 128, :].rearrange("(g p) t -> p g t", p=128))
            self.load(b_d, b_d.h[:, :, :], biasd[hk].rearrange("d p w -> p d w"), eng="pool")
            self.load(b_mk, b_mk.h[:, :, :], biasmk[hk].rearrange("d p w -> p d w"), eng="pool")
            k.dma("pool", b_qm.h[0:16, 0, :], biasqm[hk, 0:16, :], owner=b_qm, writes=[b_qm])
            k.dma("pool", b_qm.h[:, 1, :], biasqm[hk, 16:144, :], owner=b_qm, writes=[])
            b_qm.b.w = {b_qm.b.sem: k.dtotal[b_qm.b.sem]}
            self.ts(b_d, b_d.h[:, :, :], b_d, b_d.h[:, :, :], inv, None, ALU.mult)
            self.ts(b_mk, b_mk.h[:, :, :], b_mk, b_mk.h[:, :, :], inv, None, ALU.mult)
            self.ts(b_qm, b_qm.h[0:16, 0, :], b_qm, b_qm.h[0:16, 0, :], inv, None, ALU.mult)
            self.ts(b_qm, b_qm.h[:, 1, :], b_qm, b_qm.h[:, 1, :], inv, None, ALU.mult)
            for g in range(G):
                hh = hk * G + g
                self.cp(sk_e, sk_e.h[:, g, :], sk, sk.h[:, hh:hh + 1].to_broadcast([128, 128]))
            for i in range(NB + 1):
                if i < NB:
                    nq = 128
                    W_ = GW
                    qap = q.h[:, :, i * 128:(i + 1) * 128]
                    keys = [(NB, 16, b_mk.h[0:16, 0 if i == 0 else 1, :])]
                    for dlt in (-1, 0, 1):
                        j = i + dlt
                        if 0 <= j < NB:
                            keys.append((j, 128, b_d.h[:, dlt + 1, :]))
                else:
                    nq = 16
                    W_ = G * 16
                    qap = q.h[:, :, NB * 128:NB * 128 + 16]
                    keys = [(NB, 16, b_qm.h[0:16, 0, :]), (0, 128, b_qm.h[:, 1, :])]
                po, pd = self.ps[3], self.ps[4]
                rc, ob = rcs[bi % 2], obs[bi % 2]
                bi += 1
                nk = len(keys)
                for ki, (j, kk, bap) in enumerate(keys):
                    S = self.ps[si % 3]
                    pt = pts[si % 3]
                    si += 1
                    braw = b_mk if (i < NB and ki == 0) else (b_d if i < NB else b_qm)
                    self.mm(S, S.h[0:kk, 0:W_], ks.h[:, j * 128:j * 128 + kk], qap, [ks, q], True, False)
                    self.mm(S, S.h[0:kk, 0:W_], P["identb"].h[0:kk, 0:kk], bap, [P["identb"], braw], False, True)
                    self.act(pt, pt.h[0:kk, 0:W_], S, S.h[0:kk, 0:W_], AF.Exp, scale=scale)
                    self.mm(po, po.h[:, 0:W_], v.h[0:kk, j, :], pt.h[0:kk, 0:W_], [v, pt], ki == 0, ki == nk - 1)
                    self.mm(pd, pd.h[:, 0:W_], P["onesb"].h[0:kk, :], pt.h[0:kk, 0:W_], [P["onesb"], pt], ki == 0,
                            ki == nk - 1)
                pdv = pd.h[:, 0:W_].rearrange("p (g q) -> p g q", g=G)
                rcv = rc.h[:, 0:W_].rearrange("p (g q) -> p g q", g=G)
                pov = po.h[:, 0:W_].rearrange("p (g q) -> p g q", g=G)
                self.tt(rc, rcv, pd, pdv, sk_e, sk_e.h[:, :, 0:nq], ALU.add)
                k.op("dve", lambda e, rcv=rcv: e.reciprocal(rcv, rcv), reads=[rc], writes=[rc])
                self.tt(ob, ob.h[:, :, 0:nq], po, pov, rc, rcv, ALU.mult)
                t0 = i * 128
                dst = s["obT"][hk * G * 128:(hk + 1) * G * 128, t0:t0 + nq].rearrange("(g p) t -> p g t", p=128)
                self.store(dst, ob, ob.h[:, :, 0:nq])

    def phase_proj(self, l, Wa, Wb, Wo):
        cfg, k, P, s = self.cfg, self.k, self.P, self.s
        KC = cfg.KC

        def run(Xd, W, epi, nm):
            Kx = Xd.shape[0]
            X = self.load_x(nm, Xd, Kx)
            wb = [k.sb(f"w{i}", [128, Kx // 128, 512], BF16) for i in range(2)]
            self.linear(X, Kx // 128, W, [(i * 128, 128) for i in range(KC)], epi, wb)

        gts = None

        def alloc_common():
            return ([k.sb(f"g{i}", [128, 512], BF16) for i in range(3)],
                    [k.sb(f"a{i}", [128, 512], F32) for i in range(3)],
                    [k.sb(f"o{i}", [128, 512], F32) for i in range(3)],
                    [k.sb(f"ob{i}", [128, 512], BF16) for i in range(3)])

        ctr = [0]
        gt, at, ot, obt = alloc_common()

        def epiA(fi, ci, c0, n, ps):
            i = ctr[0] % 3
            ctr[0] += 1
            self.load(gt[i], gt[i].h[:, 0:n], s["gaT"][fi * 128:(fi + 1) * 128, c0:c0 + n])
            self.tt(ot[i], ot[i].h[:, 0:n], ps, ps.h[:, 0:n], gt[i], gt[i].h[:, 0:n], ALU.mult)
            self.store(s["tAT"][fi * 128:(fi + 1) * 128, c0:c0 + n], ot[i], ot[i].h[:, 0:n])

        run(s["oaT"], Wa, epiA, "Xa")
        k.barrier()
        gt, at, ot, obt = alloc_common()

        def epiB(fi, ci, c0, n, ps):
            i = ctr[0] % 3
            ctr[0] += 1
            self.load(gt[i], gt[i].h[:, 0:n], s["gbT"][fi * 128:(fi + 1) * 128, c0:c0 + n])
            self.load(at[i], at[i].h[:, 0:n], s["tAT"][fi * 128:(fi + 1) * 128, c0:c0 + n])
            self.tt(ot[i], ot[i].h[:, 0:n], ps, ps.h[:, 0:n], gt[i], gt[i].h[:, 0:n], ALU.mult)
            self.tt(obt[i], obt[i].h[:, 0:n], ot[i], ot[i].h[:, 0:n], at[i], at[i].h[:, 0:n], ALU.add)
            self.store(s["mgT"][fi * 128:(fi + 1) * 128, c0:c0 + n], obt[i], obt[i].h[:, 0:n])

        run(s["obT"], Wb, epiB, "Xb")
        k.barrier()
        gt, at, ot, obt = alloc_common()

        def epiO(fi, ci, c0, n, ps):
            i = ctr[0] % 3
            ctr[0] += 1
            self.load(at[i], at[i].h[:, 0:n], s["hres"][fi * 128:(fi + 1) * 128, c0:c0 + n])
            k.op("dve", lambda e, i=i, n=n, ps=ps: e.scalar_tensor_tensor(ot[i].h[:, 0:n], at[i].h[:, 0:n], cfg.ALPHA,
                                                                          ps.h[:, 0:n], ALU.mult, ALU.add),
                 reads=[at[i], ps], writes=[ot[i]])
            self.store(s["yT"][fi * 128:(fi + 1) * 128, c0:c0 + n], ot[i], ot[i].h[:, 0:n])

        run(s["mgT"], Wo, epiO, "Xm")

    def phase_router(self, Wr, rbb):
        cfg, k, P, s = self.cfg, self.k, self.P, self.s
        KC, NE, T = cfg.KC, cfg.NE, cfg.T
        wr = k.sb("wr", [128, KC, NE], F32)
        self.load(wr, wr.h[:, :, :], Wr.rearrange("(kc p) e -> p kc e", p=128))
        rb = k.sb("rb", [128, NE], F32)
        self.load(rb, rb.h[:, :], rbb[:, :])
        hs = [k.sb(f"h{i}", [128, KC, 512], F32) for i in range(2)]
        lg = [k.sb(f"lg{i}", [128, NE], F32) for i in range(2)]
        mx = [k.sb(f"mx{i}", [128, 8], F32) for i in range(2)]
        ex = [k.sb(f"ex{i}", [128, NE], F32) for i in range(2)]
        mk = [k.sb(f"mk{i}", [128, NE], F32) for i in range(2)]
        dn = [k.sb(f"dn{i}", [128, 2], F32) for i in range(2)]
        gtt = [k.sb(f"gtt{i}", [NE, 128], F32) for i in range(2)]
        pmt = [k.sb(f"pmt{i}", [NE, 128], F32) for i in range(2)]
        pm = [k.sb(f"pm{i}", [128, NE], F32) for i in range(2)]
        R = k.sb("R", [128, NE], F32)
        k.op("dve", lambda e: e.memset(R.h[:, :], 0.0), writes=[R])
        obf = [k.sb(f"obf{i}", [128, cfg.D], BF16) for i in range(2)]
        ti = 0
        for ci, (c0, n) in enumerate(cfg.chunks):
            h = hs[ci % 2]
            self.load(h, h.h[:, :, 0:n], self.fm(s["hres"])[:, :, c0:c0 + n])
            for a in range((n + 127) // 128):
                rows = min(128, n - a * 128)
                t0 = c0 + a * 128
                i = ti % 2
                ti += 1
                ps = self.ps[i]
                for kc in range(KC):
                    self.mm(ps, ps.h[0:rows, 0:NE], h.h[:, kc, a * 128:a * 128 + rows], wr.h[:, kc, :], [h, wr],
                            kc == 0, kc == KC - 1)
                L_, M_, E_, K_, D_ = lg[i], mx[i], ex[i], mk[i], dn[i]
                self.tt(L_, L_.h[0:rows, :], ps, ps.h[0:rows, 0:NE], rb, rb.h[0:rows, :], ALU.add)
                k.op("dve", lambda e, M_=M_, L_=L_, rows=rows: e.max(M_.h[0:rows, :], L_.h[0:rows, :]), reads=[L_],
                     writes=[M_])
                self.ts(K_, K_.h[0:rows, :], L_, L_.h[0:rows, :], M_.h[0:rows, 1:2], None, ALU.is_ge, extra_reads=[M_])
                self.ts(D_, D_.h[0:rows, 0:1], M_, M_.h[0:rows, 0:1], -1.0, None, ALU.mult)
                self.act(E_, E_.h[0:rows, :], L_, L_.h[0:rows, :], AF.Exp, extra_reads=[D_], bias=D_.h[0:rows, 0:1])
                self.tt(E_, E_.h[0:rows, :], E_, E_.h[0:rows, :], K_, K_.h[0:rows, :], ALU.mult)
                k.op("dve", lambda e, D_=D_, E_=E_, rows=rows: e.tensor_reduce(D_.h[0:rows, 1:2], E_.h[0:rows, :],
                                                                              AX.X, ALU.add),
                     reads=[E_], writes=[D_])
                k.op("dve", lambda e, D_=D_, rows=rows: e.reciprocal(D_.h[0:rows, 1:2], D_.h[0:rows, 1:2]), reads=[D_],
                     writes=[D_])
                self.ts(E_, E_.h[0:rows, :], E_, E_.h[0:rows, :], D_.h[0:rows, 1:2], None, ALU.mult, extra_reads=[D_])
                pt = self.ps[2 + i]
                self.mm(pt, pt.h[0:NE, 0:rows], E_.h[0:rows, :], P["ident"].h[0:rows, 0:rows], [E_, P["ident"]], True,
                        True)
                G_ = gtt[i]
                self.cp(G_, G_.h[:, 0:rows], pt, pt.h[0:NE, 0:rows], eng="act")
                self.store(s["gtT"][:, t0:t0 + rows], G_, G_.h[:, 0:rows])
                pp = self.ps[4 + i]
                self.mm(pp, pp.h[0:rows, 0:NE], P["upper"].h[0:rows, 0:rows], K_.h[0:rows, :], [P["upper"], K_], True,
                        ti == 1)
                if ti > 1:
                    self.mm(pp, pp.h[0:rows, 0:NE], P["ones"].h[:, 0:rows], R.h[:, :], [P["ones"], R], False, True)
                PM = pm[i]
                k.op("dve", lambda e, PM=PM, pp=pp, K_=K_, rows=rows: e.scalar_tensor_tensor(
                    PM.h[0:rows, :], pp.h[0:rows, 0:NE], 1.0, K_.h[0:rows, :], ALU.add, ALU.mult),
                    reads=[pp, K_], writes=[PM])
                self.ts(PM, PM.h[0:rows, :], PM, PM.h[0:rows, :], -1.0, None, ALU.add)
                self.tt(R, R.h[0:rows, :], R, R.h[0:rows, :], K_, K_.h[0:rows, :], ALU.add)
                self.store(s["pmtm"][t0:t0 + rows, :], PM, PM.h[0:rows, :])
                pt2 = self.ps[6 + i]
                self.mm(pt2, pt2.h[0:NE, 0:rows], PM.h[0:rows, :], P["ident"].h[0:rows, 0:rows], [PM, P["ident"]],
                        True, True)
                PT = pmt[i]
                self.cp(PT, PT.h[:, 0:rows], pt2, pt2.h[0:NE, 0:rows], eng="act")
                self.store(s["pmT"][:, t0:t0 + rows], PT, PT.h[:, 0:rows])
                ob = obf[i]
                for g in range(0, KC, 4):
                    ph = self.ps[2 + (g // 4) % 2]
                    gn = min(4, KC - g)
                    for j in range(gn):
                        self.mm(ph, ph.h[0:rows, j * 128:(j + 1) * 128], h.h[:, g + j, a * 128:a * 128 + rows],
                                P["ident"].h[:, :], [h, P["ident"]], True, True)
                    self.cp(ob, ob.h[0:rows, g * 128:(g + gn) * 128], ph, ph.h[0:rows, 0:gn * 128],
                            eng=("act" if (g // 4) % 2 else "dve"))
                self.store(s["htm"][t0:t0 + rows, :], ob, ob.h[0:rows, :])

    def slot_chunks(self, s0, s1):
        out = []
        c = s0
        while c < s1:
            n = min(512, s1 - c)
            out.append((c, n))
            c += n
        return out

    def phase_gather(self):
        cfg, k, P, s = self.cfg, self.k, self.P, self.s
        KC, NE, T, CAP = cfg.KC, cfg.NE, cfg.T, cfg.CAP
        nfull = T // 128
        rem = T - nfull * 128
        ntile = nfull + (1 if rem else 0)
        iota = k.sb("iota", [128, CAP], F32)
        self.load(iota, iota.h[:, :], self.iotac[:, :])
        pma = k.sb("pma", [128, ntile, NE], F32)
        k.dma("sp", pma.h[:, 0:nfull, :], s["pmtm"][0:nfull * 128, :].rearrange("(a p) e -> p a e", p=128), owner=pma,
              writes=[pma])
        if rem:
            k.dma("sp", pma.h[0:rem, nfull, :], s["pmtm"][nfull * 128:T, :], owner=pma, writes=[])
            pma.b.w = {pma.b.sem: k.dtotal[pma.b.sem]}
        sel = k.sb("sel", [128, ntile, CAP], BF16)
        hts = [k.sb(f"htk{i}", [128, ntile, 128], BF16) for i in range(2)]
        st = [k.sb(f"st{i}", [128, 512], BF16) for i in range(4)]
        sti = 0
        hi = 0
        for e in range(NE):
            for i in range(ntile):
                rows = 128 if i < nfull else rem
                self.ts(sel, sel.h[0:rows, i, :], iota, iota.h[0:rows, :], pma.h[0:rows, i, e:e + 1], None, ALU.is_equal,
                        extra_reads=[pma])
            for kc in range(KC):
                ht = hts[hi % 2]
                hi += 1
                k.dma("sp", ht.h[:, 0:nfull, :],
                      s["htm"][0:nfull * 128, kc * 128:(kc + 1) * 128].rearrange("(a p) d -> p a d", p=128), owner=ht,
                      writes=[ht])
                if rem:
                    k.dma("sp", ht.h[0:rem, nfull, :], s["htm"][nfull * 128:T, kc * 128:(kc + 1) * 128], owner=ht,
                          writes=[])
                    ht.b.w = {ht.b.sem: k.dtotal[ht.b.sem]}
                for (c0, n) in self.slot_chunks(0, CAP):
                    ps = self.ps[sti % 4]
                    for i in range(ntile):
                        rows = 128 if i < nfull else rem
                        self.mm(ps, ps.h[:, 0:n], ht.h[0:rows, i, :], sel.h[0:rows, i, c0:c0 + n], [ht, sel], i == 0,
                                i == ntile - 1)
                    t = st[sti % 4]
                    self.cp(t, t.h[:, 0:n], ps, ps.h[:, 0:n], eng=("act" if sti % 2 else "dve"))
                    sti += 1
                    self.store(s["XeT"][e * cfg.D + kc * 128:e * cfg.D + (kc + 1) * 128, c0:c0 + n], t, t.h[:, 0:n])

    def phase_moe_ffn(self, experts):
        cfg, k, P, s = self.cfg, self.k, self.P, self.s
        KC, DFF, CAP, D = cfg.KC, cfg.DFF, cfg.CAP, cfg.D
        FK = DFF // 128
        HALF = CAP // 2
        nst = HALF // 128
        X = k.sb("Xe", [128, KC, HALF], BF16)
        G_ = k.sb("G", [128, FK, HALF], BF16)
        w1b = [k.sb(f"w1_{i}", [128, KC, 256], BF16) for i in range(2)]
        w3b = [k.sb(f"w3_{i}", [128, KC, 256], BF16) for i in range(2)]
        w2b = [k.sb(f"w2_{i}", [128, FK, 256], BF16) for i in range(2)]
        Yb = k.sb("Yb", [128, nst, D], BF16)
        sa = [k.sb(f"sa{i}", [128, 512], F32) for i in range(2)]
        ctr = 0
        wctr = 0
        for ei, (W1, W3, W2) in enumerate(experts):
            W1v = W1.rearrange("(kc p) n -> p kc n", p=128)
            W3v = W3.rearrange("(kc p) n -> p kc n", p=128)
            W2v = W2.rearrange("(kc p) n -> p kc n", p=128)
            for s0 in range(0, CAP, HALF):
                chs = self.slot_chunks(0, HALF)
                self.load(X, X.h[:, :, :], self.fm(s["XeT"][ei * D:(ei + 1) * D, :])[:, :, s0:s0 + HALF])
                for j0 in range(0, FK, 2):
                    jn = min(2, FK - j0)
                    w1, w3 = w1b[wctr % 2], w3b[wctr % 2]
                    wctr += 1
                    self.load(w1, w1.h[:, :, 0:jn * 128], W1v[:, :, j0 * 128:(j0 + jn) * 128], eng="pool")
                    self.load(w3, w3.h[:, :, 0:jn * 128], W3v[:, :, j0 * 128:(j0 + jn) * 128], eng="pool")
                    for jj in range(jn):
                        j = j0 + jj
                        for (c0, n) in chs:
                            i = ctr % 2
                            ctr += 1
                            pa, pb = self.ps[2 * i], self.ps[2 * i + 1]
                            for kc in range(KC):
                                self.mm(pa, pa.h[:, 0:n], w1.h[:, kc, jj * 128:(jj + 1) * 128], X.h[:, kc, c0:c0 + n],
                                        [w1, X], kc == 0, kc == KC - 1)
                            for kc in range(KC):
                                self.mm(pb, pb.h[:, 0:n], w3.h[:, kc, jj * 128:(jj + 1) * 128], X.h[:, kc, c0:c0 + n],
                                        [w3, X], kc == 0, kc == KC - 1)
                            self.act(sa[i], sa[i].h[:, 0:n], pa, pa.h[:, 0:n], AF.Silu)
                            self.tt(G_, G_.h[:, j, c0:c0 + n], sa[i], sa[i].h[:, 0:n], pb, pb.h[:, 0:n], ALU.mult)
                for cg in range(0, D, 256):
                    cn = min(256, D - cg)
                    w2 = w2b[wctr % 2]
                    wctr += 1
                    self.load(w2, w2.h[:, :, 0:cn], W2v[:, :, cg:cg + cn], eng="pool")
                    for st_ in range(nst):
                        ps = self.ps[4 + ctr % 4]
                        ctr += 1
                        for j in range(FK):
                            self.mm(ps, ps.h[:, 0:cn], G_.h[:, j, st_ * 128:(st_ + 1) * 128], w2.h[:, j, 0:cn], [G_, w2],
                                    j == 0, j == FK - 1)
                        self.cp(Yb, Yb.h[:, st_, cg:cg + cn], ps, ps.h[:, 0:cn], eng=("act" if ctr % 2 else "dve"))
                r0 = ei * CAP + s0
                self.store(s["Yd"][r0:r0 + HALF, :].rearrange("(a p) d -> p a d", p=128), Yb, Yb.h[:, :, :])

    def phase_combine(self):
        cfg, k, P, s = self.cfg, self.k, self.P, self.s
        KC, NE, CAP, D = cfg.KC, cfg.NE, cfg.CAP, cfg.D
        NST = CAP // 128
        sid = k.sb("sid", [128, NST], F32)
        self.load(sid, sid.h[:, :], self.slotid[:, :])
        selT = k.sb("selT", [128, NE * NST, 512], BF16)
        pmb = [k.sb(f"pmb{i}", [128, 512], F32) for i in range(2)]
        gtb = [k.sb(f"gtb{i}", [128, 512], F32) for i in range(2)]
        tmpq = [k.sb(f"tmpq{i}", [128, 512], F32) for i in range(2)]
        FH = min(8, KC)
        yts = [k.sb(f"yt{i}", [128, FH * 128], BF16) for i in range(4)]
        hr = [k.sb(f"hr{i}", [128, 512], F32) for i in range(2)]
        ot = [k.sb(f"ot{i}", [128, 512], F32) for i in range(2)]
        yi = 0
        oi = 0
        for (c0, n) in cfg.chunks:
            for e in range(NE):
                pb_, gb_ = pmb[e % 2], gtb[e % 2]
                self.load(pb_, pb_.h[:, 0:n], s["pmT"][e, c0:c0 + n].partition_broadcast(128))
                self.load(gb_, gb_.h[:, 0:n], s["gtT"][e, c0:c0 + n].partition_broadcast(128))
                for st_ in range(NST):
                    tq = tmpq[st_ % 2]
                    self.ts(tq, tq.h[:, 0:n], pb_, pb_.h[:, 0:n], sid.h[:, st_:st_ + 1], None, ALU.is_equal,
                            extra_reads=[sid])
                    self.tt(selT, selT.h[:, e * NST + st_, 0:n], tq, tq.h[:, 0:n], gb_, gb_.h[:, 0:n], ALU.mult)
            for f0 in range(0, KC, FH):
                fn_ = min(FH, KC - f0)
                tot = NE * NST
                for q in range(tot):
                    e, st_ = divmod(q, NST)
                    yt = yts[yi % 4]
                    yi += 1
                    r0 = e * CAP + st_ * 128
                    self.load(yt, yt.h[:, 0:fn_ * 128], s["Yd"][r0:r0 + 128, f0 * 128:(f0 + fn_) * 128])
                    for f in range(fn_):
                        ps = self.ps[f]
                        self.mm(ps, ps.h[:, 0:n], yt.h[:, f * 128:(f + 1) * 128], selT.h[:, q, 0:n], [yt, selT], q == 0,
                                q == tot - 1, inc=(q == tot - 1 or f == fn_ - 1))
                for f in range(fn_):
                    ps = self.ps[f]
                    hrt, o_ = hr[oi % 2], ot[oi % 2]
                    oi += 1
                    fr = (f0 + f) * 128
                    self.load(hrt, hrt.h[:, 0:n], s["hres"][fr:fr + 128, c0:c0 + n])
                    k.op("dve", lambda en, o_=o_, hrt=hrt, ps=ps, n=n: en.scalar_tensor_tensor(
                        o_.h[:, 0:n], hrt.h[:, 0:n], cfg.ALPHA, ps.h[:, 0:n], ALU.mult, ALU.add), reads=[hrt, ps],
                        writes=[o_])
                    self.store(s["yT"][fr:fr + 128, c0:c0 + n], o_, o_.h[:, 0:n])

    def phase_ffn(self, experts, gated):
        cfg, k, P, s = self.cfg, self.k, self.P, self.s
        KC, DFF, T = cfg.KC, cfg.DFF, cfg.T
        FK = DFF // 128
        PART = cfg.FFPART
        supers = []
        cur = []
        for (c0, n) in cfg.chunks:
            if cur and sum(x[1] for x in cur) + n > cfg.SUPER + 16:
                supers.append(cur)
                cur = []
            cur.append((c0, n))
        if cur:
            supers.append(cur)
        SW = max(sum(x[1] for x in sc) for sc in supers)
        hb = k.sb("hbS", [128, KC, SW], BF16)
        G_ = k.sb("G", [128, PART, SW], BF16)
        acc = k.sb("acc", [128, KC, SW], F32)
        w1b = [k.sb(f"w1_{i}", [128, KC, 256], BF16) for i in range(2)]
        w3b = [k.sb(f"w3_{i}", [128, KC, 256], BF16) for i in range(2)]
        w2b = [k.sb(f"w2_{i}", [128, PART, 256], BF16) for i in range(2)]
        sa = [k.sb(f"sa{i}", [128, 512], F32) for i in range(2)]
        gbs = [k.sb(f"gb{i}", [128, 512], F32) for i in range(2)]
        hr = [k.sb(f"hr{i}", [128, 512], F32) for i in range(2)]
        ctr = [0]
        wctr = [0]
        for sc in supers:
            s0 = sc[0][0]
            sw = sum(x[1] for x in sc)
            self.load(hb, hb.h[:, :, 0:sw], self.fm(s["hb"])[:, :, s0:s0 + sw])
            first_acc = True
            for ei, (W1, W3, W2) in enumerate(experts):
                W1v = W1.rearrange("(kc p) n -> p kc n", p=128)
                W3v = W3.rearrange("(kc p) n -> p kc n", p=128)
                W2v = W2.rearrange("(kc p) n -> p kc n", p=128)
                for p0 in range(0, FK, PART):
                    pn = min(PART, FK - p0)
                    for j0 in range(0, pn, 2):
                        jn = min(2, pn - j0)
                        wi = wctr[0] % 2
                        wctr[0] += 1
                        w1, w3 = w1b[wi], w3b[wi]
                        col = (p0 + j0) * 128
                        self.load(w1, w1.h[:, :, 0:jn * 128], W1v[:, :, col:col + jn * 128], eng="pool")
                        self.load(w3, w3.h[:, :, 0:jn * 128], W3v[:, :, col:col + jn * 128], eng="pool")
                        for jj in range(jn):
                            j = j0 + jj
                            for (c0, n) in sc:
                                lo = c0 - s0
                                i = ctr[0] % 2
                                ctr[0] += 1
                                pa, pb = self.ps[2 * i], self.ps[2 * i + 1]
                                for kc in range(KC):
                                    self.mm(pa, pa.h[:, 0:n], w1.h[:, kc, jj * 128:(jj + 1) * 128],
                                            hb.h[:, kc, lo:lo + n], [w1, hb], kc == 0, kc == KC - 1)
                                for kc in range(KC):
                                    self.mm(pb, pb.h[:, 0:n], w3.h[:, kc, jj * 128:(jj + 1) * 128],
                                            hb.h[:, kc, lo:lo + n], [w3, hb], kc == 0, kc == KC - 1)
                                self.act(sa[i], sa[i].h[:, 0:n], pa, pa.h[:, 0:n], AF.Silu)
                                if gated:
                                    gbt = gbs[i]
                                    self.load(gbt, gbt.h[:, 0:n], s["gtT"][ei, c0:c0 + n].partition_broadcast(128))
                                    self.tt(sa[i], sa[i].h[:, 0:n], sa[i], sa[i].h[:, 0:n], gbt, gbt.h[:, 0:n],
                                            ALU.mult)
                                self.tt(G_, G_.h[:, j, lo:lo + n], sa[i], sa[i].h[:, 0:n], pb, pb.h[:, 0:n], ALU.mult)
                    for f0 in range(0, KC, 2):
                        fn_ = min(2, KC - f0)
                        wi = wctr[0] % 2
                        wctr[0] += 1
                        w2 = w2b[wi]
                        self.load(w2, w2.h[:, 0:pn, 0:fn_ * 128], W2v[:, p0:p0 + pn, f0 * 128:(f0 + fn_) * 128],
                                  eng="pool")
                        for ff in range(fn_):
                            f = f0 + ff
                            for (c0, n) in sc:
                                lo = c0 - s0
                                ps = self.ps[4 + ctr[0] % 4]
                                ctr[0] += 1
                                for j in range(pn):
                                    self.mm(ps, ps.h[:, 0:n], w2.h[:, j, ff * 128:(ff + 1) * 128], G_.h[:, j, lo:lo + n],
                                            [w2, G_], j == 0, j == pn - 1)
                                if first_acc and p0 == 0:
                                    hi = ctr[0] % 2
                                    hrt = hr[hi]
                                    self.load(hrt, hrt.h[:, 0:n], s["hres"][f * 128:(f + 1) * 128, c0:c0 + n])
                                    k.op("dve", lambda e, f=f, lo=lo, n=n, ps=ps, hrt=hrt: e.scalar_tensor_tensor(
                                        acc.h[:, f, lo:lo + n], hrt.h[:, 0:n], cfg.ALPHA, ps.h[:, 0:n], ALU.mult,
                                        ALU.add), reads=[hrt, ps], writes=[acc])
                                else:
                                    self.tt(acc, acc.h[:, f, lo:lo + n], acc, acc.h[:, f, lo:lo + n], ps, ps.h[:, 0:n],
                                            ALU.add)
                first_acc = False
            self.store(self.fm(s["yT"])[:, :, s0:s0 + sw], acc, acc.h[:, :, 0:sw])


_BUCKET_STARTS = [8, 12, 16, 23, 32, 46, 64, 91]


def _bucket(rel):
    rel = np.asarray(rel)
    n = np.abs(rel)
    b = np.where(n < 8, n, 8 + sum((n >= t).astype(np.int64) for t in _BUCKET_STARTS[1:]))
    return np.where(rel > 0, 16, 0) + b


def _host_tables(cfg, rel_bias):
    T, G = cfg.T, cfg.GRP
    pos = np.concatenate([cfg.NMETA + np.arange(cfg.SEQ), np.arange(cfg.NMETA)]).astype(np.float32)
    inv = (np.float32(10000.0) ** (-np.arange(0, cfg.ROPE, 2, dtype=np.float32) / np.float32(cfg.ROPE))).astype(np.float32)
    ang = pos[None, :] * inv[:, None]
    cos = np.cos(ang).astype(np.float32)
    sin = np.sin(ang).astype(np.float32)
    ropet = np.stack([np.concatenate([cos, cos], 0), np.concatenate([-sin, sin], 0)]).astype(np.float32)
    rb = np.asarray(rel_bias, np.float32)

    def tab(rel, masked, heads):
        bk = _bucket(rel)
        outs = []
        for h in heads:
            t = rb[bk, h]
            t = np.where(masked, np.float32(NEGV), t)
            outs.append(t)
        return np.concatenate(outs, axis=1).astype(np.float32)

    kk = np.arange(128)[:, None]
    qq = np.arange(128)[None, :]
    biasd = np.zeros((cfg.SKV, 3, 128, G * 128), np.float32)
    biasmk = np.zeros((cfg.SKV, 2, 16, G * 128), np.float32)
    biasqm = np.zeros((cfg.SKV, 144, G * 16), np.float32)
    mj = np.arange(16)[:, None]
    for hk in range(cfg.SKV):
        heads = [hk * G + g for g in range(G)]
        for d in (-1, 0, 1):
            rel = d * 128 + kk - qq
            biasd[hk, d + 1] = tab(rel, np.abs(rel) > 128, heads)
        rel0 = mj - (16 + qq)
        biasmk[hk, 0] = tab(rel0, np.zeros_like(rel0, bool), heads)
        rel1 = mj - (16 + 128 + qq)
        biasmk[hk, 1] = tab(rel1, np.zeros_like(rel1, bool), heads)
        q16 = np.arange(16)[None, :]
        relm = mj - q16
        biasqm[hk, 0:16] = tab(relm, np.zeros_like(relm, bool), heads)
        relb = (16 + kk) - q16
        biasqm[hk, 16:144] = tab(relb, relb > 128, heads)
    return ropet, biasd, biasmk, biasqm


def _fmcol(v):
    v = np.asarray(v, np.float32)
    return np.ascontiguousarray(v.reshape(-1, 128).T)


def prepare_inputs(cfg, inp):
    L = cfg.DEPTH
    ropet, biasd, biasmk, biasqm = _host_tables(cfg, inp["rel_bias"])
    consts = np.zeros((128, 384), np.float32)
    consts[:, 0:128] = np.eye(128, dtype=np.float32)
    consts[:, 128:256] = 1.0
    consts[:, 256:384] = np.triu(np.ones((128, 128), np.float32), 1)
    iotac = np.ascontiguousarray(np.broadcast_to(np.arange(cfg.CAP, dtype=np.float32)[None, :], (128, cfg.CAP)))
    slotid = (np.arange(cfg.CAP // 128, dtype=np.float32)[None, :] * 128 + np.arange(128, dtype=np.float32)[:, None])
    slotid = np.ascontiguousarray(slotid.astype(np.float32))
    cols = [_fmcol(inp["emb_ln_g"]), _fmcol(inp["emb_ln_b"])]
    for l in range(L):
        cols += [_fmcol(inp["ln_mix_g"][l]), _fmcol(inp["ln_mix_b"][l]), _fmcol(inp["ln_ffn_g"][l]),
                 _fmcol(inp["ln_ffn_b"][l]), _fmcol(inp["q_norm_g"][l]), _fmcol(inp["kv_norm_g"][l])]
    pvec = np.ascontiguousarray(np.concatenate(cols, axis=1))
    w_in = np.asarray(inp["w_in"], np.float32)
    ro = cfg.QR + cfg.KVR
    half = cfg.ROPE // 2
    rot = np.concatenate([w_in[:, :, ro + half:ro + cfg.ROPE], w_in[:, :, ro:ro + half]], axis=2)
    w_in_e = np.ascontiguousarray(np.concatenate([w_in, rot], axis=2))
    w_uq = np.asarray(inp["w_uq"], np.float32).reshape(L, cfg.QR, cfg.MH, cfg.NOPE + cfg.ROPE)
    qn = w_uq[..., :cfg.NOPE].reshape(L, cfg.QR, -1)
    qr = w_uq[..., cfg.NOPE:]
    qrot = np.concatenate([qr[..., half:], qr[..., :half]], axis=-1)
    qrr = np.concatenate([qr, qrot], axis=-1).reshape(L, cfg.QR, -1)
    w_uq_e = np.ascontiguousarray(np.concatenate([qn, qrr], axis=2))
    w_ukv = np.asarray(inp["w_ukv"], np.float32).reshape(L, cfg.KVR, cfg.MH, cfg.NOPE + cfg.VD)
    w_ukv_e = np.ascontiguousarray(np.concatenate([w_ukv[..., :cfg.NOPE].reshape(L, cfg.KVR, -1),
                                                   w_ukv[..., cfg.NOPE:].reshape(L, cfg.KVR, -1)], axis=2))
    sinkb = np.ascontiguousarray(np.broadcast_to(np.asarray(inp["sink_logits"], np.float32)[:, None, :],
                                                 (L, 128, cfg.SH)))
    shared = dict(meta=np.asarray(inp["meta_tokens"], np.float32), consts=consts, iotac=iotac, slotid=slotid,
                  pvec=pvec, ropet=ropet,
                  biasd=biasd, biasmk=biasmk, biasqm=biasqm, sinkb=sinkb, w_in=w_in_e, w_uq=w_uq_e, w_ukv=w_ukv_e,
                  w_pa=np.asarray(inp["w_proj_a"], np.float32), w_pb=np.asarray(inp["w_proj_b"], np.float32),
                  w_out=np.asarray(inp["w_out"], np.float32), ffn_w1=np.asarray(inp["ffn_w1"], np.float32),
                  ffn_w3=np.asarray(inp["ffn_w3"], np.float32), ffn_w2=np.asarray(inp["ffn_w2"], np.float32))
    if L // 2:
        nm = L // 2
        shared["router_w"] = np.asarray(inp["router_w"], np.float32)
        shared["router_bb"] = np.ascontiguousarray(np.broadcast_to(np.asarray(inp["router_b"], np.float32)[:, None, :],
                                                                   (nm, 128, cfg.NE)))
        shared["moe_w1"] = np.asarray(inp["moe_w1"], np.float32).reshape(nm * cfg.NE, cfg.D, cfg.DFF)
        shared["moe_w3"] = np.asarray(inp["moe_w3"], np.float32).reshape(nm * cfg.NE, cfg.D, cfg.DFF)
        shared["moe_w2"] = np.asarray(inp["moe_w2"], np.float32).reshape(nm * cfg.NE, cfg.DFF, cfg.D)
    return shared


def run(cfg, inp, debug=None, stop_after=None, trace=False):
    prog = Prog(cfg, debug=debug, stop_after=stop_after)
    nc = prog.build()
    shared = prepare_inputs(cfg, inp)
    x = np.asarray(inp["x"], np.float32)
    in_maps = []
    for c in range(cfg.NCORES):
        m = dict(shared)
        m["x"] = np.ascontiguousarray(x[c])
        in_maps.append(m)
    res = run_bass_kernel_spmd(nc, in_maps, core_ids=list(range(cfg.NCORES)), trace=trace)
    return res, prog


def kernel(**inputs):
    cfg = Cfg()
    res, _ = run(cfg, inputs)
    return np.stack([np.asarray(r["out"], np.float32) for r in res.results], axis=0)
```

```python
import math
import os
from contextlib import ExitStack
import numpy as np
import concourse.bass as bass
import concourse.mybir as mybir
from concourse.bass_utils import run_bass_kernel_spmd

F32 = mybir.dt.float32
BF16 = mybir.dt.bfloat16
ALU = mybir.AluOpType
AF = mybir.ActivationFunctionType
AX = mybir.AxisListType

NEGV = -1.0e30


class Cfg:
    def __init__(self, **kw):
        self.D = 2048
        self.SEQ = 4096
        self.DEPTH = 2
        self.NMETA = 16
        self.MH = 16
        self.NOPE = 128
        self.ROPE = 64
        self.VD = 128
        self.QR = 768
        self.KVR = 512
        self.SH = 16
        self.SKV = 4
        self.HD = 128
        self.DFF = 5632
        self.NE = 8
        self.NCORES = 8
        self.SUPER = 1024
        self.FFPART = 11
        self.CAP = 1536
        for k, v in kw.items():
            setattr(self, k, v)
        self.T = self.SEQ + self.NMETA
        self.NB = self.SEQ // 128
        self.KC = self.D // 128
        self.GRP = self.SH // self.SKV
        self.chunks = [(c0, 512) for c0 in range(0, self.SEQ, 512)] + [(self.SEQ, self.NMETA)]
        self.IN_DIM = self.QR + self.KVR + self.ROPE + self.SH * self.HD + 2 * self.SKV * self.HD + 2 * self.D
        self.LN_EPS = 1e-5
        self.RMS_EPS = 1e-6
        self.ALPHA = (2 * self.DEPTH) ** 0.25


class Buf:
    __slots__ = ("name", "w", "r", "sem", "cnt")

    def __init__(self, name):
        self.name = name
        self.w = {}
        self.r = {}
        self.sem = None
        self.cnt = 0


class Tl:
    __slots__ = ("b", "h")

    def __init__(self, b, h):
        self.b = b
        self.h = h


ENGS = ("pe", "act", "dve", "pool", "sp")


class KB:
    def __init__(self, nc, stack, sb_limit):
        self.nc = nc
        self.stack = stack
        self.q = {e: [] for e in ENGS}
        self.cnt = {e: 0 for e in ENGS}
        self.clock = {e: {} for e in ENGS}
        self.semh = {}
        self.esem = {}
        for e in ENGS:
            self.esem[e] = self.newsem("E_" + e)
        self.dfree = []
        self.dtotal = {}
        self.dall = []
        self.phase_bufs = []
        self.sb_base = 16512
        self.sb_off = 16512
        self.sb_limit = sb_limit
        self.uid = 0
        self.ninst = 0

    def newsem(self, name):
        h = self.stack.enter_context(self.nc.semaphore(name))
        self.semh[name] = h
        return name

    def sb(self, name, shape, dtype, persistent=False):
        esz = 4 if dtype == F32 else 2
        n = 1
        for s in shape[1:]:
            n *= s
        nbytes = (n * esz + 63) // 64 * 64
        self.uid += 1
        off = self.sb_off
        assert off + nbytes <= self.sb_limit, f"SBUF overflow allocating {name}: {off}+{nbytes}"
        h = self.nc.alloc_sbuf_tensor_at(f"{name}_{self.uid}", list(shape), dtype, offset=off)
        self.sb_off = off + nbytes
        if persistent:
            assert self.sb_base == off, "persistent allocs must come first"
            self.sb_base = self.sb_off
        b = Buf(name)
        if not persistent:
            self.phase_bufs.append(b)
        return Tl(b, h)

    def _deps(self, eng, reads, writes):
        mysem = self.esem[eng]
        ck = self.clock[eng]
        waits = {}

        def need(sem, val):
            if ck.get(sem, 0) >= val:
                return
            ck[sem] = val
            waits[sem] = val

        for t in reads:
            for sem, val in t.b.w.items():
                if sem == mysem and eng == "pe":
                    continue
                need(sem, val)
        for t in writes:
            for sem, val in t.b.w.items():
                if sem == mysem:
                    continue
                need(sem, val)
            for sem, val in t.b.r.items():
                if sem == mysem:
                    continue
                need(sem, val)
        return list(waits.items())

    def _mark(self, ev, reads, writes):
        sem, val = ev
        for t in reads:
            if t.b.r.get(sem, 0) < val:
                t.b.r[sem] = val
        for t in writes:
            t.b.w = {sem: val}
            t.b.r = {}

    def op(self, eng, fn, reads=(), writes=(), inc=True):
        waits = self._deps(eng, reads, writes)
        idx = self.cnt[eng] + 1
        if inc:
            self.cnt[eng] = idx
        ev = (self.esem[eng], idx)
        self.q[eng].append((waits, fn, (self.esem[eng], 1) if inc else None))
        self._mark(ev, reads, writes)
        self.ninst += 1
        return ev

    def dma(self, eng, out, in_, owner, reads=(), writes=(), **kw):
        b = owner.b
        if b.sem is None:
            if self.dfree:
                b.sem = self.dfree.pop()
            else:
                b.sem = self.newsem(f"D{len(self.dall)}")
                self.dall.append(b.sem)
                self.dtotal[b.sem] = 0
        waits = self._deps(eng, reads, writes)
        self.dtotal[b.sem] += 16
        ev = (b.sem, self.dtotal[b.sem])
        self.q[eng].append((waits, (lambda e: e.dma_start(out=out, in_=in_, **kw)), (b.sem, 16)))
        self._mark(ev, reads, writes)
        self.ninst += 1
        return ev

    def barrier(self):
        for e in ENGS:
            waits = []
            ck = self.clock[e]
            for e2 in ENGS:
                if e2 == e or self.cnt[e2] == 0:
                    continue
                s = self.esem[e2]
                if ck.get(s, 0) < self.cnt[e2]:
                    ck[s] = self.cnt[e2]
                    waits.append((s, self.cnt[e2]))
            for s in self.dall:
                v = self.dtotal[s]
                if v > 0 and ck.get(s, 0) < v:
                    ck[s] = v
                    waits.append((s, v))
            if waits:
                self.q[e].append((waits, None, None))
        for b in self.phase_bufs:
            if b.sem is not None:
                self.dfree.append(b.sem)
                b.sem = None
        self.phase_bufs = []
        self.sb_off = self.sb_base

    def emit(self):
        nc = self.nc
        engmap = {"pe": "tensor", "act": "scalar", "dve": "vector", "pool": "gpsimd", "sp": "sync"}
        with nc.Block() as block:
            for e in ENGS:
                q = self.q[e]
                semh = self.semh

                def body(eh, q=q):
                    for waits, fn, inc in q:
                        for s, v in waits:
                            eh.wait_ge(semh[s], v)
                        if fn is not None:
                            ins = fn(eh)
                            if inc is not None:
                                ins.then_inc(semh[inc[0]], inc[1])

                getattr(block, engmap[e])(body)


class Prog:
    def __init__(self, cfg, debug=None, stop_after=None):
        self.cfg = cfg
        self.debug = debug or []
        self.stop_after = stop_after
        self.nc = bass.Bass("TRN2", target_bir_lowering=False)
        self.stack = ExitStack()
        self.k = KB(self.nc, self.stack, sb_limit=229376 - 256)
        self.dr = {}

    def din(self, name, shape, dtype=F32):
        t = self.nc.dram_tensor(name, list(shape), dtype, kind="ExternalInput").ap()
        self.dr[name] = t
        return t

    def dscr(self, name, shape, dtype):
        kind = "ExternalOutput" if name in self.debug else "Internal"
        t = self.nc.dram_tensor(name, list(shape), dtype, kind=kind).ap()
        self.dr[name] = t
        return t

    def fm(self, ap):
        return ap.rearrange("(kc p) t -> p kc t", p=128)

    def mm(self, ps, ps_ap, lhsT_ap, rhs_ap, reads, start, stop, inc=None):
        self.k.op("pe", lambda e: e.matmul(ps_ap, lhsT_ap, rhs_ap, start=start, stop=stop),
                  reads=reads, writes=[ps], inc=(stop if inc is None else inc))

    def act(self, out_t, out_ap, in_t, in_ap, func, extra_reads=(), **kw):
        self.k.op("act", lambda e: e.activation(out_ap, in_ap, func, **kw),
                  reads=[in_t, *extra_reads], writes=[out_t])

    def tt(self, out_t, out_ap, a_t, a_ap, b_t, b_ap, op, eng="dve"):
        self.k.op(eng, lambda e: e.tensor_tensor(out_ap, a_ap, b_ap, op), reads=[a_t, b_t], writes=[out_t])

    def ts(self, out_t, out_ap, a_t, a_ap, s1, s2, op0, op1=None, extra_reads=(), eng="dve"):
        if op1 is None:
            self.k.op(eng, lambda e: e.tensor_scalar(out_ap, a_ap, s1, None, op0),
                      reads=[a_t, *extra_reads], writes=[out_t])
        else:
            self.k.op(eng, lambda e: e.tensor_scalar(out_ap, a_ap, s1, s2, op0, op1),
                      reads=[a_t, *extra_reads], writes=[out_t])

    def cp(self, out_t, out_ap, in_t, in_ap, eng="dve"):
        if eng == "act":
            self.k.op("act", lambda e: e.copy(out_ap, in_ap), reads=[in_t], writes=[out_t])
        else:
            self.k.op(eng, lambda e: e.tensor_copy(out_ap, in_ap), reads=[in_t], writes=[out_t])

    def load(self, dst_t, dst_ap, src_ap, eng="sp"):
        self.k.dma(eng, dst_ap, src_ap, owner=dst_t, writes=[dst_t])

    def store(self, dst_ap, src_t, src_ap, eng="sp"):
        self.k.dma(eng, dst_ap, src_ap, owner=src_t, reads=[src_t])

    def build(self):
        cfg = self.cfg
        k = self.k
        nc = self.nc
        D, T, KC = cfg.D, cfg.T, cfg.KC
        L = cfg.DEPTH
        x = self.din("x", [cfg.SEQ, D])
        meta = self.din("meta", [cfg.NMETA, D])
        consts = self.din("consts", [128, 3 * 128])
        self.iotac = self.din("iotac", [128, cfg.CAP])
        self.slotid = self.din("slotid", [128, cfg.CAP // 128])
        pvec = self.din("pvec", [128, self.npvec()])
        ropet = self.din("ropet", [2, 64, T])
        biasd = self.din("biasd", [cfg.SKV, 3, 128, cfg.GRP * 128])
        biasmk = self.din("biasmk", [cfg.SKV, 2, 16, cfg.GRP * 128])
        biasqm = self.din("biasqm", [cfg.SKV, 144, cfg.GRP * 16])
        sinkb = self.din("sinkb", [L, 128, cfg.SH])
        w_in = self.din("w_in", [L, D, cfg.IN_DIM + cfg.ROPE])
        w_uq = self.din("w_uq", [L, cfg.QR, cfg.MH * (cfg.NOPE + 2 * cfg.ROPE)])
        w_ukv = self.din("w_ukv", [L, cfg.KVR, cfg.MH * (cfg.NOPE + cfg.VD)])
        w_pa = self.din("w_pa", [L, cfg.MH * cfg.VD, D])
        w_pb = self.din("w_pb", [L, cfg.SH * cfg.HD, D])
        w_out = self.din("w_out", [L, D, D])
        n_dense = (L + 1) // 2
        n_moe = L // 2
        ffn_w1 = self.din("ffn_w1", [n_dense, D, cfg.DFF])
        ffn_w3 = self.din("ffn_w3", [n_dense, D, cfg.DFF])
        ffn_w2 = self.din("ffn_w2", [n_dense, cfg.DFF, D])
        if n_moe:
            router_w = self.din("router_w", [n_moe, D, cfg.NE])
            router_bb = self.din("router_bb", [n_moe, 128, cfg.NE])
            moe_w1 = self.din("moe_w1", [n_moe * cfg.NE, D, cfg.DFF])
            moe_w3 = self.din("moe_w3", [n_moe * cfg.NE, D, cfg.DFF])
            moe_w2 = self.din("moe_w2", [n_moe * cfg.NE, cfg.DFF, D])
        out = self.nc.dram_tensor("out", [cfg.SEQ, D], F32, kind="ExternalOutput").ap()
        s = self.s = {}
        s["hres"] = self.dscr("hres", [D, T], F32)
        s["hb"] = self.dscr("hb", [D, T], BF16)
        s["yT"] = self.dscr("yT", [D, T], F32)
        s["cqT"] = self.dscr("cqT", [cfg.QR, T], BF16)
        s["ckvT"] = self.dscr("ckvT", [cfg.KVR, T], BF16)
        s["krT"] = self.dscr("krT", [cfg.ROPE, T], BF16)
        s["qsT"] = self.dscr("qsT", [cfg.SH * cfg.HD, T], BF16)
        s["ksT"] = self.dscr("ksT", [cfg.SKV * cfg.HD, T], BF16)
        s["vs"] = self.dscr("vs", [T, cfg.SKV * cfg.HD], BF16)
        s["gaT"] = self.dscr("gaT", [D, T], BF16)
        s["gbT"] = self.dscr("gbT", [D, T], BF16)
        s["qnT"] = self.dscr("qnT", [cfg.MH * cfg.NOPE, T], BF16)
        s["qrT"] = self.dscr("qrT", [cfg.MH * cfg.ROPE, T], BF16)
        s["knT"] = self.dscr("knT", [cfg.MH * cfg.NOPE, T], BF16)
        s["vm"] = self.dscr("vm", [T, cfg.MH * cfg.VD], BF16)
        s["oaT"] = self.dscr("oaT", [cfg.MH * cfg.VD, T], BF16)
        s["obT"] = self.dscr("obT", [cfg.SH * cfg.HD, T], BF16)
        s["tAT"] = self.dscr("tAT", [D, T], F32)
        s["mgT"] = self.dscr("mgT", [D, T], BF16)
        s["gtT"] = self.dscr("gtT", [max(cfg.NE, 1), T], F32)
        s["pmT"] = self.dscr("pmT", [cfg.NE, T], F32)
        s["pmtm"] = self.dscr("pmtm", [T, cfg.NE], F32)
        s["htm"] = self.dscr("htm", [T, D], BF16)
        s["XeT"] = self.dscr("XeT", [cfg.NE * D, cfg.CAP], BF16)
        s["Yd"] = self.dscr("Yd", [cfg.NE * cfg.CAP, D], BF16)

        P = self.P = {}
        P["ident"] = k.sb("ident", [128, 128], F32, persistent=True)
        P["ones"] = k.sb("ones", [128, 128], F32, persistent=True)
        P["identb"] = k.sb("identb", [128, 128], BF16, persistent=True)
        P["onesb"] = k.sb("onesb", [128, 128], BF16, persistent=True)
        P["pvec"] = k.sb("pvec", [128, self.npvec()], F32, persistent=True)
        P["upper"] = k.sb("upper", [128, 128], F32, persistent=True)
        self.ps = [Tl(Buf(f"ps{i}"), nc.alloc_psum_tensor(f"ps{i}", [128, 512], F32)) for i in range(8)]
        self.load(P["ident"], P["ident"].h[:, :], consts[:, 0:128])
        self.load(P["ones"], P["ones"].h[:, :], consts[:, 128:256])
        self.load(P["identb"], P["identb"].h[:, :], consts[:, 0:128], eng="pool")
        self.load(P["onesb"], P["onesb"].h[:, :], consts[:, 128:256], eng="pool")
        self.load(P["pvec"], P["pvec"].h[:, :], pvec[:, :])
        self.load(P["upper"], P["upper"].h[:, :], consts[:, 256:384])
        self.ropet = ropet

        pv = self.pvoff()
        self.phase_embed(x, meta, pv["emb_g"], pv["emb_b"])
        k.barrier()
        done = self.stop_after == "embed"
        for l in range(L):
            if done:
                break
            self.phase_inproj(l, w_in[l])
            k.barrier()
            if self.stop_after == f"inproj{l}":
                break
            self.phase_mla_up(l, w_uq[l], w_ukv[l], pv[f"qn_g{l}"], pv[f"kvn_g{l}"])
            k.barrier()
            if self.stop_after == f"mlaup{l}":
                break
            self.phase_mla_attn(l)
            k.barrier()
            if self.stop_after == f"mlaattn{l}":
                break
            self.phase_swa(l, biasd, biasmk, biasqm, sinkb[l])
            k.barrier()
            if self.stop_after == f"swa{l}":
                break
            self.phase_proj(l, w_pa[l], w_pb[l], w_out[l])
            k.barrier()
            self.phase_ln(pv[f"mix_g{l}"], pv[f"mix_b{l}"])
            k.barrier()
            if self.stop_after == f"mix{l}":
                break
            if l % 2 == 0:
                self.phase_ffn([(ffn_w1[l // 2], ffn_w3[l // 2], ffn_w2[l // 2])], gated=False)
            else:
                self.phase_router(router_w[l // 2], router_bb[l // 2])
                k.barrier()
                if self.stop_after == f"router{l}":
                    break
                m = l // 2
                self.phase_gather()
                k.barrier()
                if self.stop_after == f"gather{l}":
                    break
                self.phase_moe_ffn([(moe_w1[m * cfg.NE + e], moe_w3[m * cfg.NE + e], moe_w2[m * cfg.NE + e])
                                    for e in range(cfg.NE)])
                k.barrier()
                if self.stop_after == f"moeffn{l}":
                    break
                self.phase_combine()
                if self.stop_after == f"combine{l}":
                    k.barrier()
                    break
            k.barrier()
            self.phase_ln(pv[f"ffn_g{l}"], pv[f"ffn_b{l}"])
            k.barrier()
        self.phase_output(out)
        k.barrier()
        k.emit()
        self.stack.close()
        return nc

    def pv_layout(self):
        cfg = self.cfg
        items = [("emb_g", cfg.KC), ("emb_b", cfg.KC)]
        for l in range(cfg.DEPTH):
            items += [(f"mix_g{l}", cfg.KC), (f"mix_b{l}", cfg.KC), (f"ffn_g{l}", cfg.KC), (f"ffn_b{l}", cfg.KC),
                      (f"qn_g{l}", cfg.QR // 128), (f"kvn_g{l}", cfg.KVR // 128)]
        return items

    def npvec(self):
        return sum(n for _, n in self.pv_layout())

    def pvoff(self):
        o = {}
        off = 0
        for name, n in self.pv_layout():
            o[name] = (off, n)
            off += n
        return o

    def ln_fm(self, y, n, gsl, bsl, c0, tmp):
        cfg, k, P = self.cfg, self.k, self.P
        KC, D = cfg.KC, cfg.D
        sq, mean, rstd, mr, msq, ob = tmp[:6]
        sbk = tmp[6] if len(tmp) > 6 else 6
        yv = y.h[:, :, 0:n]
        self.act(sq, sq.h[:, :, 0:n], y, yv, AF.Square)
        ps_s, ps_q = self.ps[sbk], self.ps[sbk + 1]
        for kc in range(KC):
            self.mm(ps_s, ps_s.h[:, 0:n], P["ones"].h[:, :], y.h[:, kc, 0:n], [P["ones"], y], kc == 0, kc == KC - 1)
        for kc in range(KC):
            self.mm(ps_q, ps_q.h[:, 0:n], P["ones"].h[:, :], sq.h[:, kc, 0:n], [P["ones"], sq], kc == 0, kc == KC - 1)
        self.ts(mean, mean.h[:, 0:n], ps_s, ps_s.h[:, 0:n], 1.0 / D, None, ALU.mult)
        self.tt(msq, msq.h[:, 0:n], mean, mean.h[:, 0:n], mean, mean.h[:, 0:n], ALU.mult)
        k.op("dve", lambda e: e.scalar_tensor_tensor(rstd.h[:, 0:n], ps_q.h[:, 0:n], 1.0 / D, msq.h[:, 0:n],
                                                     ALU.mult, ALU.subtract),
             reads=[ps_q, msq], writes=[rstd])
        self.ts(rstd, rstd.h[:, 0:n], rstd, rstd.h[:, 0:n], cfg.LN_EPS, None, ALU.add)
        self.act(rstd, rstd.h[:, 0:n], rstd, rstd.h[:, 0:n], AF.Sqrt)
        k.op('dve', lambda e: e.reciprocal(rstd.h[:, 0:n], rstd.h[:, 0:n]), reads=[rstd], writes=[rstd])
        self.tt(mr, mr.h[:, 0:n], mean, mean.h[:, 0:n], rstd, rstd.h[:, 0:n], ALU.mult)
        rb = rstd.h[:, 0:n].unsqueeze(1).to_broadcast([128, KC, n])
        mb = mr.h[:, 0:n].unsqueeze(1).to_broadcast([128, KC, n])
        self.tt(y, yv, y, yv, rstd, rb, ALU.mult)
        self.tt(y, yv, y, yv, mr, mb, ALU.subtract)
        pvh = P["pvec"]
        gb = pvh.h[:, gsl[0]:gsl[0] + KC].unsqueeze(2).to_broadcast([128, KC, n])
        bb = pvh.h[:, bsl[0]:bsl[0] + KC].unsqueeze(2).to_broadcast([128, KC, n])
        self.tt(y, yv, y, yv, pvh, gb, ALU.mult)
        self.tt(y, yv, y, yv, pvh, bb, ALU.add)
        self.cp(ob, ob.h[:, :, 0:n], y, yv, eng="act")
        self.store(self.fm(self.s["hres"])[:, :, c0:c0 + n], y, yv)
        self.store(self.fm(self.s["hb"])[:, :, c0:c0 + n], ob, ob.h[:, :, 0:n])

    def ln_tmps(self):
        k, KC = self.k, self.cfg.KC
        return (k.sb("sq", [128, KC, 512], F32), k.sb("mean", [128, 512], F32), k.sb("rstd", [128, 512], F32),
                k.sb("mr", [128, 512], F32), k.sb("msq", [128, 512], F32), k.sb("ob", [128, KC, 512], BF16))

    def phase_embed(self, x, meta, gsl, bsl):
        cfg, k, P = self.cfg, self.k, self.P
        KC = cfg.KC
        xin = [k.sb(f"xin{i}", [128, 4, cfg.D], F32) for i in range(1)]
        ys = [k.sb(f"y{i}", [128, KC, 512], F32) for i in range(2)]
        tmp = self.ln_tmps()
        for ci, (c0, n) in enumerate(cfg.chunks):
            xt = xin[0]
            y = ys[ci % 2]
            if n == 512:
                self.load(xt, xt.h[:, :, :], x[c0:c0 + 512, :].rearrange("(a p) d -> p a d", p=128))
                nt, rows = 4, 128
            else:
                self.load(xt, xt.h[0:n, 0, :], meta[:, :])
                nt, rows = 1, n
            for kc in range(KC):
                ps = self.ps[kc % 4]
                for a in range(nt):
                    self.mm(ps, ps.h[:, a * 128:a * 128 + rows], xt.h[0:rows, a, kc * 128:(kc + 1) * 128],
                            P["ident"].h[0:rows, 0:rows], [xt, P["ident"]], True, True)
                self.cp(y, y.h[:, kc, 0:n], ps, ps.h[:, 0:n], eng=("act" if kc % 2 else "dve"))
            self.ln_fm(y, n, gsl, bsl, c0, tmp)

    def phase_ln(self, gsl, bsl):
        cfg, k = self.cfg, self.k
        KC = cfg.KC
        ys = [k.sb(f"y{i}", [128, KC, 512], F32) for i in range(2)]
        tmps = [self.ln_tmps() + (4,), self.ln_tmps() + (6,)]
        for ci, (c0, n) in enumerate(cfg.chunks):
            y = ys[ci % 2]
            self.load(y, y.h[:, :, 0:n], self.fm(self.s["yT"])[:, :, c0:c0 + n])
            self.ln_fm(y, n, gsl, bsl, c0, tmps[ci % 2])

    def phase_output(self, out):
        cfg, k, P = self.cfg, self.k, self.P
        KC = cfg.KC
        hs = [k.sb(f"h{i}", [128, KC, 512], F32) for i in range(2)]
        os_ = [k.sb(f"o{i}", [128, cfg.D], F32) for i in range(2)]
        cnt = 0
        for ci, (c0, n) in enumerate(cfg.chunks):
            if n != 512:
                continue
            h = hs[ci % 2]
            self.load(h, h.h[:, :, :], self.fm(self.s["hres"])[:, :, c0:c0 + n])
            for a in range(4):
                o = os_[cnt % 2]
                cnt += 1
                for g in range(0, KC, 4):
                    ps = self.ps[(g // 4) % 4]
                    for j in range(min(4, KC - g)):
                        self.mm(ps, ps.h[:, j * 128:(j + 1) * 128], h.h[:, g + j, a * 128:(a + 1) * 128],
                                P["ident"].h[:, :], [h, P["ident"]], True, True)
                    w = min(4, KC - g) * 128
                    self.cp(o, o.h[:, g * 128:g * 128 + w], ps, ps.h[:, 0:w], eng=("act" if (g // 4) % 2 else "dve"))
                self.store(out[c0 + a * 128:c0 + (a + 1) * 128, :], o, o.h[:, :])

    def load_x(self, name, dram, K):
        cfg, k = self.cfg, self.k
        kc = K // 128
        X = k.sb(name, [128, kc, cfg.T], BF16)
        v = self.fm(dram)
        step = 1024
        for c0 in range(0, cfg.T, step):
            n = min(step, cfg.T - c0)
            k.dma("sp", X.h[:, :, c0:c0 + n], v[:, :, c0:c0 + n], owner=X, writes=[])
        X.b.w = {X.b.sem: k.dtotal[X.b.sem]}
        return X

    def linear(self, X, KCn, W, ftiles, epi, wbufs, gw=512, chunks=None, xoff=0, pre=None):
        cfg, k = self.cfg, self.k
        chunks = chunks if chunks is not None else cfg.chunks
        Wv = W.rearrange("(kc p) n -> p kc n", p=128)
        groups = []
        cur = []
        for fi, (col0, M) in enumerate(ftiles):
            if cur and (col0 != cur[-1][1] + cur[-1][2] or (col0 + M - cur[0][1]) > gw):
                groups.append(cur)
                cur = []
            cur.append((fi, col0, M))
        if cur:
            groups.append(cur)
        pi = 0
        steps = []
        for gi, grp in enumerate(groups):
            for (fi, col0, M) in grp:
                for ci, (c0, n) in enumerate(chunks):
                    steps.append((fi, ci, c0, n))
        si = 0
        if pre is not None and steps:
            pre(*steps[0])
        for gi, grp in enumerate(groups):
            wt = wbufs[gi % 2]
            g0 = grp[0][1]
            gwid = grp[-1][1] + grp[-1][2] - g0
            self.load(wt, wt.h[:, :, 0:gwid], Wv[:, :, g0:g0 + gwid], eng="pool")
            for (fi, col0, M) in grp:
                for ci, (c0, n) in enumerate(chunks):
                    if pre is not None and si + 1 < len(steps):
                        pre(*steps[si + 1])
                    si += 1
                    ps = self.ps[pi % 4]
                    pi += 1
                    for kc in range(KCn):
                        self.mm(ps, ps.h[0:M, 0:n], wt.h[:, kc, col0 - g0:col0 - g0 + M],
                                X.h[:, kc, c0 - xoff:c0 - xoff + n], [wt, X], kc == 0, kc == KCn - 1)
                    epi(fi, ci, c0, n, ps)

    def linear_tm(self, X, KCn, W, col0, ncols, epi, wt):
        cfg = self.cfg
        Wv = W.rearrange("(kc p) n -> p kc n", p=128)
        self.load(wt, wt.h[:, :, 0:ncols], Wv[:, :, col0:col0 + ncols], eng="pool")
        ntile = (cfg.T + 127) // 128
        for ti in range(ntile):
            t0 = ti * 128
            rows = min(128, cfg.T - t0)
            ps = self.ps[ti % 4]
            for kc in range(KCn):
                self.mm(ps, ps.h[0:rows, 0:ncols], X.h[:, kc, t0:t0 + rows], wt.h[:, kc, 0:ncols], [wt, X],
                        kc == 0, kc == KCn - 1)
            epi(ti, t0, rows, ps)

    def load_rope(self):
        k, P = self.k, self.P
        P["cos"] = k.sb("cos", [64, self.cfg.T], F32)
        P["sin"] = k.sb("sin", [64, self.cfg.T], F32)
        self.load(P["cos"], P["cos"].h[:, :], self.ropet[0, :, :])
        self.load(P["sin"], P["sin"].h[:, :], self.ropet[1, :, :])

    def phase_inproj(self, l, W):
        cfg, k, P, s = self.cfg, self.k, self.P, self.s
        KC = cfg.KC
        X = self.load_x("X", s["hb"], cfg.D)
        self.load_rope()
        wb = [k.sb(f"w{i}", [128, KC, 256], BF16) for i in range(2)]
        st = [k.sb(f"st{i}", [128, 512], BF16) for i in range(4)]
        t2 = k.sb("t2", [64, 512], F32)
        sti = [0]

        def stage():
            t = st[sti[0] % 4]
            sti[0] += 1
            return t

        def mk_copy(dst, row0):
            def epi(fi, ci, c0, n, ps, dst=dst, row0=row0):
                t = stage()
                r = row0 + fi * 128
                self.cp(t, t.h[:, 0:n], ps, ps.h[:, 0:n], eng=("act" if sti[0] % 2 else "dve"))
                self.store(dst[r:r + 128, c0:c0 + n], t, t.h[:, 0:n])
            return epi

        def mk_sig(dst):
            def epi(fi, ci, c0, n, ps, dst=dst):
                t = stage()
                self.act(t, t.h[:, 0:n], ps, ps.h[:, 0:n], AF.Sigmoid)
                self.store(dst[fi * 128:(fi + 1) * 128, c0:c0 + n], t, t.h[:, 0:n])
            return epi

        o = 0
        self.linear(X, KC, W, [(o + i * 128, 128) for i in range(cfg.QR // 128)], mk_copy(s["cqT"], 0), wb, gw=256)
        o += cfg.QR
        self.linear(X, KC, W, [(o + i * 128, 128) for i in range(cfg.KVR // 128)], mk_copy(s["ckvT"], 0), wb, gw=256)
        o += cfg.KVR
        t1all = k.sb("t1all", [64, cfg.T], F32)

        def epi_r1(fi, ci, c0, n, ps):
            self.tt(t1all, t1all.h[:, c0:c0 + n], ps, ps.h[0:64, 0:n], P["cos"], P["cos"].h[:, c0:c0 + n], ALU.mult)

        def epi_r2(fi, ci, c0, n, ps):
            self.tt(t2, t2.h[:, 0:n], ps, ps.h[0:64, 0:n], P["sin"], P["sin"].h[:, c0:c0 + n], ALU.mult)
            t = stage()
            self.tt(t, t.h[0:64, 0:n], t1all, t1all.h[:, c0:c0 + n], t2, t2.h[:, 0:n], ALU.add)
            self.store(s["krT"][:, c0:c0 + n], t, t.h[0:64, 0:n])

        self.linear(X, KC, W, [(o, 64)], epi_r1, wb, gw=256)
        self.linear(X, KC, W, [(cfg.IN_DIM, 64)], epi_r2, wb, gw=256)
        o += cfg.ROPE
        self.linear(X, KC, W, [(o + i * 128, 128) for i in range(cfg.SH)], mk_copy(s["qsT"], 0), wb, gw=256)
        o += cfg.SH * cfg.HD
        self.linear(X, KC, W, [(o + i * 128, 128) for i in range(cfg.SKV)], mk_copy(s["ksT"], 0), wb, gw=256)
        o += cfg.SKV * cfg.HD
        ncv = cfg.SKV * cfg.HD

        def epi_v(ti, t0, rows, ps):
            t = stage()
            self.cp(t, t.h[0:rows, 0:ncv], ps, ps.h[0:rows, 0:ncv], eng=("act" if ti % 2 else "dve"))
            self.store(s["vs"][t0:t0 + rows, :], t, t.h[0:rows, 0:ncv])

        for v0 in range(0, ncv, 256):
            vn = min(256, ncv - v0)

            def epi_v(ti, t0, rows, ps, v0=v0, vn=vn):
                t = stage()
                self.cp(t, t.h[0:rows, 0:vn], ps, ps.h[0:rows, 0:vn], eng=('act' if ti % 2 else 'dve'))
                self.store(s['vs'][t0:t0 + rows, v0:v0 + vn], t, t.h[0:rows, 0:vn])

            self.linear_tm(X, KC, W, o + v0, vn, epi_v, wb[(v0 // 256) % 2])
        o += ncv
        self.linear(X, KC, W, [(o + i * 128, 128) for i in range(KC)], mk_sig(s["gaT"]), wb, gw=256)
        o += cfg.D
        self.linear(X, KC, W, [(o + i * 128, 128) for i in range(KC)], mk_sig(s["gbT"]), wb, gw=256)

    def rms_stats(self, X, KCn, gsl, rdim, rstd_bc, rstd_tm):
        cfg, k, P = self.cfg, self.k, self.P
        sqs = [k.sb(f"rsq{i}", [128, KCn, 512], BF16) for i in range(2)]
        for ci, (c0, n) in enumerate(cfg.chunks):
            sq = sqs[ci % 2]
            self.act(sq, sq.h[:, :, 0:n], X, X.h[:, :, c0:c0 + n], AF.Square)
            ps = self.ps[4 + ci % 2]
            for kc in range(KCn):
                self.mm(ps, ps.h[:, 0:n], P["onesb"].h[:, :], sq.h[:, kc, 0:n], [P["onesb"], sq], kc == 0, kc == KCn - 1)
            self.ts(rstd_bc, rstd_bc.h[:, c0:c0 + n], ps, ps.h[:, 0:n], 1.0 / rdim, cfg.RMS_EPS, ALU.mult, ALU.add)
            self.act(rstd_bc, rstd_bc.h[:, c0:c0 + n], rstd_bc, rstd_bc.h[:, c0:c0 + n], AF.Sqrt)
            k.op('dve', lambda e, c0=c0, n=n: e.reciprocal(rstd_bc.h[:, c0:c0 + n], rstd_bc.h[:, c0:c0 + n]), reads=[rstd_bc], writes=[rstd_bc])
            if rstd_tm is not None:
                for a in range((n + 127) // 128):
                    rows = min(128, n - a * 128)
                    ti = (c0 + a * 128) // 128
                    pt = self.ps[6 + a % 2]
                    for kc in range(KCn):
                        self.mm(pt, pt.h[0:rows, 0:1], sq.h[:, kc, a * 128:a * 128 + rows], P["onesb"].h[:, 0:1],
                                [P["onesb"], sq], kc == 0, kc == KCn - 1)
                    self.ts(rstd_tm, rstd_tm.h[0:rows, ti:ti + 1], pt, pt.h[0:rows, 0:1], 1.0 / rdim, cfg.RMS_EPS,
                            ALU.mult, ALU.add)
                    self.act(rstd_tm, rstd_tm.h[0:rows, ti:ti + 1], rstd_tm, rstd_tm.h[0:rows, ti:ti + 1], AF.Sqrt)
                    k.op('dve', lambda e, rows=rows, ti=ti: e.reciprocal(rstd_tm.h[0:rows, ti:ti + 1], rstd_tm.h[0:rows, ti:ti + 1]), reads=[rstd_tm], writes=[rstd_tm])
        pvh = P["pvec"]
        for kc in range(KCn):
            self.ts(X, X.h[:, kc, :], X, X.h[:, kc, :], pvh.h[:, gsl[0] + kc:gsl[0] + kc + 1], None, ALU.mult,
                    extra_reads=[pvh])

    def phase_mla_up(self, l, Wq, Wkv, qg, kvg):
        cfg, k, P, s = self.cfg, self.k, self.P, self.s
        T = cfg.T
        MH = cfg.MH
        ntile = (T + 127) // 128
        KQ = cfg.QR // 128
        X = self.load_x("Xq", s["cqT"], cfg.QR)
        rq = k.sb("rq", [128, T], F32)
        self.load_rope()
        self.rms_stats(X, KQ, qg, cfg.QR, rq, None)
        wb = [k.sb(f"w{i}", [128, max(KQ, cfg.KVR // 128), 512], BF16) for i in range(2)]
        st = [k.sb(f"st{i}", [128, 512], BF16) for i in range(4)]
        t1 = k.sb("t1", [64, 512], F32)
        t2 = k.sb("t2", [64, 512], F32)
        sti = [0]

        def stage():
            t = st[sti[0] % 4]
            sti[0] += 1
            return t

        def epi_qn(fi, ci, c0, n, ps):
            t = stage()
            self.tt(t, t.h[:, 0:n], ps, ps.h[:, 0:n], rq, rq.h[:, c0:c0 + n], ALU.mult)
            self.store(s["qnT"][fi * 128:(fi + 1) * 128, c0:c0 + n], t, t.h[:, 0:n])

        self.linear(X, KQ, Wq, [(h * 128, 128) for h in range(MH)], epi_qn, wb)
        ro = MH * cfg.NOPE
        t1all = k.sb("t1all", [64, T], F32)

        def epi_qr(fi, ci, c0, n, ps):
            h_ = fi // 2
            if fi % 2 == 0:
                self.tt(t1all, t1all.h[:, c0:c0 + n], ps, ps.h[0:64, 0:n], P["cos"], P["cos"].h[:, c0:c0 + n],
                        ALU.mult)
            else:
                self.tt(t2, t2.h[:, 0:n], ps, ps.h[0:64, 0:n], P["sin"], P["sin"].h[:, c0:c0 + n], ALU.mult)
                self.tt(t2, t2.h[:, 0:n], t2, t2.h[:, 0:n], t1all, t1all.h[:, c0:c0 + n], ALU.add)
                t = stage()
                self.tt(t, t.h[0:64, 0:n], t2, t2.h[:, 0:n], rq, rq.h[0:64, c0:c0 + n], ALU.mult)
                self.store(s["qrT"][h_ * 64:(h_ + 1) * 64, c0:c0 + n], t, t.h[0:64, 0:n])

        fts = []
        for h in range(MH):
            fts += [(ro + h * 128, 64), (ro + h * 128 + 64, 64)]
        self.linear(X, KQ, Wq, fts, epi_qr, wb)
        self.k.barrier()
        KV = cfg.KVR // 128
        X = self.load_x("Xkv", s["ckvT"], cfg.KVR)
        rk = k.sb("rk", [128, T], F32)
        rkt = k.sb("rkt", [128, ntile], F32)
        self.rms_stats(X, KV, kvg, cfg.KVR, rk, rkt)
        wb = [k.sb(f"w{i}", [128, KV, 512], BF16) for i in range(2)]
        st = [k.sb(f"st{i}", [128, 512], BF16) for i in range(4)]

        def epi_kn(fi, ci, c0, n, ps):
            t = stage2()
            self.tt(t, t.h[:, 0:n], ps, ps.h[:, 0:n], rk, rk.h[:, c0:c0 + n], ALU.mult)
            self.store(s["knT"][fi * 128:(fi + 1) * 128, c0:c0 + n], t, t.h[:, 0:n])

        sti2 = [0]

        def stage2():
            t = st[sti2[0] % 4]
            sti2[0] += 1
            return t

        self.linear(X, KV, Wkv, [(h * 128, 128) for h in range(MH)], epi_kn, wb)
        vo = MH * cfg.NOPE
        for g0 in range(0, MH * cfg.VD, 512):
            ncv = min(512, MH * cfg.VD - g0)

            def epi_v(ti, t0, rows, ps, g0=g0, ncv=ncv):
                t = stage2()
                self.ts(t, t.h[0:rows, 0:ncv], ps, ps.h[0:rows, 0:ncv], rkt.h[0:rows, ti:ti + 1], None, ALU.mult,
                        extra_reads=[rkt])
                self.store(s["vm"][t0:t0 + rows, g0:g0 + ncv], t, t.h[0:rows, 0:ncv])

            self.linear_tm(X, KV, Wkv, vo + g0, ncv, epi_v, wb[(g0 // 512) % 2])

    def phase_mla_attn(self, l):
        cfg, k, P, s = self.cfg, self.k, self.P, self.s
        T = cfg.T
        ntile = (T + 127) // 128
        scale = (cfg.NOPE + cfg.ROPE) ** -0.5
        kr = k.sb("kr", [64, T], BF16)
        self.load(kr, kr.h[:, :], s["krT"][:, :])
        kns = [k.sb(f"kn{i}", [128, T], BF16) for i in range(2)]
        vs_ = [k.sb(f"v{i}", [128, ntile, cfg.VD], BF16) for i in range(2)]
        qns = [k.sb(f"qn{i}", [128, 512], BF16) for i in range(2)]
        qrs = [k.sb(f"qr{i}", [64, 512], BF16) for i in range(2)]
        pts = [k.sb(f"pt{i}", [128, 512], BF16) for i in range(4)]
        sbank = [self.ps[0], self.ps[1], self.ps[2], self.ps[5]]
        rcs = [k.sb(f"rc{i}", [128, 512], F32) for i in range(2)]
        obs = [k.sb(f"ob{i}", [128, 512], BF16) for i in range(2)]
        nfull = T // 128
        rem = T - nfull * 128
        qi = 0
        for h in range(cfg.MH):
            kn = kns[h % 2]
            v = vs_[h % 2]
            self.load(kn, kn.h[:, :], s["knT"][h * 128:(h + 1) * 128, :])
            k.dma("sp", v.h[:, 0:nfull, :],
                  s["vm"][0:nfull * 128, h * cfg.VD:(h + 1) * cfg.VD].rearrange("(a p) d -> p a d", p=128),
                  owner=v, writes=[v])
            if rem:
                k.dma("sp", v.h[0:rem, nfull, :], s["vm"][nfull * 128:T, h * cfg.VD:(h + 1) * cfg.VD], owner=v,
                      writes=[])
                v.b.w = {v.b.sem: k.dtotal[v.b.sem]}
            for ci, (c0, n) in enumerate(cfg.chunks):
                qn = qns[qi % 2]
                qr = qrs[qi % 2]
                rc = rcs[qi % 2]
                ob = obs[qi % 2]
                qi += 1
                self.load(qn, qn.h[:, 0:n], s["qnT"][h * 128:(h + 1) * 128, c0:c0 + n])
                self.load(qr, qr.h[:, 0:n], s["qrT"][h * 64:(h + 1) * 64, c0:c0 + n])
                po, pd = self.ps[3], self.ps[4]

                def smm(kt):
                    kk = 128 if kt < nfull else rem
                    S = sbank[kt % 4]
                    self.mm(S, S.h[0:kk, 0:n], kn.h[:, kt * 128:kt * 128 + kk], qn.h[:, 0:n], [kn, qn], True, False)
                    self.mm(S, S.h[0:kk, 0:n], kr.h[:, kt * 128:kt * 128 + kk], qr.h[:, 0:n], [kr, qr], False, True)

                smm(0)
                if ntile > 1:
                    smm(1)
                for kt in range(ntile):
                    kk = 128 if kt < nfull else rem
                    if kt + 2 < ntile:
                        smm(kt + 2)
                    S = sbank[kt % 4]
                    pt = pts[kt % 4]
                    self.act(pt, pt.h[0:kk, 0:n], S, S.h[0:kk, 0:n], AF.Exp, scale=scale)
                    self.mm(po, po.h[:, 0:n], v.h[0:kk, kt, :], pt.h[0:kk, 0:n], [v, pt], kt == 0, kt == ntile - 1)
                    self.mm(pd, pd.h[:, 0:n], P["onesb"].h[0:kk, :], pt.h[0:kk, 0:n], [P["onesb"], pt], kt == 0,
                            kt == ntile - 1)
                k.op("dve", lambda e, rc=rc, pd=pd, n=n: e.reciprocal(rc.h[:, 0:n], pd.h[:, 0:n]), reads=[pd],
                     writes=[rc])
                self.tt(ob, ob.h[:, 0:n], po, po.h[:, 0:n], rc, rc.h[:, 0:n], ALU.mult)
                self.store(s["oaT"][h * 128:(h + 1) * 128, c0:c0 + n], ob, ob.h[:, 0:n])

    def phase_swa(self, l, biasd, biasmk, biasqm, sinkb):
        cfg, k, P, s = self.cfg, self.k, self.P, self.s
        T, NB, G = cfg.T, cfg.NB, cfg.GRP
        ntile = (T + 127) // 128
        scale = cfg.HD ** -0.5
        inv = 1.0 / scale
        GW = G * 128
        sk = k.sb("sk", [128, cfg.SH], F32)
        self.load(sk, sk.h[:, :], sinkb[:, :])
        self.act(sk, sk.h[:, :], sk, sk.h[:, :], AF.Exp)
        ks_ = [k.sb(f"ks{i}", [128, T], BF16) for i in range(2)]
        vs_ = [k.sb(f"v{i}", [128, ntile, cfg.HD], BF16) for i in range(2)]
        qs_ = [k.sb(f"q{i}", [128, G, T], BF16) for i in range(2)]
        bd = [k.sb(f"bd{i}", [128, 3, GW], BF16) for i in range(2)]
        bmk = [k.sb(f"bmk{i}", [16, 2, GW], BF16) for i in range(2)]
        bqm = [k.sb(f"bqm{i}", [128, 2, G * 16], BF16) for i in range(2)]
        ske = [k.sb(f"ske{i}", [128, G, 128], F32) for i in range(2)]
        pts = [k.sb(f"pt{i}", [128, GW], BF16) for i in range(4)]
        rcs = [k.sb(f"rc{i}", [128, GW], F32) for i in range(2)]
        obs = [k.sb(f"ob{i}", [128, G, 128], BF16) for i in range(2)]
        si = 0
        bi = 0
        for hk in range(cfg.SKV):
            ks, v, q = ks_[hk % 2], vs_[hk % 2], qs_[hk % 2]
            b_d, b_mk, b_qm, sk_e = bd[hk % 2], bmk[hk % 2], bqm[hk % 2], ske[hk % 2]
            self.load(ks, ks.h[:, :], s["ksT"][hk * 128:(hk + 1) * 128, :])
            k.dma("sp", v.h[:, 0:NB, :],
                  s["vs"][0:NB * 128, hk * cfg.HD:(hk + 1) * cfg.HD].rearrange("(a p) d -> p a d", p=128),
                  owner=v, writes=[v])
            k.dma("sp", v.h[0:16, NB, :], s["vs"][NB * 128:T, hk * cfg.HD:(hk + 1) * cfg.HD], owner=v, writes=[])
            v.b.w = {v.b.sem: k.dtotal[v.b.sem]}
            self.load(q, q.h[:, :, :],
                      s["qsT"][hk * G * 128:(hk + 1) * G * 128, :].rearrange("(g p) t -> p g t", p=128))
            self.load(b_d, b_d.h[:, :, :], biasd[hk].rearrange("d p w -> p d w"), eng="pool")
            self.load(b_mk, b_mk.h[:, :, :], biasmk[hk].rearrange("d p w -> p d w"), eng="pool")
            k.dma("pool", b_qm.h[0:16, 0, :], biasqm[hk, 0:16, :], owner=b_qm, writes=[b_qm])
            k.dma("pool", b_qm.h[:, 1, :], biasqm[hk, 16:144, :], owner=b_qm, writes=[])
            b_qm.b.w = {b_qm.b.sem: k.dtotal[b_qm.b.sem]}
            self.ts(b_d, b_d.h[:, :, :], b_d, b_d.h[:, :, :], inv, None, ALU.mult)
            self.ts(b_mk, b_mk.h[:, :, :], b_mk, b_mk.h[:, :, :], inv, None, ALU.mult)
            self.ts(b_qm, b_qm.h[0:16, 0, :], b_qm, b_qm.h[0:16, 0, :], inv, None, ALU.mult)
            self.ts(b_qm, b_qm.h[:, 1, :], b_qm, b_qm.h[:, 1, :], inv, None, ALU.mult)
            for g in range(G):
                hh = hk * G + g
                self.cp(sk_e, sk_e.h[:, g, :], sk, sk.h[:, hh:hh + 1].to_broadcast([128, 128]))
            for i in range(NB + 1):
                if i < NB:
                    nq = 128
                    W_ = GW
                    qap = q.h[:, :, i * 128:(i + 1) * 128]
                    keys = [(NB, 16, b_mk.h[0:16, 0 if i == 0 else 1, :])]
                    for dlt in (-1, 0, 1):
                        j = i + dlt
                        if 0 <= j < NB:
                            keys.append((j, 128, b_d.h[:, dlt + 1, :]))
                else:
                    nq = 16
                    W_ = G * 16
                    qap = q.h[:, :, NB * 128:NB * 128 + 16]
                    keys = [(NB, 16, b_qm.h[0:16, 0, :]), (0, 128, b_qm.h[:, 1, :])]
                po, pd = self.ps[3], self.ps[4]
                rc, ob = rcs[bi % 2], obs[bi % 2]
                bi += 1
                nk = len(keys)
                sb4 = [self.ps[0], self.ps[1], self.ps[2], self.ps[5]]
                slots = []
                for ki, (j, kk, bap) in enumerate(keys):
                    S = sb4[si % 4]
                    pt = pts[si % 4]
                    si += 1
                    slots.append((S, pt))
                    braw = b_mk if (i < NB and ki == 0) else (b_d if i < NB else b_qm)
                    self.mm(S, S.h[0:kk, 0:W_], ks.h[:, j * 128:j * 128 + kk], qap, [ks, q], True, False)
                    self.mm(S, S.h[0:kk, 0:W_], P["identb"].h[0:kk, 0:kk], bap, [P["identb"], braw], False, True)
                for ki, (j, kk, bap) in enumerate(keys):
                    S, pt = slots[ki]
                    self.act(pt, pt.h[0:kk, 0:W_], S, S.h[0:kk, 0:W_], AF.Exp, scale=scale)
                    self.mm(po, po.h[:, 0:W_], v.h[0:kk, j, :], pt.h[0:kk, 0:W_], [v, pt], ki == 0, ki == nk - 1)
                    self.mm(pd, pd.h[:, 0:W_], P["onesb"].h[0:kk, :], pt.h[0:kk, 0:W_], [P["onesb"], pt], ki == 0,
                            ki == nk - 1)
                pdv = pd.h[:, 0:W_].rearrange("p (g q) -> p g q", g=G)
                rcv = rc.h[:, 0:W_].rearrange("p (g q) -> p g q", g=G)
                pov = po.h[:, 0:W_].rearrange("p (g q) -> p g q", g=G)
                self.tt(rc, rcv, pd, pdv, sk_e, sk_e.h[:, :, 0:nq], ALU.add)
                k.op("dve", lambda e, rcv=rcv: e.reciprocal(rcv, rcv), reads=[rc], writes=[rc])
                self.tt(ob, ob.h[:, :, 0:nq], po, pov, rc, rcv, ALU.mult)
                t0 = i * 128
                dst = s["obT"][hk * G * 128:(hk + 1) * G * 128, t0:t0 + nq].rearrange("(g p) t -> p g t", p=128)
                self.store(dst, ob, ob.h[:, :, 0:nq])

    def phase_proj(self, l, Wa, Wb, Wo):
        cfg, k, P, s = self.cfg, self.k, self.P, self.s
        KC = cfg.KC

        def run(Xd, W, epi, nm, pre):
            Kx = Xd.shape[0]
            X = self.load_x(nm, Xd, Kx)
            wb = [k.sb(f"w{i}", [128, Kx // 128, 512], BF16) for i in range(2)]
            self.linear(X, Kx // 128, W, [(i * 128, 128) for i in range(KC)], epi, wb, pre=pre)

        NBUF = 4

        def alloc_common():
            return ([k.sb(f"g{i}", [128, 512], BF16) for i in range(NBUF)],
                    [k.sb(f"a{i}", [128, 512], F32) for i in range(NBUF)],
                    [k.sb(f"o{i}", [128, 512], F32) for i in range(NBUF)],
                    [k.sb(f"ob{i}", [128, 512], BF16) for i in range(NBUF)])

        pc = [0]
        ec = [0]
        gt, at, ot, obt = alloc_common()

        def preA(fi, ci, c0, n):
            i = pc[0] % NBUF
            pc[0] += 1
            self.load(gt[i], gt[i].h[:, 0:n], s["gaT"][fi * 128:(fi + 1) * 128, c0:c0 + n])

        def epiA(fi, ci, c0, n, ps):
            i = ec[0] % NBUF
            ec[0] += 1
            self.tt(ot[i], ot[i].h[:, 0:n], ps, ps.h[:, 0:n], gt[i], gt[i].h[:, 0:n], ALU.mult)
            self.store(s["tAT"][fi * 128:(fi + 1) * 128, c0:c0 + n], ot[i], ot[i].h[:, 0:n])

        run(s["oaT"], Wa, epiA, "Xa", preA)
        k.barrier()
        gt, at, ot, obt = alloc_common()
        pc[0] = ec[0] = 0

        def preB(fi, ci, c0, n):
            i = pc[0] % NBUF
            pc[0] += 1
            self.load(gt[i], gt[i].h[:, 0:n], s["gbT"][fi * 128:(fi + 1) * 128, c0:c0 + n])
            self.load(at[i], at[i].h[:, 0:n], s["tAT"][fi * 128:(fi + 1) * 128, c0:c0 + n])

        def epiB(fi, ci, c0, n, ps):
            i = ec[0] % NBUF
            ec[0] += 1
            self.tt(ot[i], ot[i].h[:, 0:n], ps, ps.h[:, 0:n], gt[i], gt[i].h[:, 0:n], ALU.mult)
            self.tt(obt[i], obt[i].h[:, 0:n], ot[i], ot[i].h[:, 0:n], at[i], at[i].h[:, 0:n], ALU.add)
            self.store(s["mgT"][fi * 128:(fi + 1) * 128, c0:c0 + n], obt[i], obt[i].h[:, 0:n])

        run(s["obT"], Wb, epiB, "Xb", preB)
        k.barrier()
        gt, at, ot, obt = alloc_common()
        pc[0] = ec[0] = 0

        def preO(fi, ci, c0, n):
            i = pc[0] % NBUF
            pc[0] += 1
            self.load(at[i], at[i].h[:, 0:n], s["hres"][fi * 128:(fi + 1) * 128, c0:c0 + n])

        def epiO(fi, ci, c0, n, ps):
            i = ec[0] % NBUF
            ec[0] += 1
            k.op("dve", lambda e, i=i, n=n, ps=ps: e.scalar_tensor_tensor(ot[i].h[:, 0:n], at[i].h[:, 0:n], cfg.ALPHA,
                                                                          ps.h[:, 0:n], ALU.mult, ALU.add),
                 reads=[at[i], ps], writes=[ot[i]])
            self.store(s["yT"][fi * 128:(fi + 1) * 128, c0:c0 + n], ot[i], ot[i].h[:, 0:n])

        run(s["mgT"], Wo, epiO, "Xm", preO)

    def phase_router(self, Wr, rbb):
        cfg, k, P, s = self.cfg, self.k, self.P, self.s
        KC, NE, T = cfg.KC, cfg.NE, cfg.T
        wr = k.sb("wr", [128, KC, NE], F32)
        self.load(wr, wr.h[:, :, :], Wr.rearrange("(kc p) e -> p kc e", p=128))
        rb = k.sb("rb", [128, NE], F32)
        self.load(rb, rb.h[:, :], rbb[:, :])
        hs = [k.sb(f"h{i}", [128, KC, 512], F32) for i in range(2)]
        lg = [k.sb(f"lg{i}", [128, NE], F32) for i in range(2)]
        mx = [k.sb(f"mx{i}", [128, 8], F32) for i in range(2)]
        ex = [k.sb(f"ex{i}", [128, NE], F32) for i in range(2)]
        mk = [k.sb(f"mk{i}", [128, NE], F32) for i in range(2)]
        dn = [k.sb(f"dn{i}", [128, 2], F32) for i in range(2)]
        gtt = [k.sb(f"gtt{i}", [NE, 128], F32) for i in range(2)]
        pmt = [k.sb(f"pmt{i}", [NE, 128], F32) for i in range(2)]
        pm = [k.sb(f"pm{i}", [128, NE], F32) for i in range(2)]
        R = k.sb("R", [128, NE], F32)
        k.op("dve", lambda e: e.memset(R.h[:, :], 0.0), writes=[R])
        obf = [k.sb(f"obf{i}", [128, cfg.D], BF16) for i in range(2)]
        ti = 0
        for ci, (c0, n) in enumerate(cfg.chunks):
            h = hs[ci % 2]
            self.load(h, h.h[:, :, 0:n], self.fm(s["hres"])[:, :, c0:c0 + n])
            for a in range((n + 127) // 128):
                rows = min(128, n - a * 128)
                t0 = c0 + a * 128
                i = ti % 2
                ti += 1
                ps = self.ps[i]
                for kc in range(KC):
                    self.mm(ps, ps.h[0:rows, 0:NE], h.h[:, kc, a * 128:a * 128 + rows], wr.h[:, kc, :], [h, wr],
                            kc == 0, kc == KC - 1)
                L_, M_, E_, K_, D_ = lg[i], mx[i], ex[i], mk[i], dn[i]
                self.tt(L_, L_.h[0:rows, :], ps, ps.h[0:rows, 0:NE], rb, rb.h[0:rows, :], ALU.add)
                k.op("dve", lambda e, M_=M_, L_=L_, rows=rows: e.max(M_.h[0:rows, :], L_.h[0:rows, :]), reads=[L_],
                     writes=[M_])
                self.ts(K_, K_.h[0:rows, :], L_, L_.h[0:rows, :], M_.h[0:rows, 1:2], None, ALU.is_ge, extra_reads=[M_])
                self.ts(D_, D_.h[0:rows, 0:1], M_, M_.h[0:rows, 0:1], -1.0, None, ALU.mult)
                self.act(E_, E_.h[0:rows, :], L_, L_.h[0:rows, :], AF.Exp, extra_reads=[D_], bias=D_.h[0:rows, 0:1])
                self.tt(E_, E_.h[0:rows, :], E_, E_.h[0:rows, :], K_, K_.h[0:rows, :], ALU.mult)
                k.op("dve", lambda e, D_=D_, E_=E_, rows=rows: e.tensor_reduce(D_.h[0:rows, 1:2], E_.h[0:rows, :],
                                                                              AX.X, ALU.add),
                     reads=[E_], writes=[D_])
                k.op("dve", lambda e, D_=D_, rows=rows: e.reciprocal(D_.h[0:rows, 1:2], D_.h[0:rows, 1:2]), reads=[D_],
                     writes=[D_])
                self.ts(E_, E_.h[0:rows, :], E_, E_.h[0:rows, :], D_.h[0:rows, 1:2], None, ALU.mult, extra_reads=[D_])
                pt = self.ps[2 + i]
                self.mm(pt, pt.h[0:NE, 0:rows], E_.h[0:rows, :], P["ident"].h[0:rows, 0:rows], [E_, P["ident"]], True,
                        True)
                G_ = gtt[i]
                self.cp(G_, G_.h[:, 0:rows], pt, pt.h[0:NE, 0:rows], eng="act")
                self.store(s["gtT"][:, t0:t0 + rows], G_, G_.h[:, 0:rows])
                pp = self.ps[4 + i]
                self.mm(pp, pp.h[0:rows, 0:NE], P["upper"].h[0:rows, 0:rows], K_.h[0:rows, :], [P["upper"], K_], True,
                        ti == 1)
                if ti > 1:
                    self.mm(pp, pp.h[0:rows, 0:NE], P["ones"].h[:, 0:rows], R.h[:, :], [P["ones"], R], False, True)
                PM = pm[i]
                k.op("dve", lambda e, PM=PM, pp=pp, K_=K_, rows=rows: e.scalar_tensor_tensor(
                    PM.h[0:rows, :], pp.h[0:rows, 0:NE], 1.0, K_.h[0:rows, :], ALU.add, ALU.mult),
                    reads=[pp, K_], writes=[PM])
                self.ts(PM, PM.h[0:rows, :], PM, PM.h[0:rows, :], -1.0, None, ALU.add)
                self.tt(R, R.h[0:rows, :], R, R.h[0:rows, :], K_, K_.h[0:rows, :], ALU.add)
                self.store(s["pmtm"][t0:t0 + rows, :], PM, PM.h[0:rows, :])
                pt2 = self.ps[6 + i]
                self.mm(pt2, pt2.h[0:NE, 0:rows], PM.h[0:rows, :], P["ident"].h[0:rows, 0:rows], [PM, P["ident"]],
                        True, True)
                PT = pmt[i]
                self.cp(PT, PT.h[:, 0:rows], pt2, pt2.h[0:NE, 0:rows], eng="act")
                self.store(s["pmT"][:, t0:t0 + rows], PT, PT.h[:, 0:rows])
                ob = obf[i]
                for g in range(0, KC, 4):
                    ph = self.ps[2 + (g // 4) % 2]
                    gn = min(4, KC - g)
                    for j in range(gn):
                        self.mm(ph, ph.h[0:rows, j * 128:(j + 1) * 128], h.h[:, g + j, a * 128:a * 128 + rows],
                                P["ident"].h[:, :], [h, P["ident"]], True, True)
                    self.cp(ob, ob.h[0:rows, g * 128:(g + gn) * 128], ph, ph.h[0:rows, 0:gn * 128],
                            eng=("act" if (g // 4) % 2 else "dve"))
                self.store(s["htm"][t0:t0 + rows, :], ob, ob.h[0:rows, :])

    def slot_chunks(self, s0, s1):
        out = []
        c = s0
        while c < s1:
            n = min(512, s1 - c)
            out.append((c, n))
            c += n
        return out

    def phase_gather(self):
        cfg, k, P, s = self.cfg, self.k, self.P, self.s
        KC, NE, T, CAP = cfg.KC, cfg.NE, cfg.T, cfg.CAP
        nfull = T // 128
        rem = T - nfull * 128
        ntile = nfull + (1 if rem else 0)
        iota = k.sb("iota", [128, CAP], F32)
        self.load(iota, iota.h[:, :], self.iotac[:, :])
        pma = k.sb("pma", [128, ntile, NE], F32)
        k.dma("sp", pma.h[:, 0:nfull, :], s["pmtm"][0:nfull * 128, :].rearrange("(a p) e -> p a e", p=128), owner=pma,
              writes=[pma])
        if rem:
            k.dma("sp", pma.h[0:rem, nfull, :], s["pmtm"][nfull * 128:T, :], owner=pma, writes=[])
            pma.b.w = {pma.b.sem: k.dtotal[pma.b.sem]}
        sel = k.sb("sel", [128, ntile, CAP], BF16)
        hts = [k.sb(f"htk{i}", [128, ntile, 256], BF16) for i in range(2)]
        st = [k.sb(f"st{i}", [128, 512], BF16) for i in range(4)]
        sti = 0
        hi = 0
        for e in range(NE):
            for i in range(ntile):
                rows = 128 if i < nfull else rem
                self.ts(sel, sel.h[0:rows, i, :], iota, iota.h[0:rows, :], pma.h[0:rows, i, e:e + 1], None, ALU.is_equal,
                        extra_reads=[pma])
            for kc0 in range(0, KC, 2):
                kn_ = min(2, KC - kc0)
                ht = hts[hi % 2]
                hi += 1
                k.dma("sp", ht.h[:, 0:nfull, 0:kn_ * 128],
                      s["htm"][0:nfull * 128, kc0 * 128:(kc0 + kn_) * 128].rearrange("(a p) d -> p a d", p=128),
                      owner=ht, writes=[ht])
                if rem:
                    k.dma("sp", ht.h[0:rem, nfull, 0:kn_ * 128], s["htm"][nfull * 128:T, kc0 * 128:(kc0 + kn_) * 128],
                          owner=ht, writes=[])
                    ht.b.w = {ht.b.sem: k.dtotal[ht.b.sem]}
                for kk_ in range(kn_):
                    kc = kc0 + kk_
                    for (c0, n) in self.slot_chunks(0, CAP):
                        ps = self.ps[sti % 4]
                        for i in range(ntile):
                            rows = 128 if i < nfull else rem
                            self.mm(ps, ps.h[:, 0:n], ht.h[0:rows, i, kk_ * 128:(kk_ + 1) * 128],
                                    sel.h[0:rows, i, c0:c0 + n], [ht, sel], i == 0, i == ntile - 1)
                        t = st[sti % 4]
                        self.cp(t, t.h[:, 0:n], ps, ps.h[:, 0:n], eng=("act" if sti % 2 else "dve"))
                        sti += 1
                        self.store(s["XeT"][e * cfg.D + kc * 128:e * cfg.D + (kc + 1) * 128, c0:c0 + n], t,
                                   t.h[:, 0:n])

    def phase_moe_ffn(self, experts):
        cfg, k, P, s = self.cfg, self.k, self.P, self.s
        KC, DFF, CAP, D = cfg.KC, cfg.DFF, cfg.CAP, cfg.D
        FK = DFF // 128
        HALF = CAP // 2
        nst = HALF // 128
        X = k.sb("Xe", [128, KC, HALF], BF16)
        G_ = k.sb("G", [128, FK, HALF], BF16)
        w1b = [k.sb(f"w1_{i}", [128, KC, 256], BF16) for i in range(2)]
        w3b = [k.sb(f"w3_{i}", [128, KC, 256], BF16) for i in range(2)]
        w2b = [k.sb(f"w2_{i}", [128, FK, 256], BF16) for i in range(2)]
        Yb = k.sb("Yb", [128, nst, D], BF16)
        sa = [k.sb(f"sa{i}", [128, 512], F32) for i in range(2)]
        ctr = 0
        wctr = 0
        for ei, (W1, W3, W2) in enumerate(experts):
            W1v = W1.rearrange("(kc p) n -> p kc n", p=128)
            W3v = W3.rearrange("(kc p) n -> p kc n", p=128)
            W2v = W2.rearrange("(kc p) n -> p kc n", p=128)
            for s0 in range(0, CAP, HALF):
                chs = self.slot_chunks(0, HALF)
                self.load(X, X.h[:, :, :], self.fm(s["XeT"][ei * D:(ei + 1) * D, :])[:, :, s0:s0 + HALF])
                for j0 in range(0, FK, 2):
                    jn = min(2, FK - j0)
                    w1, w3 = w1b[wctr % 2], w3b[wctr % 2]
                    wctr += 1
                    self.load(w1, w1.h[:, :, 0:jn * 128], W1v[:, :, j0 * 128:(j0 + jn) * 128], eng="pool")
                    self.load(w3, w3.h[:, :, 0:jn * 128], W3v[:, :, j0 * 128:(j0 + jn) * 128], eng="pool")
                    for jj in range(jn):
                        j = j0 + jj
                        for (c0, n) in chs:
                            i = ctr % 2
                            ctr += 1
                            pa, pb = self.ps[2 * i], self.ps[2 * i + 1]
                            for kc in range(KC):
                                self.mm(pa, pa.h[:, 0:n], w1.h[:, kc, jj * 128:(jj + 1) * 128], X.h[:, kc, c0:c0 + n],
                                        [w1, X], kc == 0, kc == KC - 1)
                            for kc in range(KC):
                                self.mm(pb, pb.h[:, 0:n], w3.h[:, kc, jj * 128:(jj + 1) * 128], X.h[:, kc, c0:c0 + n],
                                        [w3, X], kc == 0, kc == KC - 1)
                            self.act(sa[i], sa[i].h[:, 0:n], pa, pa.h[:, 0:n], AF.Silu)
                            self.tt(G_, G_.h[:, j, c0:c0 + n], sa[i], sa[i].h[:, 0:n], pb, pb.h[:, 0:n], ALU.mult)
                for cg in range(0, D, 256):
                    cn = min(256, D - cg)
                    w2 = w2b[wctr % 2]
                    wctr += 1
                    self.load(w2, w2.h[:, :, 0:cn], W2v[:, :, cg:cg + cn], eng="pool")
                    for st_ in range(nst):
                        ps = self.ps[4 + ctr % 4]
                        ctr += 1
                        for j in range(FK):
                            self.mm(ps, ps.h[:, 0:cn], G_.h[:, j, st_ * 128:(st_ + 1) * 128], w2.h[:, j, 0:cn], [G_, w2],
                                    j == 0, j == FK - 1)
                        self.cp(Yb, Yb.h[:, st_, cg:cg + cn], ps, ps.h[:, 0:cn], eng=("act" if ctr % 2 else "dve"))
                r0 = ei * CAP + s0
                self.store(s["Yd"][r0:r0 + HALF, :].rearrange("(a p) d -> p a d", p=128), Yb, Yb.h[:, :, :])

    def phase_combine(self):
        cfg, k, P, s = self.cfg, self.k, self.P, self.s
        KC, NE, CAP, D = cfg.KC, cfg.NE, cfg.CAP, cfg.D
        NST = CAP // 128
        sid = k.sb("sid", [128, NST], F32)
        self.load(sid, sid.h[:, :], self.slotid[:, :])
        selT = k.sb("selT", [128, NE * NST, 512], BF16)
        pmb = [k.sb(f"pmb{i}", [128, 512], F32) for i in range(2)]
        gtb = [k.sb(f"gtb{i}", [128, 512], F32) for i in range(2)]
        tmpq = [k.sb(f"tmpq{i}", [128, 512], F32) for i in range(2)]
        FH = min(8, KC)
        yts = [k.sb(f"yt{i}", [128, FH * 128], BF16) for i in range(4)]
        hr = [k.sb(f"hr{i}", [128, 512], F32) for i in range(2)]
        ot = [k.sb(f"ot{i}", [128, 512], F32) for i in range(2)]
        yi = 0
        oi = 0
        for (c0, n) in cfg.chunks:
            for e in range(NE):
                pb_, gb_ = pmb[e % 2], gtb[e % 2]
                self.load(pb_, pb_.h[:, 0:n], s["pmT"][e, c0:c0 + n].partition_broadcast(128))
                self.load(gb_, gb_.h[:, 0:n], s["gtT"][e, c0:c0 + n].partition_broadcast(128))
                for st_ in range(NST):
                    k.op("dve", lambda en, e=e, st_=st_, n=n, pb_=pb_, gb_=gb_: en.scalar_tensor_tensor(
                        selT.h[:, e * NST + st_, 0:n], pb_.h[:, 0:n], sid.h[:, st_:st_ + 1], gb_.h[:, 0:n],
                        ALU.is_equal, ALU.mult), reads=[pb_, gb_, sid], writes=[selT])
            for f0 in range(0, KC, FH):
                fn_ = min(FH, KC - f0)
                tot = NE * NST
                for q in range(tot):
                    e, st_ = divmod(q, NST)
                    yt = yts[yi % 4]
                    yi += 1
                    r0 = e * CAP + st_ * 128
                    self.load(yt, yt.h[:, 0:fn_ * 128], s["Yd"][r0:r0 + 128, f0 * 128:(f0 + fn_) * 128])
                    for f in range(fn_):
                        ps = self.ps[f]
                        self.mm(ps, ps.h[:, 0:n], yt.h[:, f * 128:(f + 1) * 128], selT.h[:, q, 0:n], [yt, selT], q == 0,
                                q == tot - 1, inc=(q == tot - 1 or f == fn_ - 1))
                for f in range(fn_):
                    ps = self.ps[f]
                    hrt, o_ = hr[oi % 2], ot[oi % 2]
                    oi += 1
                    fr = (f0 + f) * 128
                    self.load(hrt, hrt.h[:, 0:n], s["hres"][fr:fr + 128, c0:c0 + n])
                    k.op("dve", lambda en, o_=o_, hrt=hrt, ps=ps, n=n: en.scalar_tensor_tensor(
                        o_.h[:, 0:n], hrt.h[:, 0:n], cfg.ALPHA, ps.h[:, 0:n], ALU.mult, ALU.add), reads=[hrt, ps],
                        writes=[o_])
                    self.store(s["yT"][fr:fr + 128, c0:c0 + n], o_, o_.h[:, 0:n])

    def phase_ffn(self, experts, gated):
        cfg, k, P, s = self.cfg, self.k, self.P, self.s
        KC, DFF, T = cfg.KC, cfg.DFF, cfg.T
        FK = DFF // 128
        PART = cfg.FFPART
        supers = []
        cur = []
        for (c0, n) in cfg.chunks:
            if cur and sum(x[1] for x in cur) + n > cfg.SUPER + 16:
                supers.append(cur)
                cur = []
            cur.append((c0, n))
        if cur:
            supers.append(cur)
        SW = max(sum(x[1] for x in sc) for sc in supers)
        hb = k.sb("hbS", [128, KC, SW], BF16)
        G_ = k.sb("G", [128, PART, SW], BF16)
        acc = k.sb("acc", [128, KC, SW], F32)
        w1b = [k.sb(f"w1_{i}", [128, KC, 256], BF16) for i in range(2)]
        w3b = [k.sb(f"w3_{i}", [128, KC, 256], BF16) for i in range(2)]
        w2b = [k.sb(f"w2_{i}", [128, PART, 256], BF16) for i in range(2)]
        sa = [k.sb(f"sa{i}", [128, 512], F32) for i in range(2)]
        gbs = [k.sb(f"gb{i}", [128, 512], F32) for i in range(2)]
        hr = [k.sb(f"hr{i}", [128, 512], F32) for i in range(2)]
        ctr = [0]
        wctr = [0]
        for sc in supers:
            s0 = sc[0][0]
            sw = sum(x[1] for x in sc)
            self.load(hb, hb.h[:, :, 0:sw], self.fm(s["hb"])[:, :, s0:s0 + sw])
            first_acc = True
            for ei, (W1, W3, W2) in enumerate(experts):
                W1v = W1.rearrange("(kc p) n -> p kc n", p=128)
                W3v = W3.rearrange("(kc p) n -> p kc n", p=128)
                W2v = W2.rearrange("(kc p) n -> p kc n", p=128)
                for p0 in range(0, FK, PART):
                    pn = min(PART, FK - p0)
                    for j0 in range(0, pn, 2):
                        jn = min(2, pn - j0)
                        wi = wctr[0] % 2
                        wctr[0] += 1
                        w1, w3 = w1b[wi], w3b[wi]
                        col = (p0 + j0) * 128
                        self.load(w1, w1.h[:, :, 0:jn * 128], W1v[:, :, col:col + jn * 128], eng="pool")
                        self.load(w3, w3.h[:, :, 0:jn * 128], W3v[:, :, col:col + jn * 128], eng="pool")
                        for jj in range(jn):
                            j = j0 + jj
                            for (c0, n) in sc:
                                lo = c0 - s0
                                i = ctr[0] % 2
                                ctr[0] += 1
                                pa, pb = self.ps[2 * i], self.ps[2 * i + 1]
                                for kc in range(KC):
                                    self.mm(pa, pa.h[:, 0:n], w1.h[:, kc, jj * 128:(jj + 1) * 128],
                                            hb.h[:, kc, lo:lo + n], [w1, hb], kc == 0, kc == KC - 1)
                                for kc in range(KC):
                                    self.mm(pb, pb.h[:, 0:n], w3.h[:, kc, jj * 128:(jj + 1) * 128],
                                            hb.h[:, kc, lo:lo + n], [w3, hb], kc == 0, kc == KC - 1)
                                self.act(sa[i], sa[i].h[:, 0:n], pa, pa.h[:, 0:n], AF.Silu)
                                if gated:
                                    gbt = gbs[i]
                                    self.load(gbt, gbt.h[:, 0:n], s["gtT"][ei, c0:c0 + n].partition_broadcast(128))
                                    self.tt(sa[i], sa[i].h[:, 0:n], sa[i], sa[i].h[:, 0:n], gbt, gbt.h[:, 0:n],
                                            ALU.mult)
                                self.tt(G_, G_.h[:, j, lo:lo + n], sa[i], sa[i].h[:, 0:n], pb, pb.h[:, 0:n], ALU.mult)
                    for f0 in range(0, KC, 2):
                        fn_ = min(2, KC - f0)
                        wi = wctr[0] % 2
                        wctr[0] += 1
                        w2 = w2b[wi]
                        self.load(w2, w2.h[:, 0:pn, 0:fn_ * 128], W2v[:, p0:p0 + pn, f0 * 128:(f0 + fn_) * 128],
                                  eng="pool")
                        for ff in range(fn_):
                            f = f0 + ff
                            for (c0, n) in sc:
                                lo = c0 - s0
                                ps = self.ps[4 + ctr[0] % 4]
                                ctr[0] += 1
                                for j in range(pn):
                                    self.mm(ps, ps.h[:, 0:n], w2.h[:, j, ff * 128:(ff + 1) * 128], G_.h[:, j, lo:lo + n],
                                            [w2, G_], j == 0, j == pn - 1)
                                if first_acc and p0 == 0:
                                    hi = ctr[0] % 2
                                    hrt = hr[hi]
                                    self.load(hrt, hrt.h[:, 0:n], s["hres"][f * 128:(f + 1) * 128, c0:c0 + n])
                                    k.op("dve", lambda e, f=f, lo=lo, n=n, ps=ps, hrt=hrt: e.scalar_tensor_tensor(
                                        acc.h[:, f, lo:lo + n], hrt.h[:, 0:n], cfg.ALPHA, ps.h[:, 0:n], ALU.mult,
                                        ALU.add), reads=[hrt, ps], writes=[acc])
                                else:
                                    self.tt(acc, acc.h[:, f, lo:lo + n], acc, acc.h[:, f, lo:lo + n], ps, ps.h[:, 0:n],
                                            ALU.add)
                first_acc = False
            self.store(self.fm(s["yT"])[:, :, s0:s0 + sw], acc, acc.h[:, :, 0:sw])


_BUCKET_STARTS = [8, 12, 16, 23, 32, 46, 64, 91]


def _bucket(rel):
    rel = np.asarray(rel)
    n = np.abs(rel)
    b = np.where(n < 8, n, 8 + sum((n >= t).astype(np.int64) for t in _BUCKET_STARTS[1:]))
    return np.where(rel > 0, 16, 0) + b


def _host_tables(cfg, rel_bias):
    T, G = cfg.T, cfg.GRP
    pos = np.concatenate([cfg.NMETA + np.arange(cfg.SEQ), np.arange(cfg.NMETA)]).astype(np.float32)
    inv = (np.float32(10000.0) ** (-np.arange(0, cfg.ROPE, 2, dtype=np.float32) / np.float32(cfg.ROPE))).astype(np.float32)
    ang = pos[None, :] * inv[:, None]
    cos = np.cos(ang).astype(np.float32)
    sin = np.sin(ang).astype(np.float32)
    ropet = np.stack([np.concatenate([cos, cos], 0), np.concatenate([-sin, sin], 0)]).astype(np.float32)
    rb = np.asarray(rel_bias, np.float32)

    def tab(rel, masked, heads):
        bk = _bucket(rel)
        outs = []
        for h in heads:
            t = rb[bk, h]
            t = np.where(masked, np.float32(NEGV), t)
            outs.append(t)
        return np.concatenate(outs, axis=1).astype(np.float32)

    kk = np.arange(128)[:, None]
    qq = np.arange(128)[None, :]
    biasd = np.zeros((cfg.SKV, 3, 128, G * 128), np.float32)
    biasmk = np.zeros((cfg.SKV, 2, 16, G * 128), np.float32)
    biasqm = np.zeros((cfg.SKV, 144, G * 16), np.float32)
    mj = np.arange(16)[:, None]
    for hk in range(cfg.SKV):
        heads = [hk * G + g for g in range(G)]
        for d in (-1, 0, 1):
            rel = d * 128 + kk - qq
            biasd[hk, d + 1] = tab(rel, np.abs(rel) > 128, heads)
        rel0 = mj - (16 + qq)
        biasmk[hk, 0] = tab(rel0, np.zeros_like(rel0, bool), heads)
        rel1 = mj - (16 + 128 + qq)
        biasmk[hk, 1] = tab(rel1, np.zeros_like(rel1, bool), heads)
        q16 = np.arange(16)[None, :]
        relm = mj - q16
        biasqm[hk, 0:16] = tab(relm, np.zeros_like(relm, bool), heads)
        relb = (16 + kk) - q16
        biasqm[hk, 16:144] = tab(relb, relb > 128, heads)
    return ropet, biasd, biasmk, biasqm


def _fmcol(v):
    v = np.asarray(v, np.float32)
    return np.ascontiguousarray(v.reshape(-1, 128).T)


def prepare_inputs(cfg, inp):
    L = cfg.DEPTH
    ropet, biasd, biasmk, biasqm = _host_tables(cfg, inp["rel_bias"])
    consts = np.zeros((128, 384), np.float32)
    consts[:, 0:128] = np.eye(128, dtype=np.float32)
    consts[:, 128:256] = 1.0
    consts[:, 256:384] = np.triu(np.ones((128, 128), np.float32), 1)
    iotac = np.ascontiguousarray(np.broadcast_to(np.arange(cfg.CAP, dtype=np.float32)[None, :], (128, cfg.CAP)))
    slotid = (np.arange(cfg.CAP // 128, dtype=np.float32)[None, :] * 128 + np.arange(128, dtype=np.float32)[:, None])
    slotid = np.ascontiguousarray(slotid.astype(np.float32))
    cols = [_fmcol(inp["emb_ln_g"]), _fmcol(inp["emb_ln_b"])]
    for l in range(L):
        cols += [_fmcol(inp["ln_mix_g"][l]), _fmcol(inp["ln_mix_b"][l]), _fmcol(inp["ln_ffn_g"][l]),
                 _fmcol(inp["ln_ffn_b"][l]), _fmcol(inp["q_norm_g"][l]), _fmcol(inp["kv_norm_g"][l])]
    pvec = np.ascontiguousarray(np.concatenate(cols, axis=1))
    w_in = np.asarray(inp["w_in"], np.float32)
    ro = cfg.QR + cfg.KVR
    half = cfg.ROPE // 2
    rot = np.concatenate([w_in[:, :, ro + half:ro + cfg.ROPE], w_in[:, :, ro:ro + half]], axis=2)
    w_in_e = np.ascontiguousarray(np.concatenate([w_in, rot], axis=2))
    w_uq = np.asarray(inp["w_uq"], np.float32).reshape(L, cfg.QR, cfg.MH, cfg.NOPE + cfg.ROPE)
    qn = w_uq[..., :cfg.NOPE].reshape(L, cfg.QR, -1)
    qr = w_uq[..., cfg.NOPE:]
    qrot = np.concatenate([qr[..., half:], qr[..., :half]], axis=-1)
    qrr = np.concatenate([qr, qrot], axis=-1).reshape(L, cfg.QR, -1)
    w_uq_e = np.ascontiguousarray(np.concatenate([qn, qrr], axis=2))
    w_ukv = np.asarray(inp["w_ukv"], np.float32).reshape(L, cfg.KVR, cfg.MH, cfg.NOPE + cfg.VD)
    w_ukv_e = np.ascontiguousarray(np.concatenate([w_ukv[..., :cfg.NOPE].reshape(L, cfg.KVR, -1),
                                                   w_ukv[..., cfg.NOPE:].reshape(L, cfg.KVR, -1)], axis=2))
    sinkb = np.ascontiguousarray(np.broadcast_to(np.asarray(inp["sink_logits"], np.float32)[:, None, :],
                                                 (L, 128, cfg.SH)))
    shared = dict(meta=np.asarray(inp["meta_tokens"], np.float32), consts=consts, iotac=iotac, slotid=slotid,
                  pvec=pvec, ropet=ropet,
                  biasd=biasd, biasmk=biasmk, biasqm=biasqm, sinkb=sinkb, w_in=w_in_e, w_uq=w_uq_e, w_ukv=w_ukv_e,
                  w_pa=np.asarray(inp["w_proj_a"], np.float32), w_pb=np.asarray(inp["w_proj_b"], np.float32),
                  w_out=np.asarray(inp["w_out"], np.float32), ffn_w1=np.asarray(inp["ffn_w1"], np.float32),
                  ffn_w3=np.asarray(inp["ffn_w3"], np.float32), ffn_w2=np.asarray(inp["ffn_w2"], np.float32))
    if L // 2:
        nm = L // 2
        shared["router_w"] = np.asarray(inp["router_w"], np.float32)
        shared["router_bb"] = np.ascontiguousarray(np.broadcast_to(np.asarray(inp["router_b"], np.float32)[:, None, :],
                                                                   (nm, 128, cfg.NE)))
        shared["moe_w1"] = np.asarray(inp["moe_w1"], np.float32).reshape(nm * cfg.NE, cfg.D, cfg.DFF)
        shared["moe_w3"] = np.asarray(inp["moe_w3"], np.float32).reshape(nm * cfg.NE, cfg.D, cfg.DFF)
        shared["moe_w2"] = np.asarray(inp["moe_w2"], np.float32).reshape(nm * cfg.NE, cfg.DFF, cfg.D)
    return shared


def run(cfg, inp, debug=None, stop_after=None, trace=False):
    prog = Prog(cfg, debug=debug, stop_after=stop_after)
    nc = prog.build()
    shared = prepare_inputs(cfg, inp)
    x = np.asarray(inp["x"], np.float32)
    in_maps = []
    for c in range(cfg.NCORES):
        m = dict(shared)
        m["x"] = np.ascontiguousarray(x[c])
        in_maps.append(m)
    res = run_bass_kernel_spmd(nc, in_maps, core_ids=list(range(cfg.NCORES)), trace=trace)
    return res, prog


def kernel(**inputs):
    cfg = Cfg()
    res, _ = run(cfg, inputs)
    return np.stack([np.asarray(r["out"], np.float32) for r in res.results], axis=0)
```

```python
import math
import os
from contextlib import ExitStack
import numpy as np
import concourse.bass as bass
import concourse.mybir as mybir
from concourse.bass_utils import run_bass_kernel_spmd

F32 = mybir.dt.float32
BF16 = mybir.dt.bfloat16
ALU = mybir.AluOpType
AF = mybir.ActivationFunctionType
AX = mybir.AxisListType

NEGV = -1.0e30


class Cfg:
    def __init__(self, **kw):
        self.D = 2048
        self.SEQ = 4096
        self.DEPTH = 2
        self.NMETA = 16
        self.MH = 16
        self.NOPE = 128
        self.ROPE = 64
        self.VD = 128
        self.QR = 768
        self.KVR = 512
        self.SH = 16
        self.SKV = 4
        self.HD = 128
        self.DFF = 5632
        self.NE = 8
        self.NCORES = 8
        self.SUPER = 1024
        self.FFPART = 11
        self.CAP = 1536
        for k, v in kw.items():
            setattr(self, k, v)
        self.T = self.SEQ + self.NMETA
        self.NB = self.SEQ // 128
        self.KC = self.D // 128
        self.GRP = self.SH // self.SKV
        self.chunks = [(c0, 512) for c0 in range(0, self.SEQ, 512)] + [(self.SEQ, self.NMETA)]
        self.IN_DIM = self.QR + self.KVR + self.ROPE + self.SH * self.HD + 2 * self.SKV * self.HD + 2 * self.D
        self.LN_EPS = 1e-5
        self.RMS_EPS = 1e-6
        self.ALPHA = (2 * self.DEPTH) ** 0.25


class Buf:
    __slots__ = ("name", "w", "r", "sem", "cnt")

    def __init__(self, name):
        self.name = name
        self.w = {}
        self.r = {}
        self.sem = None
        self.cnt = 0


class Tl:
    __slots__ = ("b", "h")

    def __init__(self, b, h):
        self.b = b
        self.h = h


ENGS = ("pe", "act", "dve", "pool", "sp")


class KB:
    def __init__(self, nc, stack, sb_limit):
        self.nc = nc
        self.stack = stack
        self.q = {e: [] for e in ENGS}
        self.cnt = {e: 0 for e in ENGS}
        self.clock = {e: {} for e in ENGS}
        self.semh = {}
        self.esem = {}
        for e in ENGS:
            self.esem[e] = self.newsem("E_" + e)
        self.dfree = []
        self.dtotal = {}
        self.dall = []
        self.phase_bufs = []
        self.sb_base = 16512
        self.sb_off = 16512
        self.sb_limit = sb_limit
        self.uid = 0
        self.ninst = 0

    def newsem(self, name):
        h = self.stack.enter_context(self.nc.semaphore(name))
        self.semh[name] = h
        return name

    def sb(self, name, shape, dtype, persistent=False):
        esz = 4 if dtype == F32 else 2
        n = 1
        for s in shape[1:]:
            n *= s
        nbytes = (n * esz + 63) // 64 * 64
        self.uid += 1
        off = self.sb_off
        assert off + nbytes <= self.sb_limit, f"SBUF overflow allocating {name}: {off}+{nbytes}"
        h = self.nc.alloc_sbuf_tensor_at(f"{name}_{self.uid}", list(shape), dtype, offset=off)
        self.sb_off = off + nbytes
        if persistent:
            assert self.sb_base == off, "persistent allocs must come first"
            self.sb_base = self.sb_off
        b = Buf(name)
        if not persistent:
            self.phase_bufs.append(b)
        return Tl(b, h)

    def _deps(self, eng, reads, writes):
        mysem = self.esem[eng]
        ck = self.clock[eng]
        waits = {}

        def need(sem, val):
            if ck.get(sem, 0) >= val:
                return
            ck[sem] = val
            waits[sem] = val

        for t in reads:
            for sem, val in t.b.w.items():
                if sem == mysem and eng == "pe":
                    continue
                need(sem, val)
        for t in writes:
            for sem, val in t.b.w.items():
                if sem == mysem:
                    continue
                need(sem, val)
            for sem, val in t.b.r.items():
                if sem == mysem:
                    continue
                need(sem, val)
        return list(waits.items())

    def _mark(self, ev, reads, writes):
        sem, val = ev
        for t in reads:
            if t.b.r.get(sem, 0) < val:
                t.b.r[sem] = val
        for t in writes:
            t.b.w = {sem: val}
            t.b.r = {}

    def op(self, eng, fn, reads=(), writes=(), inc=True):
        waits = self._deps(eng, reads, writes)
        idx = self.cnt[eng] + 1
        if inc:
            self.cnt[eng] = idx
        ev = (self.esem[eng], idx)
        self.q[eng].append((waits, fn, (self.esem[eng], 1) if inc else None))
        self._mark(ev, reads, writes)
        self.ninst += 1
        return ev

    def dma(self, eng, out, in_, owner, reads=(), writes=(), **kw):
        b = owner.b
        if b.sem is None:
            if self.dfree:
                b.sem = self.dfree.pop()
            else:
                b.sem = self.newsem(f"D{len(self.dall)}")
                self.dall.append(b.sem)
                self.dtotal[b.sem] = 0
        waits = self._deps(eng, reads, writes)
        self.dtotal[b.sem] += 16
        ev = (b.sem, self.dtotal[b.sem])
        self.q[eng].append((waits, (lambda e: e.dma_start(out=out, in_=in_, **kw)), (b.sem, 16)))
        self._mark(ev, reads, writes)
        self.ninst += 1
        return ev

    def barrier(self):
        for e in ENGS:
            waits = []
            ck = self.clock[e]
            for e2 in ENGS:
                if e2 == e or self.cnt[e2] == 0:
                    continue
                s = self.esem[e2]
                if ck.get(s, 0) < self.cnt[e2]:
                    ck[s] = self.cnt[e2]
                    waits.append((s, self.cnt[e2]))
            for s in self.dall:
                v = self.dtotal[s]
                if v > 0 and ck.get(s, 0) < v:
                    ck[s] = v
                    waits.append((s, v))
            if waits:
                self.q[e].append((waits, None, None))
        for b in self.phase_bufs:
            if b.sem is not None:
                self.dfree.append(b.sem)
                b.sem = None
        self.phase_bufs = []
        self.sb_off = self.sb_base

    def emit(self):
        nc = self.nc
        engmap = {"pe": "tensor", "act": "scalar", "dve": "vector", "pool": "gpsimd", "sp": "sync"}
        with nc.Block() as block:
            for e in ENGS:
                q = self.q[e]
                semh = self.semh

                def body(eh, q=q):
                    for waits, fn, inc in q:
                        for s, v in waits:
                            eh.wait_ge(semh[s], v)
                        if fn is not None:
                            ins = fn(eh)
                            if inc is not None:
                                ins.then_inc(semh[inc[0]], inc[1])

                getattr(block, engmap[e])(body)


class Prog:
    def __init__(self, cfg, debug=None, stop_after=None):
        self.cfg = cfg
        self.debug = debug or []
        self.stop_after = stop_after
        self.nc = bass.Bass("TRN2", target_bir_lowering=False)
        self.stack = ExitStack()
        self.k = KB(self.nc, self.stack, sb_limit=229376 - 256)
        self.dr = {}

    def din(self, name, shape, dtype=F32):
        t = self.nc.dram_tensor(name, list(shape), dtype, kind="ExternalInput").ap()
        self.dr[name] = t
        return t

    def dscr(self, name, shape, dtype):
        kind = "ExternalOutput" if name in self.debug else "Internal"
        t = self.nc.dram_tensor(name, list(shape), dtype, kind=kind).ap()
        self.dr[name] = t
        return t

    def fm(self, ap):
        return ap.rearrange("(kc p) t -> p kc t", p=128)

    def mm(self, ps, ps_ap, lhsT_ap, rhs_ap, reads, start, stop, inc=None):
        self.k.op("pe", lambda e: e.matmul(ps_ap, lhsT_ap, rhs_ap, start=start, stop=stop),
                  reads=reads, writes=[ps], inc=(stop if inc is None else inc))

    def act(self, out_t, out_ap, in_t, in_ap, func, extra_reads=(), **kw):
        self.k.op("act", lambda e: e.activation(out_ap, in_ap, func, **kw),
                  reads=[in_t, *extra_reads], writes=[out_t])

    def tt(self, out_t, out_ap, a_t, a_ap, b_t, b_ap, op, eng="dve"):
        self.k.op(eng, lambda e: e.tensor_tensor(out_ap, a_ap, b_ap, op), reads=[a_t, b_t], writes=[out_t])

    def ts(self, out_t, out_ap, a_t, a_ap, s1, s2, op0, op1=None, extra_reads=(), eng="dve"):
        if op1 is None:
            self.k.op(eng, lambda e: e.tensor_scalar(out_ap, a_ap, s1, None, op0),
                      reads=[a_t, *extra_reads], writes=[out_t])
        else:
            self.k.op(eng, lambda e: e.tensor_scalar(out_ap, a_ap, s1, s2, op0, op1),
                      reads=[a_t, *extra_reads], writes=[out_t])

    def cp(self, out_t, out_ap, in_t, in_ap, eng="dve"):
        if eng == "act":
            self.k.op("act", lambda e: e.copy(out_ap, in_ap), reads=[in_t], writes=[out_t])
        else:
            self.k.op(eng, lambda e: e.tensor_copy(out_ap, in_ap), reads=[in_t], writes=[out_t])

    def load(self, dst_t, dst_ap, src_ap, eng="sp"):
        self.k.dma(eng, dst_ap, src_ap, owner=dst_t, writes=[dst_t])

    def store(self, dst_ap, src_t, src_ap, eng="sp"):
        self.k.dma(eng, dst_ap, src_ap, owner=src_t, reads=[src_t])

    def build(self):
        cfg = self.cfg
        k = self.k
        nc = self.nc
        D, T, KC = cfg.D, cfg.T, cfg.KC
        L = cfg.DEPTH
        x = self.din("x", [cfg.SEQ, D])
        meta = self.din("meta", [cfg.NMETA, D])
        consts = self.din("consts", [128, 3 * 128])
        self.iotac = self.din("iotac", [128, cfg.CAP])
        self.slotid = self.din("slotid", [128, cfg.CAP // 128])
        pvec = self.din("pvec", [128, self.npvec()])
        ropet = self.din("ropet", [2, 64, T])
        biasd = self.din("biasd", [cfg.SKV, 3, 128, cfg.GRP * 128])
        biasmk = self.din("biasmk", [cfg.SKV, 2, 16, cfg.GRP * 128])
        biasqm = self.din("biasqm", [cfg.SKV, 144, cfg.GRP * 16])
        sinkb = self.din("sinkb", [L, 128, cfg.SH])
        w_in = self.din("w_in", [L, D, cfg.IN_DIM + cfg.ROPE])
        w_uq = self.din("w_uq", [L, cfg.QR, cfg.MH * (cfg.NOPE + 2 * cfg.ROPE)])
        w_ukv = self.din("w_ukv", [L, cfg.KVR, cfg.MH * (cfg.NOPE + cfg.VD)])
        w_pa = self.din("w_pa", [L, cfg.MH * cfg.VD, D])
        w_pb = self.din("w_pb", [L, cfg.SH * cfg.HD, D])
        w_out = self.din("w_out", [L, D, D])
        n_dense = (L + 1) // 2
        n_moe = L // 2
        ffn_w1 = self.din("ffn_w1", [n_dense, D, cfg.DFF])
        ffn_w3 = self.din("ffn_w3", [n_dense, D, cfg.DFF])
        ffn_w2 = self.din("ffn_w2", [n_dense, cfg.DFF, D])
        if n_moe:
            router_w = self.din("router_w", [n_moe, D, cfg.NE])
            router_bb = self.din("router_bb", [n_moe, 128, cfg.NE])
            moe_w1 = self.din("moe_w1", [n_moe * cfg.NE, D, cfg.DFF])
            moe_w3 = self.din("moe_w3", [n_moe * cfg.NE, D, cfg.DFF])
            moe_w2 = self.din("moe_w2", [n_moe * cfg.NE, cfg.DFF, D])
        out = self.nc.dram_tensor("out", [cfg.SEQ, D], F32, kind="ExternalOutput").ap()
        s = self.s = {}
        s["hres"] = self.dscr("hres", [D, T], F32)
        s["hb"] = self.dscr("hb", [D, T], BF16)
        s["yT"] = self.dscr("yT", [D, T], F32)
        s["cqT"] = self.dscr("cqT", [cfg.QR, T], BF16)
        s["ckvT"] = self.dscr("ckvT", [cfg.KVR, T], BF16)
        s["krT"] = self.dscr("krT", [cfg.ROPE, T], BF16)
        s["qsT"] = self.dscr("qsT", [cfg.SH * cfg.HD, T], BF16)
        s["ksT"] = self.dscr("ksT", [cfg.SKV * cfg.HD, T], BF16)
        s["vs"] = self.dscr("vs", [T, cfg.SKV * cfg.HD], BF16)
        s["gaT"] = self.dscr("gaT", [D, T], BF16)
        s["gbT"] = self.dscr("gbT", [D, T], BF16)
        s["qnT"] = self.dscr("qnT", [cfg.MH * cfg.NOPE, T], BF16)
        s["qrT"] = self.dscr("qrT", [cfg.MH * cfg.ROPE, T], BF16)
        s["knT"] = self.dscr("knT", [cfg.MH * cfg.NOPE, T], BF16)
        s["vm"] = self.dscr("vm", [T, cfg.MH * cfg.VD], BF16)
        s["oaT"] = self.dscr("oaT", [cfg.MH * cfg.VD, T], BF16)
        s["obT"] = self.dscr("obT", [cfg.SH * cfg.HD, T], BF16)
        s["tAT"] = self.dscr("tAT", [D, T], F32)
        s["mgT"] = self.dscr("mgT", [D, T], BF16)
        s["gtT"] = self.dscr("gtT", [max(cfg.NE, 1), T], F32)
        s["pmT"] = self.dscr("pmT", [cfg.NE, T], F32)
        s["pmtm"] = self.dscr("pmtm", [T, cfg.NE], F32)
        s["htm"] = self.dscr("htm", [T, D], BF16)
        s["XeT"] = self.dscr("XeT", [cfg.NE * D, cfg.CAP], BF16)
        s["Yd"] = self.dscr("Yd", [cfg.NE * cfg.CAP, D], BF16)

        P = self.P = {}
        P["ident"] = k.sb("ident", [128, 128], F32, persistent=True)
        P["ones"] = k.sb("ones", [128, 128], F32, persistent=True)
        P["identb"] = k.sb("identb", [128, 128], BF16, persistent=True)
        P["onesb"] = k.sb("onesb", [128, 128], BF16, persistent=True)
        P["pvec"] = k.sb("pvec", [128, self.npvec()], F32, persistent=True)
        P["upper"] = k.sb("upper", [128, 128], F32, persistent=True)
        self.ps = [Tl(Buf(f"ps{i}"), nc.alloc_psum_tensor(f"ps{i}", [128, 512], F32)) for i in range(8)]
        self.load(P["ident"], P["ident"].h[:, :], consts[:, 0:128])
        self.load(P["ones"], P["ones"].h[:, :], consts[:, 128:256])
        self.load(P["identb"], P["identb"].h[:, :], consts[:, 0:128], eng="pool")
        self.load(P["onesb"], P["onesb"].h[:, :], consts[:, 128:256], eng="pool")
        self.load(P["pvec"], P["pvec"].h[:, :], pvec[:, :])
        self.load(P["upper"], P["upper"].h[:, :], consts[:, 256:384])
        self.ropet = ropet

        pv = self.pvoff()
        self.phase_embed(x, meta, pv["emb_g"], pv["emb_b"])
        k.barrier()
        done = self.stop_after == "embed"
        for l in range(L):
            if done:
                break
            self.phase_inproj(l, w_in[l])
            k.barrier()
            if self.stop_after == f"inproj{l}":
                break
            self.phase_mla_up(l, w_uq[l], w_ukv[l], pv[f"qn_g{l}"], pv[f"kvn_g{l}"])
            k.barrier()
            if self.stop_after == f"mlaup{l}":
                break
            self.phase_mla_attn(l)
            k.barrier()
            if self.stop_after == f"mlaattn{l}":
                break
            self.phase_swa(l, biasd, biasmk, biasqm, sinkb[l])
            k.barrier()
            if self.stop_after == f"swa{l}":
                break
            self.phase_proj(l, w_pa[l], w_pb[l], w_out[l])
            k.barrier()
            self.phase_ln(pv[f"mix_g{l}"], pv[f"mix_b{l}"])
            k.barrier()
            if self.stop_after == f"mix{l}":
                break
            if l % 2 == 0:
                self.phase_ffn([(ffn_w1[l // 2], ffn_w3[l // 2], ffn_w2[l // 2])], gated=False)
            else:
                self.phase_router(router_w[l // 2], router_bb[l // 2])
                k.barrier()
                if self.stop_after == f"router{l}":
                    break
                m = l // 2
                self.phase_gather()
                k.barrier()
                if self.stop_after == f"gather{l}":
                    break
                self.phase_moe_ffn([(moe_w1[m * cfg.NE + e], moe_w3[m * cfg.NE + e], moe_w2[m * cfg.NE + e])
                                    for e in range(cfg.NE)])
                k.barrier()
                if self.stop_after == f"moeffn{l}":
                    break
                self.phase_combine()
                if self.stop_after == f"combine{l}":
                    k.barrier()
                    break
            k.barrier()
            self.phase_ln(pv[f"ffn_g{l}"], pv[f"ffn_b{l}"])
            k.barrier()
        self.phase_output(out)
        k.barrier()
        k.emit()
        self.stack.close()
        return nc

    def pv_layout(self):
        cfg = self.cfg
        items = [("emb_g", cfg.KC), ("emb_b", cfg.KC)]
        for l in range(cfg.DEPTH):
            items += [(f"mix_g{l}", cfg.KC), (f"mix_b{l}", cfg.KC), (f"ffn_g{l}", cfg.KC), (f"ffn_b{l}", cfg.KC),
                      (f"qn_g{l}", cfg.QR // 128), (f"kvn_g{l}", cfg.KVR // 128)]
        return items

    def npvec(self):
        return sum(n for _, n in self.pv_layout())

    def pvoff(self):
        o = {}
        off = 0
        for name, n in self.pv_layout():
            o[name] = (off, n)
            off += n
        return o

    def ln_fm(self, y, n, gsl, bsl, c0, tmp):
        cfg, k, P = self.cfg, self.k, self.P
        KC, D = cfg.KC, cfg.D
        sq, mean, rstd, mr, msq, ob = tmp[:6]
        sbk = tmp[6] if len(tmp) > 6 else 6
        yv = y.h[:, :, 0:n]
        self.act(sq, sq.h[:, :, 0:n], y, yv, AF.Square)
        ps_s, ps_q = self.ps[sbk], self.ps[sbk + 1]
        for kc in range(KC):
            self.mm(ps_s, ps_s.h[:, 0:n], P["ones"].h[:, :], y.h[:, kc, 0:n], [P["ones"], y], kc == 0, kc == KC - 1)
        for kc in range(KC):
            self.mm(ps_q, ps_q.h[:, 0:n], P["ones"].h[:, :], sq.h[:, kc, 0:n], [P["ones"], sq], kc == 0, kc == KC - 1)
        self.ts(mean, mean.h[:, 0:n], ps_s, ps_s.h[:, 0:n], 1.0 / D, None, ALU.mult)
        self.tt(msq, msq.h[:, 0:n], mean, mean.h[:, 0:n], mean, mean.h[:, 0:n], ALU.mult)
        k.op("dve", lambda e: e.scalar_tensor_tensor(rstd.h[:, 0:n], ps_q.h[:, 0:n], 1.0 / D, msq.h[:, 0:n],
                                                     ALU.mult, ALU.subtract),
             reads=[ps_q, msq], writes=[rstd])
        self.ts(rstd, rstd.h[:, 0:n], rstd, rstd.h[:, 0:n], cfg.LN_EPS, None, ALU.add)
        self.act(rstd, rstd.h[:, 0:n], rstd, rstd.h[:, 0:n], AF.Sqrt)
        k.op('dve', lambda e: e.reciprocal(rstd.h[:, 0:n], rstd.h[:, 0:n]), reads=[rstd], writes=[rstd])
        self.tt(mr, mr.h[:, 0:n], mean, mean.h[:, 0:n], rstd, rstd.h[:, 0:n], ALU.mult)
        rb = rstd.h[:, 0:n].unsqueeze(1).to_broadcast([128, KC, n])
        mb = mr.h[:, 0:n].unsqueeze(1).to_broadcast([128, KC, n])
        self.tt(y, yv, y, yv, rstd, rb, ALU.mult)
        self.tt(y, yv, y, yv, mr, mb, ALU.subtract)
        pvh = P["pvec"]
        gb = pvh.h[:, gsl[0]:gsl[0] + KC].unsqueeze(2).to_broadcast([128, KC, n])
        bb = pvh.h[:, bsl[0]:bsl[0] + KC].unsqueeze(2).to_broadcast([128, KC, n])
        self.tt(y, yv, y, yv, pvh, gb, ALU.mult)
        self.tt(y, yv, y, yv, pvh, bb, ALU.add)
        self.cp(ob, ob.h[:, :, 0:n], y, yv, eng="act")
        self.store(self.fm(self.s["hres"])[:, :, c0:c0 + n], y, yv)
        self.store(self.fm(self.s["hb"])[:, :, c0:c0 + n], ob, ob.h[:, :, 0:n])

    def ln_tmps(self):
        k, KC = self.k, self.cfg.KC
        return (k.sb("sq", [128, KC, 512], F32), k.sb("mean", [128, 512], F32), k.sb("rstd", [128, 512], F32),
                k.sb("mr", [128, 512], F32), k.sb("msq", [128, 512], F32), k.sb("ob", [128, KC, 512], BF16))

    def phase_embed(self, x, meta, gsl, bsl):
        cfg, k, P = self.cfg, self.k, self.P
        KC = cfg.KC
        xin = [k.sb(f"xin{i}", [128, 4, cfg.D], F32) for i in range(1)]
        ys = [k.sb(f"y{i}", [128, KC, 512], F32) for i in range(2)]
        tmp = self.ln_tmps()
        def ldx(ci):
            c0, n = cfg.chunks[ci]
            xt = xin[0]
            if n == 512:
                self.load(xt, xt.h[:, :, :], x[c0:c0 + 512, :].rearrange("(a p) d -> p a d", p=128))
            else:
                self.load(xt, xt.h[0:n, 0, :], meta[:, :])

        ldx(0)
        for ci, (c0, n) in enumerate(cfg.chunks):
            xt = xin[0]
            y = ys[ci % 2]
            if n == 512:
                nt, rows = 4, 128
            else:
                nt, rows = 1, n
            for kc in range(KC):
                ps = self.ps[kc % 4]
                for a in range(nt):
                    self.mm(ps, ps.h[:, a * 128:a * 128 + rows], xt.h[0:rows, a, kc * 128:(kc + 1) * 128],
                            P["ident"].h[0:rows, 0:rows], [xt, P["ident"]], True, True)
                self.cp(y, y.h[:, kc, 0:n], ps, ps.h[:, 0:n], eng=("act" if kc % 2 else "dve"))
            if ci + 1 < len(cfg.chunks):
                ldx(ci + 1)
            self.ln_fm(y, n, gsl, bsl, c0, tmp)

    def phase_ln(self, gsl, bsl):
        cfg, k = self.cfg, self.k
        KC = cfg.KC
        ys = [k.sb(f"y{i}", [128, KC, 512], F32) for i in range(2)]
        tmps = [self.ln_tmps() + (4,), self.ln_tmps() + (6,)]
        def ld(ci):
            c0, n = cfg.chunks[ci]
            y = ys[ci % 2]
            self.load(y, y.h[:, :, 0:n], self.fm(self.s["yT"])[:, :, c0:c0 + n])

        ld(0)
        for ci, (c0, n) in enumerate(cfg.chunks):
            if ci + 1 < len(cfg.chunks):
                ld(ci + 1)
            self.ln_fm(ys[ci % 2], n, gsl, bsl, c0, tmps[ci % 2])

    def phase_output(self, out):
        cfg, k, P = self.cfg, self.k, self.P
        KC = cfg.KC
        hs = [k.sb(f"h{i}", [128, KC, 512], F32) for i in range(2)]
        os_ = [k.sb(f"o{i}", [128, cfg.D], F32) for i in range(2)]
        cnt = 0

        def ldo(ci):
            c0, n = cfg.chunks[ci]
            if n == 512:
                self.load(hs[ci % 2], hs[ci % 2].h[:, :, :], self.fm(self.s["hres"])[:, :, c0:c0 + n])

        ldo(0)
        for ci, (c0, n) in enumerate(cfg.chunks):
            if n != 512:
                continue
            h = hs[ci % 2]
            if ci + 1 < len(cfg.chunks):
                ldo(ci + 1)
            for a in range(4):
                o = os_[cnt % 2]
                cnt += 1
                for g in range(0, KC, 4):
                    ps = self.ps[(g // 4) % 4]
                    for j in range(min(4, KC - g)):
                        self.mm(ps, ps.h[:, j * 128:(j + 1) * 128], h.h[:, g + j, a * 128:(a + 1) * 128],
                                P["ident"].h[:, :], [h, P["ident"]], True, True)
                    w = min(4, KC - g) * 128
                    self.cp(o, o.h[:, g * 128:g * 128 + w], ps, ps.h[:, 0:w], eng=("act" if (g // 4) % 2 else "dve"))
                self.store(out[c0 + a * 128:c0 + (a + 1) * 128, :], o, o.h[:, :])

    def load_x(self, name, dram, K):
        cfg, k = self.cfg, self.k
        kc = K // 128
        X = k.sb(name, [128, kc, cfg.T], BF16)
        v = self.fm(dram)
        step = 1024
        for c0 in range(0, cfg.T, step):
            n = min(step, cfg.T - c0)
            k.dma("sp", X.h[:, :, c0:c0 + n], v[:, :, c0:c0 + n], owner=X, writes=[])
        X.b.w = {X.b.sem: k.dtotal[X.b.sem]}
        return X

    def linear(self, X, KCn, W, ftiles, epi, wbufs, gw=512, chunks=None, xoff=0, pre=None):
        cfg, k = self.cfg, self.k
        chunks = chunks if chunks is not None else cfg.chunks
        Wv = W.rearrange("(kc p) n -> p kc n", p=128)
        groups = []
        cur = []
        for fi, (col0, M) in enumerate(ftiles):
            if cur and (col0 != cur[-1][1] + cur[-1][2] or (col0 + M - cur[0][1]) > gw):
                groups.append(cur)
                cur = []
            cur.append((fi, col0, M))
        if cur:
            groups.append(cur)
        pi = 0
        steps = []
        for gi, grp in enumerate(groups):
            for (fi, col0, M) in grp:
                for ci, (c0, n) in enumerate(chunks):
                    steps.append((fi, ci, c0, n))
        si = 0
        if pre is not None and steps:
            pre(*steps[0])
        for gi, grp in enumerate(groups):
            wt = wbufs[gi % 2]
            g0 = grp[0][1]
            gwid = grp[-1][1] + grp[-1][2] - g0
            self.load(wt, wt.h[:, :, 0:gwid], Wv[:, :, g0:g0 + gwid], eng="pool")
            for (fi, col0, M) in grp:
                for ci, (c0, n) in enumerate(chunks):
                    if pre is not None and si + 1 < len(steps):
                        pre(*steps[si + 1])
                    si += 1
                    ps = self.ps[pi % 4]
                    pi += 1
                    for kc in range(KCn):
                        self.mm(ps, ps.h[0:M, 0:n], wt.h[:, kc, col0 - g0:col0 - g0 + M],
                                X.h[:, kc, c0 - xoff:c0 - xoff + n], [wt, X], kc == 0, kc == KCn - 1)
                    epi(fi, ci, c0, n, ps)

    def linear_tm(self, X, KCn, W, col0, ncols, epi, wt):
        cfg = self.cfg
        Wv = W.rearrange("(kc p) n -> p kc n", p=128)
        self.load(wt, wt.h[:, :, 0:ncols], Wv[:, :, col0:col0 + ncols], eng="pool")
        ntile = (cfg.T + 127) // 128
        for ti in range(ntile):
            t0 = ti * 128
            rows = min(128, cfg.T - t0)
            ps = self.ps[ti % 4]
            for kc in range(KCn):
                self.mm(ps, ps.h[0:rows, 0:ncols], X.h[:, kc, t0:t0 + rows], wt.h[:, kc, 0:ncols], [wt, X],
                        kc == 0, kc == KCn - 1)
            epi(ti, t0, rows, ps)

    def load_rope(self):
        k, P = self.k, self.P
        P["cos"] = k.sb("cos", [64, self.cfg.T], F32)
        P["sin"] = k.sb("sin", [64, self.cfg.T], F32)
        self.load(P["cos"], P["cos"].h[:, :], self.ropet[0, :, :])
        self.load(P["sin"], P["sin"].h[:, :], self.ropet[1, :, :])

    def phase_inproj(self, l, W):
        cfg, k, P, s = self.cfg, self.k, self.P, self.s
        KC = cfg.KC
        X = self.load_x("X", s["hb"], cfg.D)
        self.load_rope()
        wb = [k.sb(f"w{i}", [128, KC, 256], BF16) for i in range(2)]
        st = [k.sb(f"st{i}", [128, 512], BF16) for i in range(4)]
        t2 = k.sb("t2", [64, 512], F32)
        sti = [0]

        def stage():
            t = st[sti[0] % 4]
            sti[0] += 1
            return t

        def mk_copy(dst, row0):
            def epi(fi, ci, c0, n, ps, dst=dst, row0=row0):
                t = stage()
                r = row0 + fi * 128
                self.cp(t, t.h[:, 0:n], ps, ps.h[:, 0:n], eng=("act" if sti[0] % 2 else "dve"))
                self.store(dst[r:r + 128, c0:c0 + n], t, t.h[:, 0:n])
            return epi

        def mk_sig(dst):
            def epi(fi, ci, c0, n, ps, dst=dst):
                t = stage()
                self.act(t, t.h[:, 0:n], ps, ps.h[:, 0:n], AF.Sigmoid)
                self.store(dst[fi * 128:(fi + 1) * 128, c0:c0 + n], t, t.h[:, 0:n])
            return epi

        o = 0
        self.linear(X, KC, W, [(o + i * 128, 128) for i in range(cfg.QR // 128)], mk_copy(s["cqT"], 0), wb, gw=256)
        o += cfg.QR
        self.linear(X, KC, W, [(o + i * 128, 128) for i in range(cfg.KVR // 128)], mk_copy(s["ckvT"], 0), wb, gw=256)
        o += cfg.KVR
        t1all = k.sb("t1all", [64, cfg.T], F32)

        def epi_r1(fi, ci, c0, n, ps):
            self.tt(t1all, t1all.h[:, c0:c0 + n], ps, ps.h[0:64, 0:n], P["cos"], P["cos"].h[:, c0:c0 + n], ALU.mult)

        def epi_r2(fi, ci, c0, n, ps):
            self.tt(t2, t2.h[:, 0:n], ps, ps.h[0:64, 0:n], P["sin"], P["sin"].h[:, c0:c0 + n], ALU.mult)
            t = stage()
            self.tt(t, t.h[0:64, 0:n], t1all, t1all.h[:, c0:c0 + n], t2, t2.h[:, 0:n], ALU.add)
            self.store(s["krT"][:, c0:c0 + n], t, t.h[0:64, 0:n])

        self.linear(X, KC, W, [(o, 64)], epi_r1, wb, gw=256)
        self.linear(X, KC, W, [(cfg.IN_DIM, 64)], epi_r2, wb, gw=256)
        o += cfg.ROPE
        self.linear(X, KC, W, [(o + i * 128, 128) for i in range(cfg.SH)], mk_copy(s["qsT"], 0), wb, gw=256)
        o += cfg.SH * cfg.HD
        self.linear(X, KC, W, [(o + i * 128, 128) for i in range(cfg.SKV)], mk_copy(s["ksT"], 0), wb, gw=256)
        o += cfg.SKV * cfg.HD
        ncv = cfg.SKV * cfg.HD

        def epi_v(ti, t0, rows, ps):
            t = stage()
            self.cp(t, t.h[0:rows, 0:ncv], ps, ps.h[0:rows, 0:ncv], eng=("act" if ti % 2 else "dve"))
            self.store(s["vs"][t0:t0 + rows, :], t, t.h[0:rows, 0:ncv])

        for v0 in range(0, ncv, 256):
            vn = min(256, ncv - v0)

            def epi_v(ti, t0, rows, ps, v0=v0, vn=vn):
                t = stage()
                self.cp(t, t.h[0:rows, 0:vn], ps, ps.h[0:rows, 0:vn], eng=('act' if ti % 2 else 'dve'))
                self.store(s['vs'][t0:t0 + rows, v0:v0 + vn], t, t.h[0:rows, 0:vn])

            self.linear_tm(X, KC, W, o + v0, vn, epi_v, wb[(v0 // 256) % 2])
        o += ncv
        self.linear(X, KC, W, [(o + i * 128, 128) for i in range(KC)], mk_sig(s["gaT"]), wb, gw=256)
        o += cfg.D
        self.linear(X, KC, W, [(o + i * 128, 128) for i in range(KC)], mk_sig(s["gbT"]), wb, gw=256)

    def rms_stats(self, X, KCn, gsl, rdim, rstd_bc, rstd_tm):
        cfg, k, P = self.cfg, self.k, self.P
        sqs = [k.sb(f"rsq{i}", [128, KCn, 512], BF16) for i in range(2)]
        for ci, (c0, n) in enumerate(cfg.chunks):
            sq = sqs[ci % 2]
            self.act(sq, sq.h[:, :, 0:n], X, X.h[:, :, c0:c0 + n], AF.Square)
            ps = self.ps[4 + ci % 2]
            for kc in range(KCn):
                self.mm(ps, ps.h[:, 0:n], P["onesb"].h[:, :], sq.h[:, kc, 0:n], [P["onesb"], sq], kc == 0, kc == KCn - 1)
            self.ts(rstd_bc, rstd_bc.h[:, c0:c0 + n], ps, ps.h[:, 0:n], 1.0 / rdim, cfg.RMS_EPS, ALU.mult, ALU.add)
            self.act(rstd_bc, rstd_bc.h[:, c0:c0 + n], rstd_bc, rstd_bc.h[:, c0:c0 + n], AF.Sqrt)
            k.op('dve', lambda e, c0=c0, n=n: e.reciprocal(rstd_bc.h[:, c0:c0 + n], rstd_bc.h[:, c0:c0 + n]), reads=[rstd_bc], writes=[rstd_bc])
            if rstd_tm is not None:
                for a in range((n + 127) // 128):
                    rows = min(128, n - a * 128)
                    ti = (c0 + a * 128) // 128
                    pt = self.ps[6 + a % 2]
                    for kc in range(KCn):
                        self.mm(pt, pt.h[0:rows, 0:1], sq.h[:, kc, a * 128:a * 128 + rows], P["onesb"].h[:, 0:1],
                                [P["onesb"], sq], kc == 0, kc == KCn - 1)
                    self.ts(rstd_tm, rstd_tm.h[0:rows, ti:ti + 1], pt, pt.h[0:rows, 0:1], 1.0 / rdim, cfg.RMS_EPS,
                            ALU.mult, ALU.add)
                    self.act(rstd_tm, rstd_tm.h[0:rows, ti:ti + 1], rstd_tm, rstd_tm.h[0:rows, ti:ti + 1], AF.Sqrt)
                    k.op('dve', lambda e, rows=rows, ti=ti: e.reciprocal(rstd_tm.h[0:rows, ti:ti + 1], rstd_tm.h[0:rows, ti:ti + 1]), reads=[rstd_tm], writes=[rstd_tm])
        pvh = P["pvec"]
        for kc in range(KCn):
            self.ts(X, X.h[:, kc, :], X, X.h[:, kc, :], pvh.h[:, gsl[0] + kc:gsl[0] + kc + 1], None, ALU.mult,
                    extra_reads=[pvh])

    def phase_mla_up(self, l, Wq, Wkv, qg, kvg):
        cfg, k, P, s = self.cfg, self.k, self.P, self.s
        T = cfg.T
        MH = cfg.MH
        ntile = (T + 127) // 128
        KQ = cfg.QR // 128
        X = self.load_x("Xq", s["cqT"], cfg.QR)
        rq = k.sb("rq", [128, T], F32)
        self.load_rope()
        self.rms_stats(X, KQ, qg, cfg.QR, rq, None)
        wb = [k.sb(f"w{i}", [128, max(KQ, cfg.KVR // 128), 512], BF16) for i in range(2)]
        st = [k.sb(f"st{i}", [128, 512], BF16) for i in range(4)]
        t1 = k.sb("t1", [64, 512], F32)
        t2 = k.sb("t2", [64, 512], F32)
        sti = [0]

        def stage():
            t = st[sti[0] % 4]
            sti[0] += 1
            return t

        def epi_qn(fi, ci, c0, n, ps):
            t = stage()
            self.tt(t, t.h[:, 0:n], ps, ps.h[:, 0:n], rq, rq.h[:, c0:c0 + n], ALU.mult)
            self.store(s["qnT"][fi * 128:(fi + 1) * 128, c0:c0 + n], t, t.h[:, 0:n])

        self.linear(X, KQ, Wq, [(h * 128, 128) for h in range(MH)], epi_qn, wb)
        ro = MH * cfg.NOPE
        t1all = k.sb("t1all", [64, T], F32)

        def epi_qr(fi, ci, c0, n, ps):
            h_ = fi // 2
            if fi % 2 == 0:
                self.tt(t1all, t1all.h[:, c0:c0 + n], ps, ps.h[0:64, 0:n], P["cos"], P["cos"].h[:, c0:c0 + n],
                        ALU.mult)
            else:
                self.tt(t2, t2.h[:, 0:n], ps, ps.h[0:64, 0:n], P["sin"], P["sin"].h[:, c0:c0 + n], ALU.mult)
                self.tt(t2, t2.h[:, 0:n], t2, t2.h[:, 0:n], t1all, t1all.h[:, c0:c0 + n], ALU.add)
                t = stage()
                self.tt(t, t.h[0:64, 0:n], t2, t2.h[:, 0:n], rq, rq.h[0:64, c0:c0 + n], ALU.mult)
                self.store(s["qrT"][h_ * 64:(h_ + 1) * 64, c0:c0 + n], t, t.h[0:64, 0:n])

        fts = []
        for h in range(MH):
            fts += [(ro + h * 128, 64), (ro + h * 128 + 64, 64)]
        self.linear(X, KQ, Wq, fts, epi_qr, wb)
        self.k.barrier()
        KV = cfg.KVR // 128
        X = self.load_x("Xkv", s["ckvT"], cfg.KVR)
        rk = k.sb("rk", [128, T], F32)
        rkt = k.sb("rkt", [128, ntile], F32)
        self.rms_stats(X, KV, kvg, cfg.KVR, rk, rkt)
        wb = [k.sb(f"w{i}", [128, KV, 512], BF16) for i in range(2)]
        st = [k.sb(f"st{i}", [128, 512], BF16) for i in range(4)]

        def epi_kn(fi, ci, c0, n, ps):
            t = stage2()
            self.tt(t, t.h[:, 0:n], ps, ps.h[:, 0:n], rk, rk.h[:, c0:c0 + n], ALU.mult)
            self.store(s["knT"][fi * 128:(fi + 1) * 128, c0:c0 + n], t, t.h[:, 0:n])

        sti2 = [0]

        def stage2():
            t = st[sti2[0] % 4]
            sti2[0] += 1
            return t

        self.linear(X, KV, Wkv, [(h * 128, 128) for h in range(MH)], epi_kn, wb)
        vo = MH * cfg.NOPE
        for g0 in range(0, MH * cfg.VD, 512):
            ncv = min(512, MH * cfg.VD - g0)

            def epi_v(ti, t0, rows, ps, g0=g0, ncv=ncv):
                t = stage2()
                self.ts(t, t.h[0:rows, 0:ncv], ps, ps.h[0:rows, 0:ncv], rkt.h[0:rows, ti:ti + 1], None, ALU.mult,
                        extra_reads=[rkt])
                self.store(s["vm"][t0:t0 + rows, g0:g0 + ncv], t, t.h[0:rows, 0:ncv])

            self.linear_tm(X, KV, Wkv, vo + g0, ncv, epi_v, wb[(g0 // 512) % 2])

    def phase_mla_attn(self, l):
        cfg, k, P, s = self.cfg, self.k, self.P, self.s
        T = cfg.T
        ntile = (T + 127) // 128
        scale = (cfg.NOPE + cfg.ROPE) ** -0.5
        kr = k.sb("kr", [128, T], BF16)
        k.op("dve", lambda e: e.memset(kr.h[64:128, :], 0.0), writes=[kr])
        self.load(kr, kr.h[0:64, :], s["krT"][:, :])
        kns = [k.sb(f"kn{i}", [128, T], BF16) for i in range(2)]
        vs_ = [k.sb(f"v{i}", [128, ntile, cfg.VD], BF16) for i in range(2)]
        qns = [k.sb(f"qn{i}", [128, 512], BF16) for i in range(2)]
        qrs = [k.sb(f"qr{i}", [128, 512], BF16) for i in range(2)]
        for t in qrs:
            k.op("dve", lambda e, t=t: e.memset(t.h[64:128, :], 0.0), writes=[t])
        pts = [k.sb(f"pt{i}", [128, 512], BF16) for i in range(4)]
        sbank = [self.ps[0], self.ps[1], self.ps[2], self.ps[5]]
        rcs = [k.sb(f"rc{i}", [128, 512], F32) for i in range(2)]
        obs = [k.sb(f"ob{i}", [128, 512], BF16) for i in range(2)]
        nfull = T // 128
        rem = T - nfull * 128

        def load_head(h):
            kn = kns[h % 2]
            v = vs_[h % 2]
            self.load(kn, kn.h[:, :], s["knT"][h * 128:(h + 1) * 128, :])
            k.dma("sp", v.h[:, 0:nfull, :],
                  s["vm"][0:nfull * 128, h * cfg.VD:(h + 1) * cfg.VD].rearrange("(a p) d -> p a d", p=128),
                  owner=v, writes=[v])
            if rem:
                k.dma("sp", v.h[0:rem, nfull, :], s["vm"][nfull * 128:T, h * cfg.VD:(h + 1) * cfg.VD], owner=v,
                      writes=[])
                v.b.w = {v.b.sem: k.dtotal[v.b.sem]}

        steps = [(h, c0, n) for h in range(cfg.MH) for (c0, n) in cfg.chunks]

        def load_q(si):
            h, c0, n = steps[si]
            qn, qr = qns[si % 2], qrs[si % 2]
            self.load(qn, qn.h[:, 0:n], s["qnT"][h * 128:(h + 1) * 128, c0:c0 + n])
            self.load(qr, qr.h[0:64, 0:n], s["qrT"][h * 64:(h + 1) * 64, c0:c0 + n])

        load_head(0)
        load_q(0)
        for si, (h, c0, n) in enumerate(steps):
            kn = kns[h % 2]
            v = vs_[h % 2]
            if c0 == 0 and h + 1 < cfg.MH:
                load_head(h + 1)
            if si + 1 < len(steps):
                load_q(si + 1)
            qn, qr, rc, ob = qns[si % 2], qrs[si % 2], rcs[si % 2], obs[si % 2]
            po, pd = self.ps[3], self.ps[4]

            def smm(kt):
                kk = 128 if kt < nfull else rem
                S = sbank[kt % 4]
                self.mm(S, S.h[0:kk, 0:n], kn.h[:, kt * 128:kt * 128 + kk], qn.h[:, 0:n], [kn, qn], True, False)
                self.mm(S, S.h[0:kk, 0:n], kr.h[:, kt * 128:kt * 128 + kk], qr.h[:, 0:n], [kr, qr], False, True)

            smm(0)
            if ntile > 1:
                smm(1)
            for kt in range(ntile):
                kk = 128 if kt < nfull else rem
                if kt + 2 < ntile:
                    smm(kt + 2)
                S = sbank[kt % 4]
                pt = pts[kt % 4]
                self.act(pt, pt.h[0:kk, 0:n], S, S.h[0:kk, 0:n], AF.Exp, scale=scale)
                self.mm(po, po.h[:, 0:n], v.h[0:kk, kt, :], pt.h[0:kk, 0:n], [v, pt], kt == 0, kt == ntile - 1)
                self.mm(pd, pd.h[:, 0:n], P["onesb"].h[0:kk, :], pt.h[0:kk, 0:n], [P["onesb"], pt], kt == 0,
                        kt == ntile - 1)
            k.op("dve", lambda e, rc=rc, pd=pd, n=n: e.reciprocal(rc.h[:, 0:n], pd.h[:, 0:n]), reads=[pd],
                 writes=[rc])
            self.tt(ob, ob.h[:, 0:n], po, po.h[:, 0:n], rc, rc.h[:, 0:n], ALU.mult)
            self.store(s["oaT"][h * 128:(h + 1) * 128, c0:c0 + n], ob, ob.h[:, 0:n])

    def phase_swa(self, l, biasd, biasmk, biasqm, sinkb):
        cfg, k, P, s = self.cfg, self.k, self.P, self.s
        T, NB, G = cfg.T, cfg.NB, cfg.GRP
        ntile = (T + 127) // 128
        scale = cfg.HD ** -0.5
        inv = 1.0 / scale
        GW = G * 128
        sk = k.sb("sk", [128, cfg.SH], F32)
        self.load(sk, sk.h[:, :], sinkb[:, :])
        self.act(sk, sk.h[:, :], sk, sk.h[:, :], AF.Exp)
        ks_ = [k.sb(f"ks{i}", [128, T], BF16) for i in range(2)]
        vs_ = [k.sb(f"v{i}", [128, ntile, cfg.HD], BF16) for i in range(2)]
        qs_ = [k.sb(f"q{i}", [128, G, T], BF16) for i in range(2)]
        bd = [k.sb(f"bd{i}", [128, 3, GW], BF16) for i in range(2)]
        bmk = [k.sb(f"bmk{i}", [16, 2, GW], BF16) for i in range(2)]
        bqm = [k.sb(f"bqm{i}", [128, 2, G * 16], BF16) for i in range(2)]
        ske = [k.sb(f"ske{i}", [128, G, 128], F32) for i in range(2)]
        pts = [k.sb(f"pt{i}", [128, GW], BF16) for i in range(4)]
        rcs = [k.sb(f"rc{i}", [128, GW], F32) for i in range(2)]
        obs = [k.sb(f"ob{i}", [128, G, 128], BF16) for i in range(2)]
        si = 0
        bi = 0
        def load_hk(hk):
            ks, v, q = ks_[hk % 2], vs_[hk % 2], qs_[hk % 2]
            b_d, b_mk, b_qm = bd[hk % 2], bmk[hk % 2], bqm[hk % 2]
            self.load(ks, ks.h[:, :], s["ksT"][hk * 128:(hk + 1) * 128, :])
            k.dma("sp", v.h[:, 0:NB, :],
                  s["vs"][0:NB * 128, hk * cfg.HD:(hk + 1) * cfg.HD].rearrange("(a p) d -> p a d", p=128),
                  owner=v, writes=[v])
            k.dma("sp", v.h[0:16, NB, :], s["vs"][NB * 128:T, hk * cfg.HD:(hk + 1) * cfg.HD], owner=v, writes=[])
            v.b.w = {v.b.sem: k.dtotal[v.b.sem]}
            self.load(q, q.h[:, :, :],
                      s["qsT"][hk * G * 128:(hk + 1) * G * 128, :].rearrange("(g p) t -> p g t", p=128))
            self.load(b_d, b_d.h[:, :, :], biasd[hk].rearrange("d p w -> p d w"), eng="pool")
            self.load(b_mk, b_mk.h[:, :, :], biasmk[hk].rearrange("d p w -> p d w"), eng="pool")
            k.dma("pool", b_qm.h[0:16, 0, :], biasqm[hk, 0:16, :], owner=b_qm, writes=[b_qm])
            k.dma("pool", b_qm.h[:, 1, :], biasqm[hk, 16:144, :], owner=b_qm, writes=[])
            b_qm.b.w = {b_qm.b.sem: k.dtotal[b_qm.b.sem]}

        load_hk(0)
        for hk in range(cfg.SKV):
            ks, v, q = ks_[hk % 2], vs_[hk % 2], qs_[hk % 2]
            b_d, b_mk, b_qm, sk_e = bd[hk % 2], bmk[hk % 2], bqm[hk % 2], ske[hk % 2]
            if hk + 1 < cfg.SKV:
                load_hk(hk + 1)
            self.ts(b_d, b_d.h[:, :, :], b_d, b_d.h[:, :, :], inv, None, ALU.mult)
            self.ts(b_mk, b_mk.h[:, :, :], b_mk, b_mk.h[:, :, :], inv, None, ALU.mult)
            self.ts(b_qm, b_qm.h[0:16, 0, :], b_qm, b_qm.h[0:16, 0, :], inv, None, ALU.mult)
            self.ts(b_qm, b_qm.h[:, 1, :], b_qm, b_qm.h[:, 1, :], inv, None, ALU.mult)
            for g in range(G):
                hh = hk * G + g
                self.cp(sk_e, sk_e.h[:, g, :], sk, sk.h[:, hh:hh + 1].to_broadcast([128, 128]))
            for i in range(NB + 1):
                if i < NB:
                    nq = 128
                    W_ = GW
                    qap = q.h[:, :, i * 128:(i + 1) * 128]
                    keys = [(NB, 16, b_mk.h[0:16, 0 if i == 0 else 1, :])]
                    for dlt in (-1, 0, 1):
                        j = i + dlt
                        if 0 <= j < NB:
                            keys.append((j, 128, b_d.h[:, dlt + 1, :]))
                else:
                    nq = 16
                    W_ = G * 16
                    qap = q.h[:, :, NB * 128:NB * 128 + 16]
                    keys = [(NB, 16, b_qm.h[0:16, 0, :]), (0, 128, b_qm.h[:, 1, :])]
                po, pd = self.ps[3], self.ps[4]
                rc, ob = rcs[bi % 2], obs[bi % 2]
                bi += 1
                nk = len(keys)
                sb4 = [self.ps[0], self.ps[1], self.ps[2], self.ps[5]]
                slots = []
                for ki, (j, kk, bap) in enumerate(keys):
                    S = sb4[si % 4]
                    pt = pts[si % 4]
                    si += 1
                    slots.append((S, pt))
                    braw = b_mk if (i < NB and ki == 0) else (b_d if i < NB else b_qm)
                    self.mm(S, S.h[0:kk, 0:W_], ks.h[:, j * 128:j * 128 + kk], qap, [ks, q], True, False)
                    self.mm(S, S.h[0:kk, 0:W_], P["identb"].h[0:kk, 0:kk], bap, [P["identb"], braw], False, True)
                for ki, (j, kk, bap) in enumerate(keys):
                    S, pt = slots[ki]
                    self.act(pt, pt.h[0:kk, 0:W_], S, S.h[0:kk, 0:W_], AF.Exp, scale=scale)
                    self.mm(po, po.h[:, 0:W_], v.h[0:kk, j, :], pt.h[0:kk, 0:W_], [v, pt], ki == 0, ki == nk - 1)
                    self.mm(pd, pd.h[:, 0:W_], P["onesb"].h[0:kk, :], pt.h[0:kk, 0:W_], [P["onesb"], pt], ki == 0,
                            ki == nk - 1)
                pdv = pd.h[:, 0:W_].rearrange("p (g q) -> p g q", g=G)
                rcv = rc.h[:, 0:W_].rearrange("p (g q) -> p g q", g=G)
                pov = po.h[:, 0:W_].rearrange("p (g q) -> p g q", g=G)
                self.tt(rc, rcv, pd, pdv, sk_e, sk_e.h[:, :, 0:nq], ALU.add)
                k.op("dve", lambda e, rcv=rcv: e.reciprocal(rcv, rcv), reads=[rc], writes=[rc])
                self.tt(ob, ob.h[:, :, 0:nq], po, pov, rc, rcv, ALU.mult)
                t0 = i * 128
                dst = s["obT"][hk * G * 128:(hk + 1) * G * 128, t0:t0 + nq].rearrange("(g p) t -> p g t", p=128)
                self.store(dst, ob, ob.h[:, :, 0:nq])

    def phase_proj(self, l, Wa, Wb, Wo):
        cfg, k, P, s = self.cfg, self.k, self.P, self.s
        KC = cfg.KC

        def run(Xd, W, epi, nm, pre):
            Kx = Xd.shape[0]
            X = self.load_x(nm, Xd, Kx)
            wb = [k.sb(f"w{i}", [128, Kx // 128, 512], BF16) for i in range(2)]
            self.linear(X, Kx // 128, W, [(i * 128, 128) for i in range(KC)], epi, wb, pre=pre)

        NBUF = 4

        def alloc_common():
            return ([k.sb(f"g{i}", [128, 512], BF16) for i in range(NBUF)],
                    [k.sb(f"a{i}", [128, 512], F32) for i in range(NBUF)],
                    [k.sb(f"o{i}", [128, 512], F32) for i in range(NBUF)],
                    [k.sb(f"ob{i}", [128, 512], BF16) for i in range(NBUF)])

        pc = [0]
        ec = [0]
        gt, at, ot, obt = alloc_common()

        def preA(fi, ci, c0, n):
            i = pc[0] % NBUF
            pc[0] += 1
            self.load(gt[i], gt[i].h[:, 0:n], s["gaT"][fi * 128:(fi + 1) * 128, c0:c0 + n])

        def epiA(fi, ci, c0, n, ps):
            i = ec[0] % NBUF
            ec[0] += 1
            self.tt(ot[i], ot[i].h[:, 0:n], ps, ps.h[:, 0:n], gt[i], gt[i].h[:, 0:n], ALU.mult)
            self.store(s["tAT"][fi * 128:(fi + 1) * 128, c0:c0 + n], ot[i], ot[i].h[:, 0:n])

        run(s["oaT"], Wa, epiA, "Xa", preA)
        k.barrier()
        gt, at, ot, obt = alloc_common()
        pc[0] = ec[0] = 0

        def preB(fi, ci, c0, n):
            i = pc[0] % NBUF
            pc[0] += 1
            self.load(gt[i], gt[i].h[:, 0:n], s["gbT"][fi * 128:(fi + 1) * 128, c0:c0 + n])
            self.load(at[i], at[i].h[:, 0:n], s["tAT"][fi * 128:(fi + 1) * 128, c0:c0 + n])

        def epiB(fi, ci, c0, n, ps):
            i = ec[0] % NBUF
            ec[0] += 1
            self.tt(ot[i], ot[i].h[:, 0:n], ps, ps.h[:, 0:n], gt[i], gt[i].h[:, 0:n], ALU.mult)
            self.tt(obt[i], obt[i].h[:, 0:n], ot[i], ot[i].h[:, 0:n], at[i], at[i].h[:, 0:n], ALU.add)
            self.store(s["mgT"][fi * 128:(fi + 1) * 128, c0:c0 + n], obt[i], obt[i].h[:, 0:n])

        run(s["obT"], Wb, epiB, "Xb", preB)
        k.barrier()
        gt, at, ot, obt = alloc_common()
        pc[0] = ec[0] = 0

        def preO(fi, ci, c0, n):
            i = pc[0] % NBUF
            pc[0] += 1
            self.load(at[i], at[i].h[:, 0:n], s["hres"][fi * 128:(fi + 1) * 128, c0:c0 + n])

        def epiO(fi, ci, c0, n, ps):
            i = ec[0] % NBUF
            ec[0] += 1
            k.op("dve", lambda e, i=i, n=n, ps=ps: e.scalar_tensor_tensor(ot[i].h[:, 0:n], at[i].h[:, 0:n], cfg.ALPHA,
                                                                          ps.h[:, 0:n], ALU.mult, ALU.add),
                 reads=[at[i], ps], writes=[ot[i]])
            self.store(s["yT"][fi * 128:(fi + 1) * 128, c0:c0 + n], ot[i], ot[i].h[:, 0:n])

        run(s["mgT"], Wo, epiO, "Xm", preO)

    def phase_router(self, Wr, rbb):
        cfg, k, P, s = self.cfg, self.k, self.P, self.s
        KC, NE, T = cfg.KC, cfg.NE, cfg.T
        wr = k.sb("wr", [128, KC, NE], F32)
        self.load(wr, wr.h[:, :, :], Wr.rearrange("(kc p) e -> p kc e", p=128))
        rb = k.sb("rb", [128, NE], F32)
        self.load(rb, rb.h[:, :], rbb[:, :])
        hs = [k.sb(f"h{i}", [128, KC, 512], F32) for i in range(2)]
        lg = [k.sb(f"lg{i}", [128, NE], F32) for i in range(2)]
        mx = [k.sb(f"mx{i}", [128, 8], F32) for i in range(2)]
        ex = [k.sb(f"ex{i}", [128, NE], F32) for i in range(2)]
        mk = [k.sb(f"mk{i}", [128, NE], F32) for i in range(2)]
        dn = [k.sb(f"dn{i}", [128, 2], F32) for i in range(2)]
        gtt = [k.sb(f"gtt{i}", [NE, 128], F32) for i in range(2)]
        pmt = [k.sb(f"pmt{i}", [NE, 128], F32) for i in range(2)]
        pm = [k.sb(f"pm{i}", [128, NE], F32) for i in range(2)]
        R = k.sb("R", [128, NE], F32)
        k.op("dve", lambda e: e.memset(R.h[:, :], 0.0), writes=[R])
        obf = [k.sb(f"obf{i}", [128, cfg.D], BF16) for i in range(2)]
        ti = 0
        def ldh(ci):
            c0, n = cfg.chunks[ci]
            h = hs[ci % 2]
            self.load(h, h.h[:, :, 0:n], self.fm(s["hres"])[:, :, c0:c0 + n])

        ldh(0)
        for ci, (c0, n) in enumerate(cfg.chunks):
            h = hs[ci % 2]
            if ci + 1 < len(cfg.chunks):
                ldh(ci + 1)
            for a in range((n + 127) // 128):
                rows = min(128, n - a * 128)
                t0 = c0 + a * 128
                i = ti % 2
                ti += 1
                ps = self.ps[i]
                for kc in range(KC):
                    self.mm(ps, ps.h[0:rows, 0:NE], h.h[:, kc, a * 128:a * 128 + rows], wr.h[:, kc, :], [h, wr],
                            kc == 0, kc == KC - 1)
                L_, M_, E_, K_, D_ = lg[i], mx[i], ex[i], mk[i], dn[i]
                self.tt(L_, L_.h[0:rows, :], ps, ps.h[0:rows, 0:NE], rb, rb.h[0:rows, :], ALU.add)
                k.op("dve", lambda e, M_=M_, L_=L_, rows=rows: e.max(M_.h[0:rows, :], L_.h[0:rows, :]), reads=[L_],
                     writes=[M_])
                self.ts(K_, K_.h[0:rows, :], L_, L_.h[0:rows, :], M_.h[0:rows, 1:2], None, ALU.is_ge, extra_reads=[M_])
                self.ts(D_, D_.h[0:rows, 0:1], M_, M_.h[0:rows, 0:1], -1.0, None, ALU.mult)
                self.act(E_, E_.h[0:rows, :], L_, L_.h[0:rows, :], AF.Exp, extra_reads=[D_], bias=D_.h[0:rows, 0:1])
                self.tt(E_, E_.h[0:rows, :], E_, E_.h[0:rows, :], K_, K_.h[0:rows, :], ALU.mult)
                k.op("dve", lambda e, D_=D_, E_=E_, rows=rows: e.tensor_reduce(D_.h[0:rows, 1:2], E_.h[0:rows, :],
                                                                              AX.X, ALU.add),
                     reads=[E_], writes=[D_])
                k.op("dve", lambda e, D_=D_, rows=rows: e.reciprocal(D_.h[0:rows, 1:2], D_.h[0:rows, 1:2]), reads=[D_],
                     writes=[D_])
                self.ts(E_, E_.h[0:rows, :], E_, E_.h[0:rows, :], D_.h[0:rows, 1:2], None, ALU.mult, extra_reads=[D_])
                pt = self.ps[2 + i]
                self.mm(pt, pt.h[0:NE, 0:rows], E_.h[0:rows, :], P["ident"].h[0:rows, 0:rows], [E_, P["ident"]], True,
                        True)
                G_ = gtt[i]
                self.cp(G_, G_.h[:, 0:rows], pt, pt.h[0:NE, 0:rows], eng="act")
                self.store(s["gtT"][:, t0:t0 + rows], G_, G_.h[:, 0:rows])
                pp = self.ps[4 + i]
                self.mm(pp, pp.h[0:rows, 0:NE], P["upper"].h[0:rows, 0:rows], K_.h[0:rows, :], [P["upper"], K_], True,
                        ti == 1)
                if ti > 1:
                    self.mm(pp, pp.h[0:rows, 0:NE], P["ones"].h[:, 0:rows], R.h[:, :], [P["ones"], R], False, True)
                PM = pm[i]
                k.op("dve", lambda e, PM=PM, pp=pp, K_=K_, rows=rows: e.scalar_tensor_tensor(
                    PM.h[0:rows, :], pp.h[0:rows, 0:NE], 1.0, K_.h[0:rows, :], ALU.add, ALU.mult),
                    reads=[pp, K_], writes=[PM])
                self.ts(PM, PM.h[0:rows, :], PM, PM.h[0:rows, :], -1.0, None, ALU.add)
                self.tt(R, R.h[0:rows, :], R, R.h[0:rows, :], K_, K_.h[0:rows, :], ALU.add)
                self.store(s["pmtm"][t0:t0 + rows, :], PM, PM.h[0:rows, :])
                pt2 = self.ps[6 + i]
                self.mm(pt2, pt2.h[0:NE, 0:rows], PM.h[0:rows, :], P["ident"].h[0:rows, 0:rows], [PM, P["ident"]],
                        True, True)
                PT = pmt[i]
                self.cp(PT, PT.h[:, 0:rows], pt2, pt2.h[0:NE, 0:rows], eng="act")
                self.store(s["pmT"][:, t0:t0 + rows], PT, PT.h[:, 0:rows])
                ob = obf[i]
                for g in range(0, KC, 4):
                    ph = self.ps[2 + (g // 4) % 2]
                    gn = min(4, KC - g)
                    for j in range(gn):
                        self.mm(ph, ph.h[0:rows, j * 128:(j + 1) * 128], h.h[:, g + j, a * 128:a * 128 + rows],
                                P["ident"].h[:, :], [h, P["ident"]], True, True)
                    self.cp(ob, ob.h[0:rows, g * 128:(g + gn) * 128], ph, ph.h[0:rows, 0:gn * 128],
                            eng=("act" if (g // 4) % 2 else "dve"))
                self.store(s["htm"][t0:t0 + rows, :], ob, ob.h[0:rows, :])

    def slot_chunks(self, s0, s1):
        out = []
        c = s0
        while c < s1:
            n = min(512, s1 - c)
            out.append((c, n))
            c += n
        return out

    def phase_gather(self):
        cfg, k, P, s = self.cfg, self.k, self.P, self.s
        KC, NE, T, CAP = cfg.KC, cfg.NE, cfg.T, cfg.CAP
        nfull = T // 128
        rem = T - nfull * 128
        ntile = nfull + (1 if rem else 0)
        iota = k.sb("iota", [128, CAP], F32)
        self.load(iota, iota.h[:, :], self.iotac[:, :])
        pma = k.sb("pma", [128, ntile, NE], F32)
        k.dma("sp", pma.h[:, 0:nfull, :], s["pmtm"][0:nfull * 128, :].rearrange("(a p) e -> p a e", p=128), owner=pma,
              writes=[pma])
        if rem:
            k.dma("sp", pma.h[0:rem, nfull, :], s["pmtm"][nfull * 128:T, :], owner=pma, writes=[])
            pma.b.w = {pma.b.sem: k.dtotal[pma.b.sem]}
        sel = k.sb("sel", [128, ntile, CAP], BF16)
        hts = [k.sb(f"htk{i}", [128, ntile, 256], BF16) for i in range(2)]
        st = [k.sb(f"st{i}", [128, 512], BF16) for i in range(4)]
        sti = 0
        hi = 0
        def ld_ht(idx):
            kc0 = (idx % ((KC + 1) // 2)) * 2
            kn_ = min(2, KC - kc0)
            ht = hts[idx % 2]
            k.dma("sp", ht.h[:, 0:nfull, 0:kn_ * 128],
                  s["htm"][0:nfull * 128, kc0 * 128:(kc0 + kn_) * 128].rearrange("(a p) d -> p a d", p=128),
                  owner=ht, writes=[ht])
            if rem:
                k.dma("sp", ht.h[0:rem, nfull, 0:kn_ * 128], s["htm"][nfull * 128:T, kc0 * 128:(kc0 + kn_) * 128],
                      owner=ht, writes=[])
                ht.b.w = {ht.b.sem: k.dtotal[ht.b.sem]}

        nk2 = (KC + 1) // 2
        total = NE * nk2
        ld_ht(0)
        for e in range(NE):
            for i in range(ntile):
                rows = 128 if i < nfull else rem
                self.ts(sel, sel.h[0:rows, i, :], iota, iota.h[0:rows, :], pma.h[0:rows, i, e:e + 1], None, ALU.is_equal,
                        extra_reads=[pma])
            for kq in range(nk2):
                idx = e * nk2 + kq
                kc0 = kq * 2
                kn_ = min(2, KC - kc0)
                ht = hts[idx % 2]
                if idx + 1 < total:
                    ld_ht(idx + 1)
                for kk_ in range(kn_):
                    kc = kc0 + kk_
                    for (c0, n) in self.slot_chunks(0, CAP):
                        ps = self.ps[sti % 4]
                        for i in range(ntile):
                            rows = 128 if i < nfull else rem
                            self.mm(ps, ps.h[:, 0:n], ht.h[0:rows, i, kk_ * 128:(kk_ + 1) * 128],
                                    sel.h[0:rows, i, c0:c0 + n], [ht, sel], i == 0, i == ntile - 1)
                        t = st[sti % 4]
                        self.cp(t, t.h[:, 0:n], ps, ps.h[:, 0:n], eng=("act" if sti % 2 else "dve"))
                        sti += 1
                        self.store(s["XeT"][e * cfg.D + kc * 128:e * cfg.D + (kc + 1) * 128, c0:c0 + n], t,
                                   t.h[:, 0:n])

    def phase_moe_ffn(self, experts):
        cfg, k, P, s = self.cfg, self.k, self.P, self.s
        KC, DFF, CAP, D = cfg.KC, cfg.DFF, cfg.CAP, cfg.D
        FK = DFF // 128
        HALF = CAP // 2
        nst = HALF // 128
        X = k.sb("Xe", [128, KC, HALF], BF16)
        G_ = k.sb("G", [128, FK, HALF], BF16)
        w1b = [k.sb(f"w1_{i}", [128, KC, 256], BF16) for i in range(2)]
        w3b = [k.sb(f"w3_{i}", [128, KC, 256], BF16) for i in range(2)]
        w2b = [k.sb(f"w2_{i}", [128, FK, 256], BF16) for i in range(2)]
        Yb = k.sb("Yb", [128, nst, D], BF16)
        sa = [k.sb(f"sa{i}", [128, 512], F32) for i in range(2)]
        ctr = 0
        wctr = 0
        for ei, (W1, W3, W2) in enumerate(experts):
            W1v = W1.rearrange("(kc p) n -> p kc n", p=128)
            W3v = W3.rearrange("(kc p) n -> p kc n", p=128)
            W2v = W2.rearrange("(kc p) n -> p kc n", p=128)
            for s0 in range(0, CAP, HALF):
                chs = self.slot_chunks(0, HALF)
                self.load(X, X.h[:, :, :], self.fm(s["XeT"][ei * D:(ei + 1) * D, :])[:, :, s0:s0 + HALF])
                for j0 in range(0, FK, 2):
                    jn = min(2, FK - j0)
                    w1, w3 = w1b[wctr % 2], w3b[wctr % 2]
                    wctr += 1
                    self.load(w1, w1.h[:, :, 0:jn * 128], W1v[:, :, j0 * 128:(j0 + jn) * 128], eng="pool")
                    self.load(w3, w3.h[:, :, 0:jn * 128], W3v[:, :, j0 * 128:(j0 + jn) * 128], eng="pool")
                    for jj in range(jn):
                        j = j0 + jj
                        for (c0, n) in chs:
                            i = ctr % 2
                            ctr += 1
                            pa, pb = self.ps[2 * i], self.ps[2 * i + 1]
                            for kc in range(KC):
                                self.mm(pa, pa.h[:, 0:n], w1.h[:, kc, jj * 128:(jj + 1) * 128], X.h[:, kc, c0:c0 + n],
                                        [w1, X], kc == 0, kc == KC - 1)
                            for kc in range(KC):
                                self.mm(pb, pb.h[:, 0:n], w3.h[:, kc, jj * 128:(jj + 1) * 128], X.h[:, kc, c0:c0 + n],
                                        [w3, X], kc == 0, kc == KC - 1)
                            self.act(sa[i], sa[i].h[:, 0:n], pa, pa.h[:, 0:n], AF.Silu)
                            self.tt(G_, G_.h[:, j, c0:c0 + n], sa[i], sa[i].h[:, 0:n], pb, pb.h[:, 0:n], ALU.mult)
                for cg in range(0, D, 256):
                    cn = min(256, D - cg)
                    w2 = w2b[wctr % 2]
                    wctr += 1
                    self.load(w2, w2.h[:, :, 0:cn], W2v[:, :, cg:cg + cn], eng="pool")
                    for st_ in range(nst):
                        ps = self.ps[4 + ctr % 4]
                        ctr += 1
                        for j in range(FK):
                            self.mm(ps, ps.h[:, 0:cn], G_.h[:, j, st_ * 128:(st_ + 1) * 128], w2.h[:, j, 0:cn], [G_, w2],
                                    j == 0, j == FK - 1)
                        self.cp(Yb, Yb.h[:, st_, cg:cg + cn], ps, ps.h[:, 0:cn], eng=("act" if ctr % 2 else "dve"))
                r0 = ei * CAP + s0
                self.store(s["Yd"][r0:r0 + HALF, :].rearrange("(a p) d -> p a d", p=128), Yb, Yb.h[:, :, :])

    def phase_combine(self):
        cfg, k, P, s = self.cfg, self.k, self.P, self.s
        KC, NE, CAP, D = cfg.KC, cfg.NE, cfg.CAP, cfg.D
        NST = CAP // 128
        sid = k.sb("sid", [128, NST], F32)
        self.load(sid, sid.h[:, :], self.slotid[:, :])
        selT = k.sb("selT", [128, NE * NST, 512], BF16)
        pmb = [k.sb(f"pmb{i}", [128, 512], F32) for i in range(2)]
        gtb = [k.sb(f"gtb{i}", [128, 512], F32) for i in range(2)]
        tmpq = [k.sb(f"tmpq{i}", [128, 512], F32) for i in range(2)]
        FH = min(8, KC)
        yts = [k.sb(f"yt{i}", [128, FH * 128], BF16) for i in range(4)]
        hr = [k.sb(f"hr{i}", [128, 512], F32) for i in range(FH)]
        ot = [k.sb(f"ot{i}", [128, 512], F32) for i in range(2)]
        yi = 0
        oi = 0
        for (c0, n) in cfg.chunks:
            for e in range(NE):
                pb_, gb_ = pmb[e % 2], gtb[e % 2]
                self.load(pb_, pb_.h[:, 0:n], s["pmT"][e, c0:c0 + n].partition_broadcast(128))
                self.load(gb_, gb_.h[:, 0:n], s["gtT"][e, c0:c0 + n].partition_broadcast(128))
                for st_ in range(NST):
                    k.op("dve", lambda en, e=e, st_=st_, n=n, pb_=pb_, gb_=gb_: en.scalar_tensor_tensor(
                        selT.h[:, e * NST + st_, 0:n], pb_.h[:, 0:n], sid.h[:, st_:st_ + 1], gb_.h[:, 0:n],
                        ALU.is_equal, ALU.mult), reads=[pb_, gb_, sid], writes=[selT])
            for f0 in range(0, KC, FH):
                fn_ = min(FH, KC - f0)
                tot = NE * NST
                for f in range(fn_):
                    fr = (f0 + f) * 128
                    self.load(hr[f], hr[f].h[:, 0:n], s["hres"][fr:fr + 128, c0:c0 + n])
                for q in range(tot):
                    e, st_ = divmod(q, NST)
                    yt = yts[yi % 4]
                    yi += 1
                    r0 = e * CAP + st_ * 128
                    self.load(yt, yt.h[:, 0:fn_ * 128], s["Yd"][r0:r0 + 128, f0 * 128:(f0 + fn_) * 128])
                    for f in range(fn_):
                        ps = self.ps[f]
                        self.mm(ps, ps.h[:, 0:n], yt.h[:, f * 128:(f + 1) * 128], selT.h[:, q, 0:n], [yt, selT], q == 0,
                                q == tot - 1, inc=(q == tot - 1 or f == fn_ - 1))
                for f in range(fn_):
                    ps = self.ps[f]
                    hrt, o_ = hr[f], ot[oi % 2]
                    oi += 1
                    fr = (f0 + f) * 128
                    k.op("dve", lambda en, o_=o_, hrt=hrt, ps=ps, n=n: en.scalar_tensor_tensor(
                        o_.h[:, 0:n], hrt.h[:, 0:n], cfg.ALPHA, ps.h[:, 0:n], ALU.mult, ALU.add), reads=[hrt, ps],
                        writes=[o_])
                    self.store(s["yT"][fr:fr + 128, c0:c0 + n], o_, o_.h[:, 0:n])

    def phase_ffn(self, experts, gated):
        cfg, k, P, s = self.cfg, self.k, self.P, self.s
        KC, DFF, T = cfg.KC, cfg.DFF, cfg.T
        FK = DFF // 128
        PART = cfg.FFPART
        supers = []
        cur = []
        for (c0, n) in cfg.chunks:
            if cur and sum(x[1] for x in cur) + n > cfg.SUPER + 16:
                supers.append(cur)
                cur = []
            cur.append((c0, n))
        if cur:
            supers.append(cur)
        SW = max(sum(x[1] for x in sc) for sc in supers)
        hb = k.sb("hbS", [128, KC, SW], BF16)
        G_ = k.sb("G", [128, PART, SW], BF16)
        acc = k.sb("acc", [128, KC, SW], F32)
        w1b = [k.sb(f"w1_{i}", [128, KC, 256], BF16) for i in range(2)]
        w3b = [k.sb(f"w3_{i}", [128, KC, 256], BF16) for i in range(2)]
        w2b = [k.sb(f"w2_{i}", [128, PART, 256], BF16) for i in range(2)]
        sa = [k.sb(f"sa{i}", [128, 512], F32) for i in range(2)]
        gbs = [k.sb(f"gb{i}", [128, 512], F32) for i in range(2)]
        hr = [k.sb(f"hr{i}", [128, 512], F32) for i in range(2)]
        ctr = [0]
        wctr = [0]
        for sc in supers:
            s0 = sc[0][0]
            sw = sum(x[1] for x in sc)
            self.load(hb, hb.h[:, :, 0:sw], self.fm(s["hb"])[:, :, s0:s0 + sw])
            first_acc = True
            for ei, (W1, W3, W2) in enumerate(experts):
                W1v = W1.rearrange("(kc p) n -> p kc n", p=128)
                W3v = W3.rearrange("(kc p) n -> p kc n", p=128)
                W2v = W2.rearrange("(kc p) n -> p kc n", p=128)
                for p0 in range(0, FK, PART):
                    pn = min(PART, FK - p0)
                    for j0 in range(0, pn, 2):
                        jn = min(2, pn - j0)
                        wi = wctr[0] % 2
                        wctr[0] += 1
                        w1, w3 = w1b[wi], w3b[wi]
                        col = (p0 + j0) * 128
                        self.load(w1, w1.h[:, :, 0:jn * 128], W1v[:, :, col:col + jn * 128], eng="pool")
                        self.load(w3, w3.h[:, :, 0:jn * 128], W3v[:, :, col:col + jn * 128], eng="pool")
                        for jj in range(jn):
                            j = j0 + jj
                            for (c0, n) in sc:
                                lo = c0 - s0
                                i = ctr[0] % 2
                                ctr[0] += 1
                                pa, pb = self.ps[2 * i], self.ps[2 * i + 1]
                                for kc in range(KC):
                                    self.mm(pa, pa.h[:, 0:n], w1.h[:, kc, jj * 128:(jj + 1) * 128],
                                            hb.h[:, kc, lo:lo + n], [w1, hb], kc == 0, kc == KC - 1)
                                for kc in range(KC):
                                    self.mm(pb, pb.h[:, 0:n], w3.h[:, kc, jj * 128:(jj + 1) * 128],
                                            hb.h[:, kc, lo:lo + n], [w3, hb], kc == 0, kc == KC - 1)
                                self.act(sa[i], sa[i].h[:, 0:n], pa, pa.h[:, 0:n], AF.Silu)
                                if gated:
                                    gbt = gbs[i]
                                    self.load(gbt, gbt.h[:, 0:n], s["gtT"][ei, c0:c0 + n].partition_broadcast(128))
                                    self.tt(sa[i], sa[i].h[:, 0:n], sa[i], sa[i].h[:, 0:n], gbt, gbt.h[:, 0:n],
                                            ALU.mult)
                                self.tt(G_, G_.h[:, j, lo:lo + n], sa[i], sa[i].h[:, 0:n], pb, pb.h[:, 0:n], ALU.mult)
                    for f0 in range(0, KC, 2):
                        fn_ = min(2, KC - f0)
                        wi = wctr[0] % 2
                        wctr[0] += 1
                        w2 = w2b[wi]
                        self.load(w2, w2.h[:, 0:pn, 0:fn_ * 128], W2v[:, p0:p0 + pn, f0 * 128:(f0 + fn_) * 128],
                                  eng="pool")
                        for ff in range(fn_):
                            f = f0 + ff
                            for (c0, n) in sc:
                                lo = c0 - s0
                                ps = self.ps[4 + ctr[0] % 4]
                                ctr[0] += 1
                                for j in range(pn):
                                    self.mm(ps, ps.h[:, 0:n], w2.h[:, j, ff * 128:(ff + 1) * 128], G_.h[:, j, lo:lo + n],
                                            [w2, G_], j == 0, j == pn - 1)
                                if first_acc and p0 == 0:
                                    hi = ctr[0] % 2
                                    hrt = hr[hi]
                                    self.load(hrt, hrt.h[:, 0:n], s["hres"][f * 128:(f + 1) * 128, c0:c0 + n])
                                    k.op("dve", lambda e, f=f, lo=lo, n=n, ps=ps, hrt=hrt: e.scalar_tensor_tensor(
                                        acc.h[:, f, lo:lo + n], hrt.h[:, 0:n], cfg.ALPHA, ps.h[:, 0:n], ALU.mult,
                                        ALU.add), reads=[hrt, ps], writes=[acc])
                                else:
                                    self.tt(acc, acc.h[:, f, lo:lo + n], acc, acc.h[:, f, lo:lo + n], ps, ps.h[:, 0:n],
                                            ALU.add)
                first_acc = False
            self.store(self.fm(s["yT"])[:, :, s0:s0 + sw], acc, acc.h[:, :, 0:sw])


_BUCKET_STARTS = [8, 12, 16, 23, 32, 46, 64, 91]


def _bucket(rel):
    rel = np.asarray(rel)
    n = np.abs(rel)
    b = np.where(n < 8, n, 8 + sum((n >= t).astype(np.int64) for t in _BUCKET_STARTS[1:]))
    return np.where(rel > 0, 16, 0) + b


def _host_tables(cfg, rel_bias):
    T, G = cfg.T, cfg.GRP
    pos = np.concatenate([cfg.NMETA + np.arange(cfg.SEQ), np.arange(cfg.NMETA)]).astype(np.float32)
    inv = (np.float32(10000.0) ** (-np.arange(0, cfg.ROPE, 2, dtype=np.float32) / np.float32(cfg.ROPE))).astype(np.float32)
    ang = pos[None, :] * inv[:, None]
    cos = np.cos(ang).astype(np.float32)
    sin = np.sin(ang).astype(np.float32)
    ropet = np.stack([np.concatenate([cos, cos], 0), np.concatenate([-sin, sin], 0)]).astype(np.float32)
    rb = np.asarray(rel_bias, np.float32)

    def tab(rel, masked, heads):
        bk = _bucket(rel)
        outs = []
        for h in heads:
            t = rb[bk, h]
            t = np.where(masked, np.float32(NEGV), t)
            outs.append(t)
        return np.concatenate(outs, axis=1).astype(np.float32)

    kk = np.arange(128)[:, None]
    qq = np.arange(128)[None, :]
    biasd = np.zeros((cfg.SKV, 3, 128, G * 128), np.float32)
    biasmk = np.zeros((cfg.SKV, 2, 16, G * 128), np.float32)
    biasqm = np.zeros((cfg.SKV, 144, G * 16), np.float32)
    mj = np.arange(16)[:, None]
    for hk in range(cfg.SKV):
        heads = [hk * G + g for g in range(G)]
        for d in (-1, 0, 1):
            rel = d * 128 + kk - qq
            biasd[hk, d + 1] = tab(rel, np.abs(rel) > 128, heads)
        rel0 = mj - (16 + qq)
        biasmk[hk, 0] = tab(rel0, np.zeros_like(rel0, bool), heads)
        rel1 = mj - (16 + 128 + qq)
        biasmk[hk, 1] = tab(rel1, np.zeros_like(rel1, bool), heads)
        q16 = np.arange(16)[None, :]
        relm = mj - q16
        biasqm[hk, 0:16] = tab(relm, np.zeros_like(relm, bool), heads)
        relb = (16 + kk) - q16
        biasqm[hk, 16:144] = tab(relb, relb > 128, heads)
    return ropet, biasd, biasmk, biasqm


def _fmcol(v):
    v = np.asarray(v, np.float32)
    return np.ascontiguousarray(v.reshape(-1, 128).T)


def prepare_inputs(cfg, inp):
    L = cfg.DEPTH
    ropet, biasd, biasmk, biasqm = _host_tables(cfg, inp["rel_bias"])
    consts = np.zeros((128, 384), np.float32)
    consts[:, 0:128] = np.eye(128, dtype=np.float32)
    consts[:, 128:256] = 1.0
    consts[:, 256:384] = np.triu(np.ones((128, 128), np.float32), 1)
    iotac = np.ascontiguousarray(np.broadcast_to(np.arange(cfg.CAP, dtype=np.float32)[None, :], (128, cfg.CAP)))
    slotid = (np.arange(cfg.CAP // 128, dtype=np.float32)[None, :] * 128 + np.arange(128, dtype=np.float32)[:, None])
    slotid = np.ascontiguousarray(slotid.astype(np.float32))
    cols = [_fmcol(inp["emb_ln_g"]), _fmcol(inp["emb_ln_b"])]
    for l in range(L):
        cols += [_fmcol(inp["ln_mix_g"][l]), _fmcol(inp["ln_mix_b"][l]), _fmcol(inp["ln_ffn_g"][l]),
                 _fmcol(inp["ln_ffn_b"][l]), _fmcol(inp["q_norm_g"][l]), _fmcol(inp["kv_norm_g"][l])]
    pvec = np.ascontiguousarray(np.concatenate(cols, axis=1))
    w_in = np.asarray(inp["w_in"], np.float32)
    ro = cfg.QR + cfg.KVR
    half = cfg.ROPE // 2
    rot = np.concatenate([w_in[:, :, ro + half:ro + cfg.ROPE], w_in[:, :, ro:ro + half]], axis=2)
    w_in_e = np.ascontiguousarray(np.concatenate([w_in, rot], axis=2))
    w_uq = np.asarray(inp["w_uq"], np.float32).reshape(L, cfg.QR, cfg.MH, cfg.NOPE + cfg.ROPE)
    qn = w_uq[..., :cfg.NOPE].reshape(L, cfg.QR, -1)
    qr = w_uq[..., cfg.NOPE:]
    qrot = np.concatenate([qr[..., half:], qr[..., :half]], axis=-1)
    qrr = np.concatenate([qr, qrot], axis=-1).reshape(L, cfg.QR, -1)
    w_uq_e = np.ascontiguousarray(np.concatenate([qn, qrr], axis=2))
    w_ukv = np.asarray(inp["w_ukv"], np.float32).reshape(L, cfg.KVR, cfg.MH, cfg.NOPE + cfg.VD)
    w_ukv_e = np.ascontiguousarray(np.concatenate([w_ukv[..., :cfg.NOPE].reshape(L, cfg.KVR, -1),
                                                   w_ukv[..., cfg.NOPE:].reshape(L, cfg.KVR, -1)], axis=2))
    sinkb = np.ascontiguousarray(np.broadcast_to(np.asarray(inp["sink_logits"], np.float32)[:, None, :],
                                                 (L, 128, cfg.SH)))
    shared = dict(meta=np.asarray(inp["meta_tokens"], np.float32), consts=consts, iotac=iotac, slotid=slotid,
                  pvec=pvec, ropet=ropet,
                  biasd=biasd, biasmk=biasmk, biasqm=biasqm, sinkb=sinkb, w_in=w_in_e, w_uq=w_uq_e, w_ukv=w_ukv_e,
                  w_pa=np.asarray(inp["w_proj_a"], np.float32), w_pb=np.asarray(inp["w_proj_b"], np.float32),
                  w_out=np.asarray(inp["w_out"], np.float32), ffn_w1=np.asarray(inp["ffn_w1"], np.float32),
                  ffn_w3=np.asarray(inp["ffn_w3"], np.float32), ffn_w2=np.asarray(inp["ffn_w2"], np.float32))
    if L // 2:
        nm = L // 2
        shared["router_w"] = np.asarray(inp["router_w"], np.float32)
        shared["router_bb"] = np.ascontiguousarray(np.broadcast_to(np.asarray(inp["router_b"], np.float32)[:, None, :],
                                                                   (nm, 128, cfg.NE)))
        shared["moe_w1"] = np.asarray(inp["moe_w1"], np.float32).reshape(nm * cfg.NE, cfg.D, cfg.DFF)
        shared["moe_w3"] = np.asarray(inp["moe_w3"], np.float32).reshape(nm * cfg.NE, cfg.D, cfg.DFF)
        shared["moe_w2"] = np.asarray(inp["moe_w2"], np.float32).reshape(nm * cfg.NE, cfg.DFF, cfg.D)
    return shared


def run(cfg, inp, debug=None, stop_after=None, trace=False):
    prog = Prog(cfg, debug=debug, stop_after=stop_after)
    nc = prog.build()
    shared = prepare_inputs(cfg, inp)
    x = np.asarray(inp["x"], np.float32)
    in_maps = []
    for c in range(cfg.NCORES):
        m = dict(shared)
        m["x"] = np.ascontiguousarray(x[c])
        in_maps.append(m)
    res = run_bass_kernel_spmd(nc, in_maps, core_ids=list(range(cfg.NCORES)), trace=trace)
    return res, prog


def kernel(**inputs):
    cfg = Cfg()
    res, _ = run(cfg, inputs)
    return np.stack([np.asarray(r["out"], np.float32) for r in res.results], axis=0)
```

```python
import math
import os
from contextlib import ExitStack
import numpy as np
import concourse.bass as bass
import concourse.mybir as mybir
from concourse.bass_utils import run_bass_kernel_spmd

F32 = mybir.dt.float32
BF16 = mybir.dt.bfloat16
ALU = mybir.AluOpType
AF = mybir.ActivationFunctionType
AX = mybir.AxisListType

NEGV = -1.0e30


class Cfg:
    def __init__(self, **kw):
        self.D = 2048
        self.SEQ = 4096
        self.DEPTH = 2
        self.NMETA = 16
        self.MH = 16
        self.NOPE = 128
        self.ROPE = 64
        self.VD = 128
        self.QR = 768
        self.KVR = 512
        self.SH = 16
        self.SKV = 4
        self.HD = 128
        self.DFF = 5632
        self.NE = 8
        self.NCORES = 8
        self.SUPER = 1024
        self.FFPART = 11
        self.CAP = 1280
        self.HALVES = None
        for k, v in kw.items():
            setattr(self, k, v)
        self.T = self.SEQ + self.NMETA
        if self.HALVES is None:
            h0 = min(self.CAP, ((self.CAP // 2 + 255) // 256) * 256)
            self.HALVES = [(0, h0)] + ([(h0, self.CAP)] if h0 < self.CAP else [])
        self.NB = self.SEQ // 128
        self.KC = self.D // 128
        self.GRP = self.SH // self.SKV
        self.chunks = [(c0, 512) for c0 in range(0, self.SEQ, 512)] + [(self.SEQ, self.NMETA)]
        self.IN_DIM = self.QR + self.KVR + self.ROPE + self.SH * self.HD + 2 * self.SKV * self.HD + 2 * self.D
        self.LN_EPS = 1e-5
        self.RMS_EPS = 1e-6
        self.ALPHA = (2 * self.DEPTH) ** 0.25


class Buf:
    __slots__ = ("name", "w", "r", "sem", "cnt")

    def __init__(self, name):
        self.name = name
        self.w = {}
        self.r = {}
        self.sem = None
        self.cnt = 0


class Tl:
    __slots__ = ("b", "h")

    def __init__(self, b, h):
        self.b = b
        self.h = h


ENGS = ("pe", "act", "dve", "pool", "sp")


class KB:
    def __init__(self, nc, stack, sb_limit):
        self.nc = nc
        self.stack = stack
        self.q = {e: [] for e in ENGS}
        self.cnt = {e: 0 for e in ENGS}
        self.clock = {e: {} for e in ENGS}
        self.semh = {}
        self.esem = {}
        for e in ENGS:
            self.esem[e] = self.newsem("E_" + e)
        self.dfree = []
        self.dtotal = {}
        self.dall = []
        self.phase_bufs = []
        self.sb_base = 16512
        self.sb_off = 16512
        self.sb_limit = sb_limit
        self.uid = 0
        self.ninst = 0

    def newsem(self, name):
        h = self.stack.enter_context(self.nc.semaphore(name))
        self.semh[name] = h
        return name

    def sb(self, name, shape, dtype, persistent=False):
        esz = 4 if dtype == F32 else 2
        n = 1
        for s in shape[1:]:
            n *= s
        nbytes = (n * esz + 63) // 64 * 64
        self.uid += 1
        off = self.sb_off
        assert off + nbytes <= self.sb_limit, f"SBUF overflow allocating {name}: {off}+{nbytes}"
        h = self.nc.alloc_sbuf_tensor_at(f"{name}_{self.uid}", list(shape), dtype, offset=off)
        self.sb_off = off + nbytes
        if persistent:
            assert self.sb_base == off, "persistent allocs must come first"
            self.sb_base = self.sb_off
        b = Buf(name)
        if not persistent:
            self.phase_bufs.append(b)
        return Tl(b, h)

    def _deps(self, eng, reads, writes):
        mysem = self.esem[eng]
        ck = self.clock[eng]
        waits = {}

        def need(sem, val):
            if ck.get(sem, 0) >= val:
                return
            ck[sem] = val
            waits[sem] = val

        for t in reads:
            for sem, val in t.b.w.items():
                if sem == mysem and eng == "pe":
                    continue
                need(sem, val)
        for t in writes:
            for sem, val in t.b.w.items():
                if sem == mysem:
                    continue
                need(sem, val)
            for sem, val in t.b.r.items():
                if sem == mysem:
                    continue
                need(sem, val)
        return list(waits.items())

    def _mark(self, ev, reads, writes):
        sem, val = ev
        for t in reads:
            if t.b.r.get(sem, 0) < val:
                t.b.r[sem] = val
        for t in writes:
            t.b.w = {sem: val}
            t.b.r = {}

    def op(self, eng, fn, reads=(), writes=(), inc=True):
        waits = self._deps(eng, reads, writes)
        idx = self.cnt[eng] + 1
        if inc:
            self.cnt[eng] = idx
        ev = (self.esem[eng], idx)
        self.q[eng].append((waits, fn, (self.esem[eng], 1) if inc else None))
        self._mark(ev, reads, writes)
        self.ninst += 1
        return ev

    def dma(self, eng, out, in_, owner, reads=(), writes=(), **kw):
        b = owner.b
        if b.sem is None:
            if self.dfree:
                b.sem = self.dfree.pop()
            else:
                b.sem = self.newsem(f"D{len(self.dall)}")
                self.dall.append(b.sem)
                self.dtotal[b.sem] = 0
        waits = self._deps(eng, reads, writes)
        self.dtotal[b.sem] += 16
        ev = (b.sem, self.dtotal[b.sem])
        self.q[eng].append((waits, (lambda e: e.dma_start(out=out, in_=in_, **kw)), (b.sem, 16)))
        self._mark(ev, reads, writes)
        self.ninst += 1
        return ev

    def barrier(self):
        for e in ENGS:
            waits = []
            ck = self.clock[e]
            for e2 in ENGS:
                if e2 == e or self.cnt[e2] == 0:
                    continue
                s = self.esem[e2]
                if ck.get(s, 0) < self.cnt[e2]:
                    ck[s] = self.cnt[e2]
                    waits.append((s, self.cnt[e2]))
            for s in self.dall:
                v = self.dtotal[s]
                if v > 0 and ck.get(s, 0) < v:
                    ck[s] = v
                    waits.append((s, v))
            if waits:
                self.q[e].append((waits, None, None))
        for b in self.phase_bufs:
            if b.sem is not None:
                self.dfree.append(b.sem)
                b.sem = None
        self.phase_bufs = []
        self.sb_off = self.sb_base

    def emit(self):
        nc = self.nc
        engmap = {"pe": "tensor", "act": "scalar", "dve": "vector", "pool": "gpsimd", "sp": "sync"}
        with nc.Block() as block:
            for e in ENGS:
                q = self.q[e]
                semh = self.semh

                def body(eh, q=q):
                    for waits, fn, inc in q:
                        for s, v in waits:
                            eh.wait_ge(semh[s], v)
                        if fn is not None:
                            ins = fn(eh)
                            if inc is not None:
                                ins.then_inc(semh[inc[0]], inc[1])

                getattr(block, engmap[e])(body)


class Prog:
    def __init__(self, cfg, debug=None, stop_after=None):
        self.cfg = cfg
        self.debug = debug or []
        self.stop_after = stop_after
        self.nc = bass.Bass("TRN2", target_bir_lowering=False)
        self.stack = ExitStack()
        self.k = KB(self.nc, self.stack, sb_limit=229376 - 256)
        self.dr = {}

    def din(self, name, shape, dtype=F32):
        t = self.nc.dram_tensor(name, list(shape), dtype, kind="ExternalInput").ap()
        self.dr[name] = t
        return t

    def dscr(self, name, shape, dtype):
        kind = "ExternalOutput" if name in self.debug else "Internal"
        t = self.nc.dram_tensor(name, list(shape), dtype, kind=kind).ap()
        self.dr[name] = t
        return t

    def fm(self, ap):
        return ap.rearrange("(kc p) t -> p kc t", p=128)

    def mm(self, ps, ps_ap, lhsT_ap, rhs_ap, reads, start, stop, inc=None):
        self.k.op("pe", lambda e: e.matmul(ps_ap, lhsT_ap, rhs_ap, start=start, stop=stop),
                  reads=reads, writes=[ps], inc=(stop if inc is None else inc))

    def act(self, out_t, out_ap, in_t, in_ap, func, extra_reads=(), **kw):
        self.k.op("act", lambda e: e.activation(out_ap, in_ap, func, **kw),
                  reads=[in_t, *extra_reads], writes=[out_t])

    def tt(self, out_t, out_ap, a_t, a_ap, b_t, b_ap, op, eng="dve"):
        self.k.op(eng, lambda e: e.tensor_tensor(out_ap, a_ap, b_ap, op), reads=[a_t, b_t], writes=[out_t])

    def ts(self, out_t, out_ap, a_t, a_ap, s1, s2, op0, op1=None, extra_reads=(), eng="dve"):
        if op1 is None:
            self.k.op(eng, lambda e: e.tensor_scalar(out_ap, a_ap, s1, None, op0),
                      reads=[a_t, *extra_reads], writes=[out_t])
        else:
            self.k.op(eng, lambda e: e.tensor_scalar(out_ap, a_ap, s1, s2, op0, op1),
                      reads=[a_t, *extra_reads], writes=[out_t])

    def cp(self, out_t, out_ap, in_t, in_ap, eng="dve"):
        if eng == "act":
            self.k.op("act", lambda e: e.copy(out_ap, in_ap), reads=[in_t], writes=[out_t])
        else:
            self.k.op(eng, lambda e: e.tensor_copy(out_ap, in_ap), reads=[in_t], writes=[out_t])

    def load(self, dst_t, dst_ap, src_ap, eng="sp"):
        self.k.dma(eng, dst_ap, src_ap, owner=dst_t, writes=[dst_t])

    def store(self, dst_ap, src_t, src_ap, eng="sp"):
        self.k.dma(eng, dst_ap, src_ap, owner=src_t, reads=[src_t])

    def build(self):
        cfg = self.cfg
        k = self.k
        nc = self.nc
        D, T, KC = cfg.D, cfg.T, cfg.KC
        L = cfg.DEPTH
        x = self.din("x", [cfg.SEQ, D])
        meta = self.din("meta", [cfg.NMETA, D])
        consts = self.din("consts", [128, 3 * 128])
        self.iotac = self.din("iotac", [128, cfg.CAP])
        self.slotid = self.din("slotid", [128, cfg.CAP // 128])
        pvec = self.din("pvec", [128, self.npvec()])
        ropet = self.din("ropet", [2, 64, T])
        biasd = self.din("biasd", [cfg.SKV, 3, 128, cfg.GRP * 128])
        biasmk = self.din("biasmk", [cfg.SKV, 2, 16, cfg.GRP * 128])
        biasqm = self.din("biasqm", [cfg.SKV, 144, cfg.GRP * 16])
        sinkb = self.din("sinkb", [L, 128, cfg.SH])
        w_in = self.din("w_in", [L, D, cfg.IN_DIM + cfg.ROPE])
        w_uq = self.din("w_uq", [L, cfg.QR, cfg.MH * (cfg.NOPE + 2 * cfg.ROPE)])
        w_ukv = self.din("w_ukv", [L, cfg.KVR, cfg.MH * (cfg.NOPE + cfg.VD)])
        w_pa = self.din("w_pa", [L, cfg.MH * cfg.VD, D])
        w_pb = self.din("w_pb", [L, cfg.SH * cfg.HD, D])
        w_out = self.din("w_out", [L, D, D])
        n_dense = (L + 1) // 2
        n_moe = L // 2
        ffn_w1 = self.din("ffn_w1", [n_dense, D, cfg.DFF])
        ffn_w3 = self.din("ffn_w3", [n_dense, D, cfg.DFF])
        ffn_w2 = self.din("ffn_w2", [n_dense, cfg.DFF, D])
        if n_moe:
            router_w = self.din("router_w", [n_moe, D, cfg.NE])
            router_bb = self.din("router_bb", [n_moe, 128, cfg.NE])
            moe_w1 = self.din("moe_w1", [n_moe * cfg.NE, D, cfg.DFF])
            moe_w3 = self.din("moe_w3", [n_moe * cfg.NE, D, cfg.DFF])
            moe_w2 = self.din("moe_w2", [n_moe * cfg.NE, cfg.DFF, D])
        out = self.nc.dram_tensor("out", [cfg.SEQ, D], F32, kind="ExternalOutput").ap()
        s = self.s = {}
        s["hres"] = self.dscr("hres", [D, T], F32)
        s["hb"] = self.dscr("hb", [D, T], BF16)
        s["yT"] = self.dscr("yT", [D, T], F32)
        s["cqT"] = self.dscr("cqT", [cfg.QR, T], BF16)
        s["ckvT"] = self.dscr("ckvT", [cfg.KVR, T], BF16)
        s["krT"] = self.dscr("krT", [cfg.ROPE, T], BF16)
        s["qsT"] = self.dscr("qsT", [cfg.SH * cfg.HD, T], BF16)
        s["ksT"] = self.dscr("ksT", [cfg.SKV * cfg.HD, T], BF16)
        s["vs"] = self.dscr("vs", [T, cfg.SKV * cfg.HD], BF16)
        s["gaT"] = self.dscr("gaT", [D, T], BF16)
        s["gbT"] = self.dscr("gbT", [D, T], BF16)
        s["qnT"] = self.dscr("qnT", [cfg.MH * cfg.NOPE, T], BF16)
        s["qrT"] = self.dscr("qrT", [cfg.MH * cfg.ROPE, T], BF16)
        s["knT"] = self.dscr("knT", [cfg.MH * cfg.NOPE, T], BF16)
        s["vm"] = self.dscr("vm", [T, cfg.MH * cfg.VD], BF16)
        s["oaT"] = self.dscr("oaT", [cfg.MH * cfg.VD, T], BF16)
        s["obT"] = self.dscr("obT", [cfg.SH * cfg.HD, T], BF16)
        s["tAT"] = self.dscr("tAT", [D, T], F32)
        s["mgT"] = self.dscr("mgT", [D, T], BF16)
        s["gtT"] = self.dscr("gtT", [max(cfg.NE, 1), T], F32)
        s["pmT"] = self.dscr("pmT", [cfg.NE, T], F32)
        s["pmtm"] = self.dscr("pmtm", [T, cfg.NE], F32)
        s["htm"] = self.dscr("htm", [T, D], BF16)
        s["XeT"] = self.dscr("XeT", [cfg.NE * D, cfg.CAP], BF16)
        s["Yd"] = self.dscr("Yd", [cfg.NE * cfg.CAP, D], BF16)

        P = self.P = {}
        P["ident"] = k.sb("ident", [128, 128], F32, persistent=True)
        P["ones"] = k.sb("ones", [128, 128], F32, persistent=True)
        P["identb"] = k.sb("identb", [128, 128], BF16, persistent=True)
        P["onesb"] = k.sb("onesb", [128, 128], BF16, persistent=True)
        P["pvec"] = k.sb("pvec", [128, self.npvec()], F32, persistent=True)
        P["upper"] = k.sb("upper", [128, 128], F32, persistent=True)
        self.ps = [Tl(Buf(f"ps{i}"), nc.alloc_psum_tensor(f"ps{i}", [128, 512], F32)) for i in range(8)]
        self.load(P["ident"], P["ident"].h[:, :], consts[:, 0:128])
        self.load(P["ones"], P["ones"].h[:, :], consts[:, 128:256])
        self.load(P["identb"], P["identb"].h[:, :], consts[:, 0:128], eng="pool")
        self.load(P["onesb"], P["onesb"].h[:, :], consts[:, 128:256], eng="pool")
        self.load(P["pvec"], P["pvec"].h[:, :], pvec[:, :])
        self.load(P["upper"], P["upper"].h[:, :], consts[:, 256:384])
        self.ropet = ropet

        pv = self.pvoff()
        self.phase_embed(x, meta, pv["emb_g"], pv["emb_b"])
        k.barrier()
        done = self.stop_after == "embed"
        for l in range(L):
            if done:
                break
            self.phase_inproj(l, w_in[l])
            k.barrier()
            if self.stop_after == f"inproj{l}":
                break
            self.phase_mla_up(l, w_uq[l], w_ukv[l], pv[f"qn_g{l}"], pv[f"kvn_g{l}"])
            k.barrier()
            if self.stop_after == f"mlaup{l}":
                break
            self.phase_mla_attn(l)
            k.barrier()
            if self.stop_after == f"mlaattn{l}":
                break
            self.phase_swa(l, biasd, biasmk, biasqm, sinkb[l])
            k.barrier()
            if self.stop_after == f"swa{l}":
                break
            self.phase_proj(l, w_pa[l], w_pb[l], w_out[l])
            k.barrier()
            self.phase_ln(pv[f"mix_g{l}"], pv[f"mix_b{l}"])
            k.barrier()
            if self.stop_after == f"mix{l}":
                break
            if l % 2 == 0:
                self.phase_ffn([(ffn_w1[l // 2], ffn_w3[l // 2], ffn_w2[l // 2])], gated=False)
            else:
                self.phase_router(router_w[l // 2], router_bb[l // 2])
                k.barrier()
                if self.stop_after == f"router{l}":
                    break
                m = l // 2
                self.phase_gather()
                k.barrier()
                if self.stop_after == f"gather{l}":
                    break
                self.phase_moe_ffn([(moe_w1[m * cfg.NE + e], moe_w3[m * cfg.NE + e], moe_w2[m * cfg.NE + e])
                                    for e in range(cfg.NE)])
                k.barrier()
                if self.stop_after == f"moeffn{l}":
                    break
                self.phase_combine()
                if self.stop_after == f"combine{l}":
                    k.barrier()
                    break
            k.barrier()
            self.phase_ln(pv[f"ffn_g{l}"], pv[f"ffn_b{l}"])
            k.barrier()
        self.phase_output(out)
        k.barrier()
        k.emit()
        self.stack.close()
        return nc

    def pv_layout(self):
        cfg = self.cfg
        items = [("emb_g", cfg.KC), ("emb_b", cfg.KC)]
        for l in range(cfg.DEPTH):
            items += [(f"mix_g{l}", cfg.KC), (f"mix_b{l}", cfg.KC), (f"ffn_g{l}", cfg.KC), (f"ffn_b{l}", cfg.KC),
                      (f"qn_g{l}", cfg.QR // 128), (f"kvn_g{l}", cfg.KVR // 128)]
        return items

    def npvec(self):
        return sum(n for _, n in self.pv_layout())

    def pvoff(self):
        o = {}
        off = 0
        for name, n in self.pv_layout():
            o[name] = (off, n)
            off += n
        return o

    def ln_fm(self, y, n, gsl, bsl, c0, tmp):
        cfg, k, P = self.cfg, self.k, self.P
        KC, D = cfg.KC, cfg.D
        sq, mean, rstd, mr, msq, ob = tmp[:6]
        sbk = tmp[6] if len(tmp) > 6 else 6
        yv = y.h[:, :, 0:n]
        self.act(sq, sq.h[:, :, 0:n], y, yv, AF.Square)
        ps_s, ps_q = self.ps[sbk], self.ps[sbk + 1]
        for kc in range(KC):
            self.mm(ps_s, ps_s.h[:, 0:n], P["ones"].h[:, :], y.h[:, kc, 0:n], [P["ones"], y], kc == 0, kc == KC - 1)
        for kc in range(KC):
            self.mm(ps_q, ps_q.h[:, 0:n], P["ones"].h[:, :], sq.h[:, kc, 0:n], [P["ones"], sq], kc == 0, kc == KC - 1)
        self.ts(mean, mean.h[:, 0:n], ps_s, ps_s.h[:, 0:n], 1.0 / D, None, ALU.mult)
        self.tt(msq, msq.h[:, 0:n], mean, mean.h[:, 0:n], mean, mean.h[:, 0:n], ALU.mult)
        k.op("dve", lambda e: e.scalar_tensor_tensor(rstd.h[:, 0:n], ps_q.h[:, 0:n], 1.0 / D, msq.h[:, 0:n],
                                                     ALU.mult, ALU.subtract),
             reads=[ps_q, msq], writes=[rstd])
        self.ts(rstd, rstd.h[:, 0:n], rstd, rstd.h[:, 0:n], cfg.LN_EPS, None, ALU.add)
        self.act(rstd, rstd.h[:, 0:n], rstd, rstd.h[:, 0:n], AF.Sqrt)
        k.op('dve', lambda e: e.reciprocal(rstd.h[:, 0:n], rstd.h[:, 0:n]), reads=[rstd], writes=[rstd])
        self.tt(mr, mr.h[:, 0:n], mean, mean.h[:, 0:n], rstd, rstd.h[:, 0:n], ALU.mult)
        rb = rstd.h[:, 0:n].unsqueeze(1).to_broadcast([128, KC, n])
        mb = mr.h[:, 0:n].unsqueeze(1).to_broadcast([128, KC, n])
        self.tt(y, yv, y, yv, rstd, rb, ALU.mult)
        self.tt(y, yv, y, yv, mr, mb, ALU.subtract)
        pvh = P["pvec"]
        gb = pvh.h[:, gsl[0]:gsl[0] + KC].unsqueeze(2).to_broadcast([128, KC, n])
        bb = pvh.h[:, bsl[0]:bsl[0] + KC].unsqueeze(2).to_broadcast([128, KC, n])
        self.tt(y, yv, y, yv, pvh, gb, ALU.mult)
        self.tt(y, yv, y, yv, pvh, bb, ALU.add)
        self.cp(ob, ob.h[:, :, 0:n], y, yv, eng="act")
        self.store(self.fm(self.s["hres"])[:, :, c0:c0 + n], y, yv)
        self.store(self.fm(self.s["hb"])[:, :, c0:c0 + n], ob, ob.h[:, :, 0:n])

    def ln_tmps(self):
        k, KC = self.k, self.cfg.KC
        return (k.sb("sq", [128, KC, 512], F32), k.sb("mean", [128, 512], F32), k.sb("rstd", [128, 512], F32),
                k.sb("mr", [128, 512], F32), k.sb("msq", [128, 512], F32), k.sb("ob", [128, KC, 512], BF16))

    def phase_embed(self, x, meta, gsl, bsl):
        cfg, k, P = self.cfg, self.k, self.P
        KC = cfg.KC
        xin = [k.sb(f"xin{i}", [128, 4, cfg.D], F32) for i in range(1)]
        ys = [k.sb(f"y{i}", [128, KC, 512], F32) for i in range(2)]
        tmp = self.ln_tmps()
        def ldx(ci):
            c0, n = cfg.chunks[ci]
            xt = xin[0]
            if n == 512:
                self.load(xt, xt.h[:, :, :], x[c0:c0 + 512, :].rearrange("(a p) d -> p a d", p=128))
            else:
                self.load(xt, xt.h[0:n, 0, :], meta[:, :])

        ldx(0)
        for ci, (c0, n) in enumerate(cfg.chunks):
            xt = xin[0]
            y = ys[ci % 2]
            if n == 512:
                nt, rows = 4, 128
            else:
                nt, rows = 1, n
            for kc in range(KC):
                ps = self.ps[kc % 4]
                for a in range(nt):
                    self.mm(ps, ps.h[:, a * 128:a * 128 + rows], xt.h[0:rows, a, kc * 128:(kc + 1) * 128],
                            P["ident"].h[0:rows, 0:rows], [xt, P["ident"]], True, True)
                self.cp(y, y.h[:, kc, 0:n], ps, ps.h[:, 0:n], eng=("act" if kc % 2 else "dve"))
            if ci + 1 < len(cfg.chunks):
                ldx(ci + 1)
            self.ln_fm(y, n, gsl, bsl, c0, tmp)

    def phase_ln(self, gsl, bsl):
        cfg, k = self.cfg, self.k
        KC = cfg.KC
        ys = [k.sb(f"y{i}", [128, KC, 512], F32) for i in range(2)]
        tmps = [self.ln_tmps() + (4,), self.ln_tmps() + (6,)]
        def ld(ci):
            c0, n = cfg.chunks[ci]
            y = ys[ci % 2]
            self.load(y, y.h[:, :, 0:n], self.fm(self.s["yT"])[:, :, c0:c0 + n])

        ld(0)
        for ci, (c0, n) in enumerate(cfg.chunks):
            if ci + 1 < len(cfg.chunks):
                ld(ci + 1)
            self.ln_fm(ys[ci % 2], n, gsl, bsl, c0, tmps[ci % 2])

    def phase_output(self, out):
        cfg, k, P = self.cfg, self.k, self.P
        KC = cfg.KC
        hs = [k.sb(f"h{i}", [128, KC, 512], F32) for i in range(2)]
        os_ = [k.sb(f"o{i}", [128, cfg.D], F32) for i in range(2)]
        cnt = 0

        def ldo(ci):
            c0, n = cfg.chunks[ci]
            if n == 512:
                self.load(hs[ci % 2], hs[ci % 2].h[:, :, :], self.fm(self.s["hres"])[:, :, c0:c0 + n])

        ldo(0)
        for ci, (c0, n) in enumerate(cfg.chunks):
            if n != 512:
                continue
            h = hs[ci % 2]
            if ci + 1 < len(cfg.chunks):
                ldo(ci + 1)
            for a in range(4):
                o = os_[cnt % 2]
                cnt += 1
                for g in range(0, KC, 4):
                    ps = self.ps[(g // 4) % 4]
                    for j in range(min(4, KC - g)):
                        self.mm(ps, ps.h[:, j * 128:(j + 1) * 128], h.h[:, g + j, a * 128:(a + 1) * 128],
                                P["ident"].h[:, :], [h, P["ident"]], True, True)
                    w = min(4, KC - g) * 128
                    self.cp(o, o.h[:, g * 128:g * 128 + w], ps, ps.h[:, 0:w], eng=("act" if (g // 4) % 2 else "dve"))
                self.store(out[c0 + a * 128:c0 + (a + 1) * 128, :], o, o.h[:, :])

    def load_x(self, name, dram, K):
        cfg, k = self.cfg, self.k
        kc = K // 128
        X = k.sb(name, [128, kc, cfg.T], BF16)
        v = self.fm(dram)
        step = 1024
        for c0 in range(0, cfg.T, step):
            n = min(step, cfg.T - c0)
            k.dma("sp", X.h[:, :, c0:c0 + n], v[:, :, c0:c0 + n], owner=X, writes=[])
        X.b.w = {X.b.sem: k.dtotal[X.b.sem]}
        return X

    def linear(self, X, KCn, W, ftiles, epi, wbufs, gw=512, chunks=None, xoff=0, pre=None):
        cfg, k = self.cfg, self.k
        chunks = chunks if chunks is not None else cfg.chunks
        Wv = W.rearrange("(kc p) n -> p kc n", p=128)
        groups = []
        cur = []
        for fi, (col0, M) in enumerate(ftiles):
            if cur and (col0 != cur[-1][1] + cur[-1][2] or (col0 + M - cur[0][1]) > gw):
                groups.append(cur)
                cur = []
            cur.append((fi, col0, M))
        if cur:
            groups.append(cur)
        pi = 0
        steps = []
        for gi, grp in enumerate(groups):
            for (fi, col0, M) in grp:
                for ci, (c0, n) in enumerate(chunks):
                    steps.append((fi, ci, c0, n))
        si = 0
        if pre is not None and steps:
            pre(*steps[0])
        for gi, grp in enumerate(groups):
            wt = wbufs[gi % 2]
            g0 = grp[0][1]
            gwid = grp[-1][1] + grp[-1][2] - g0
            self.load(wt, wt.h[:, :, 0:gwid], Wv[:, :, g0:g0 + gwid], eng="pool")
            for (fi, col0, M) in grp:
                for ci, (c0, n) in enumerate(chunks):
                    if pre is not None and si + 1 < len(steps):
                        pre(*steps[si + 1])
                    si += 1
                    ps = self.ps[pi % 4]
                    pi += 1
                    for kc in range(KCn):
                        self.mm(ps, ps.h[0:M, 0:n], wt.h[:, kc, col0 - g0:col0 - g0 + M],
                                X.h[:, kc, c0 - xoff:c0 - xoff + n], [wt, X], kc == 0, kc == KCn - 1)
                    epi(fi, ci, c0, n, ps)

    def linear_tm(self, X, KCn, W, col0, ncols, epi, wt):
        cfg = self.cfg
        Wv = W.rearrange("(kc p) n -> p kc n", p=128)
        self.load(wt, wt.h[:, :, 0:ncols], Wv[:, :, col0:col0 + ncols], eng="pool")
        ntile = (cfg.T + 127) // 128
        for ti in range(ntile):
            t0 = ti * 128
            rows = min(128, cfg.T - t0)
            ps = self.ps[ti % 4]
            for kc in range(KCn):
                self.mm(ps, ps.h[0:rows, 0:ncols], X.h[:, kc, t0:t0 + rows], wt.h[:, kc, 0:ncols], [wt, X],
                        kc == 0, kc == KCn - 1)
            epi(ti, t0, rows, ps)

    def load_rope(self):
        k, P = self.k, self.P
        P["cos"] = k.sb("cos", [64, self.cfg.T], F32)
        P["sin"] = k.sb("sin", [64, self.cfg.T], F32)
        self.load(P["cos"], P["cos"].h[:, :], self.ropet[0, :, :])
        self.load(P["sin"], P["sin"].h[:, :], self.ropet[1, :, :])

    def phase_inproj(self, l, W):
        cfg, k, P, s = self.cfg, self.k, self.P, self.s
        KC = cfg.KC
        X = self.load_x("X", s["hb"], cfg.D)
        self.load_rope()
        wb = [k.sb(f"w{i}", [128, KC, 256], BF16) for i in range(2)]
        st = [k.sb(f"st{i}", [128, 512], BF16) for i in range(4)]
        t2 = k.sb("t2", [64, 512], F32)
        sti = [0]

        def stage():
            t = st[sti[0] % 4]
            sti[0] += 1
            return t

        def mk_copy(dst, row0):
            def epi(fi, ci, c0, n, ps, dst=dst, row0=row0):
                t = stage()
                r = row0 + fi * 128
                self.cp(t, t.h[:, 0:n], ps, ps.h[:, 0:n], eng=("act" if sti[0] % 2 else "dve"))
                self.store(dst[r:r + 128, c0:c0 + n], t, t.h[:, 0:n])
            return epi

        def mk_sig(dst):
            def epi(fi, ci, c0, n, ps, dst=dst):
                t = stage()
                self.act(t, t.h[:, 0:n], ps, ps.h[:, 0:n], AF.Sigmoid)
                self.store(dst[fi * 128:(fi + 1) * 128, c0:c0 + n], t, t.h[:, 0:n])
            return epi

        o = 0
        self.linear(X, KC, W, [(o + i * 128, 128) for i in range(cfg.QR // 128)], mk_copy(s["cqT"], 0), wb, gw=256)
        o += cfg.QR
        self.linear(X, KC, W, [(o + i * 128, 128) for i in range(cfg.KVR // 128)], mk_copy(s["ckvT"], 0), wb, gw=256)
        o += cfg.KVR
        t1all = k.sb("t1all", [64, cfg.T], F32)

        def epi_r1(fi, ci, c0, n, ps):
            self.tt(t1all, t1all.h[:, c0:c0 + n], ps, ps.h[0:64, 0:n], P["cos"], P["cos"].h[:, c0:c0 + n], ALU.mult)

        def epi_r2(fi, ci, c0, n, ps):
            self.tt(t2, t2.h[:, 0:n], ps, ps.h[0:64, 0:n], P["sin"], P["sin"].h[:, c0:c0 + n], ALU.mult)
            t = stage()
            self.tt(t, t.h[0:64, 0:n], t1all, t1all.h[:, c0:c0 + n], t2, t2.h[:, 0:n], ALU.add)
            self.store(s["krT"][:, c0:c0 + n], t, t.h[0:64, 0:n])

        self.linear(X, KC, W, [(o, 64)], epi_r1, wb, gw=256)
        self.linear(X, KC, W, [(cfg.IN_DIM, 64)], epi_r2, wb, gw=256)
        o += cfg.ROPE
        self.linear(X, KC, W, [(o + i * 128, 128) for i in range(cfg.SH)], mk_copy(s["qsT"], 0), wb, gw=256)
        o += cfg.SH * cfg.HD
        self.linear(X, KC, W, [(o + i * 128, 128) for i in range(cfg.SKV)], mk_copy(s["ksT"], 0), wb, gw=256)
        o += cfg.SKV * cfg.HD
        ncv = cfg.SKV * cfg.HD

        def epi_v(ti, t0, rows, ps):
            t = stage()
            self.cp(t, t.h[0:rows, 0:ncv], ps, ps.h[0:rows, 0:ncv], eng=("act" if ti % 2 else "dve"))
            self.store(s["vs"][t0:t0 + rows, :], t, t.h[0:rows, 0:ncv])

        for v0 in range(0, ncv, 256):
            vn = min(256, ncv - v0)

            def epi_v(ti, t0, rows, ps, v0=v0, vn=vn):
                t = stage()
                self.cp(t, t.h[0:rows, 0:vn], ps, ps.h[0:rows, 0:vn], eng=('act' if ti % 2 else 'dve'))
                self.store(s['vs'][t0:t0 + rows, v0:v0 + vn], t, t.h[0:rows, 0:vn])

            self.linear_tm(X, KC, W, o + v0, vn, epi_v, wb[(v0 // 256) % 2])
        o += ncv
        self.linear(X, KC, W, [(o + i * 128, 128) for i in range(KC)], mk_sig(s["gaT"]), wb, gw=256)
        o += cfg.D
        self.linear(X, KC, W, [(o + i * 128, 128) for i in range(KC)], mk_sig(s["gbT"]), wb, gw=256)

    def rms_stats(self, X, KCn, gsl, rdim, rstd_bc, rstd_tm):
        cfg, k, P = self.cfg, self.k, self.P
        sqs = [k.sb(f"rsq{i}", [128, KCn, 512], BF16) for i in range(2)]
        for ci, (c0, n) in enumerate(cfg.chunks):
            sq = sqs[ci % 2]
            self.act(sq, sq.h[:, :, 0:n], X, X.h[:, :, c0:c0 + n], AF.Square)
            ps = self.ps[4 + ci % 2]
            for kc in range(KCn):
                self.mm(ps, ps.h[:, 0:n], P["onesb"].h[:, :], sq.h[:, kc, 0:n], [P["onesb"], sq], kc == 0, kc == KCn - 1)
            self.ts(rstd_bc, rstd_bc.h[:, c0:c0 + n], ps, ps.h[:, 0:n], 1.0 / rdim, cfg.RMS_EPS, ALU.mult, ALU.add)
            self.act(rstd_bc, rstd_bc.h[:, c0:c0 + n], rstd_bc, rstd_bc.h[:, c0:c0 + n], AF.Sqrt)
            k.op('dve', lambda e, c0=c0, n=n: e.reciprocal(rstd_bc.h[:, c0:c0 + n], rstd_bc.h[:, c0:c0 + n]), reads=[rstd_bc], writes=[rstd_bc])
            if rstd_tm is not None:
                for a in range((n + 127) // 128):
                    rows = min(128, n - a * 128)
                    ti = (c0 + a * 128) // 128
                    pt = self.ps[6 + a % 2]
                    for kc in range(KCn):
                        self.mm(pt, pt.h[0:rows, 0:1], sq.h[:, kc, a * 128:a * 128 + rows], P["onesb"].h[:, 0:1],
                                [P["onesb"], sq], kc == 0, kc == KCn - 1)
                    self.ts(rstd_tm, rstd_tm.h[0:rows, ti:ti + 1], pt, pt.h[0:rows, 0:1], 1.0 / rdim, cfg.RMS_EPS,
                            ALU.mult, ALU.add)
                    self.act(rstd_tm, rstd_tm.h[0:rows, ti:ti + 1], rstd_tm, rstd_tm.h[0:rows, ti:ti + 1], AF.Sqrt)
                    k.op('dve', lambda e, rows=rows, ti=ti: e.reciprocal(rstd_tm.h[0:rows, ti:ti + 1], rstd_tm.h[0:rows, ti:ti + 1]), reads=[rstd_tm], writes=[rstd_tm])
        pvh = P["pvec"]
        for kc in range(KCn):
            self.ts(X, X.h[:, kc, :], X, X.h[:, kc, :], pvh.h[:, gsl[0] + kc:gsl[0] + kc + 1], None, ALU.mult,
                    extra_reads=[pvh])

    def phase_mla_up(self, l, Wq, Wkv, qg, kvg):
        cfg, k, P, s = self.cfg, self.k, self.P, self.s
        T = cfg.T
        MH = cfg.MH
        ntile = (T + 127) // 128
        KQ = cfg.QR // 128
        X = self.load_x("Xq", s["cqT"], cfg.QR)
        rq = k.sb("rq", [128, T], F32)
        self.load_rope()
        self.rms_stats(X, KQ, qg, cfg.QR, rq, None)
        wb = [k.sb(f"w{i}", [128, max(KQ, cfg.KVR // 128), 512], BF16) for i in range(2)]
        st = [k.sb(f"st{i}", [128, 512], BF16) for i in range(4)]
        t1 = k.sb("t1", [64, 512], F32)
        t2 = k.sb("t2", [64, 512], F32)
        sti = [0]

        def stage():
            t = st[sti[0] % 4]
            sti[0] += 1
            return t

        def epi_qn(fi, ci, c0, n, ps):
            t = stage()
            self.tt(t, t.h[:, 0:n], ps, ps.h[:, 0:n], rq, rq.h[:, c0:c0 + n], ALU.mult)
            self.store(s["qnT"][fi * 128:(fi + 1) * 128, c0:c0 + n], t, t.h[:, 0:n])

        self.linear(X, KQ, Wq, [(h * 128, 128) for h in range(MH)], epi_qn, wb)
        ro = MH * cfg.NOPE
        t1all = k.sb("t1all", [64, T], F32)

        def epi_qr(fi, ci, c0, n, ps):
            h_ = fi // 2
            if fi % 2 == 0:
                self.tt(t1all, t1all.h[:, c0:c0 + n], ps, ps.h[0:64, 0:n], P["cos"], P["cos"].h[:, c0:c0 + n],
                        ALU.mult)
            else:
                self.tt(t2, t2.h[:, 0:n], ps, ps.h[0:64, 0:n], P["sin"], P["sin"].h[:, c0:c0 + n], ALU.mult)
                self.tt(t2, t2.h[:, 0:n], t2, t2.h[:, 0:n], t1all, t1all.h[:, c0:c0 + n], ALU.add)
                t = stage()
                self.tt(t, t.h[0:64, 0:n], t2, t2.h[:, 0:n], rq, rq.h[0:64, c0:c0 + n], ALU.mult)
                self.store(s["qrT"][h_ * 64:(h_ + 1) * 64, c0:c0 + n], t, t.h[0:64, 0:n])

        fts = []
        for h in range(MH):
            fts += [(ro + h * 128, 64), (ro + h * 128 + 64, 64)]
        self.linear(X, KQ, Wq, fts, epi_qr, wb)
        self.k.barrier()
        KV = cfg.KVR // 128
        X = self.load_x("Xkv", s["ckvT"], cfg.KVR)
        rk = k.sb("rk", [128, T], F32)
        rkt = k.sb("rkt", [128, ntile], F32)
        self.rms_stats(X, KV, kvg, cfg.KVR, rk, rkt)
        wb = [k.sb(f"w{i}", [128, KV, 512], BF16) for i in range(2)]
        st = [k.sb(f"st{i}", [128, 512], BF16) for i in range(4)]

        def epi_kn(fi, ci, c0, n, ps):
            t = stage2()
            self.tt(t, t.h[:, 0:n], ps, ps.h[:, 0:n], rk, rk.h[:, c0:c0 + n], ALU.mult)
            self.store(s["knT"][fi * 128:(fi + 1) * 128, c0:c0 + n], t, t.h[:, 0:n])

        sti2 = [0]

        def stage2():
            t = st[sti2[0] % 4]
            sti2[0] += 1
            return t

        self.linear(X, KV, Wkv, [(h * 128, 128) for h in range(MH)], epi_kn, wb)
        vo = MH * cfg.NOPE
        for g0 in range(0, MH * cfg.VD, 512):
            ncv = min(512, MH * cfg.VD - g0)

            def epi_v(ti, t0, rows, ps, g0=g0, ncv=ncv):
                t = stage2()
                self.ts(t, t.h[0:rows, 0:ncv], ps, ps.h[0:rows, 0:ncv], rkt.h[0:rows, ti:ti + 1], None, ALU.mult,
                        extra_reads=[rkt])
                self.store(s["vm"][t0:t0 + rows, g0:g0 + ncv], t, t.h[0:rows, 0:ncv])

            self.linear_tm(X, KV, Wkv, vo + g0, ncv, epi_v, wb[(g0 // 512) % 2])

    def phase_mla_attn(self, l):
        cfg, k, P, s = self.cfg, self.k, self.P, self.s
        T = cfg.T
        ntile = (T + 127) // 128
        scale = (cfg.NOPE + cfg.ROPE) ** -0.5
        kr = k.sb("kr", [128, T], BF16)
        k.op("dve", lambda e: e.memset(kr.h[64:128, :], 0.0), writes=[kr])
        self.load(kr, kr.h[0:64, :], s["krT"][:, :])
        kns = [k.sb(f"kn{i}", [128, T], BF16) for i in range(2)]
        vs_ = [k.sb(f"v{i}", [128, ntile, cfg.VD], BF16) for i in range(2)]
        qns = [k.sb(f"qn{i}", [128, 512], BF16) for i in range(2)]
        qrs = [k.sb(f"qr{i}", [128, 512], BF16) for i in range(2)]
        for t in qrs:
            k.op("dve", lambda e, t=t: e.memset(t.h[64:128, :], 0.0), writes=[t])
        pts = [k.sb(f"pt{i}", [128, 512], BF16) for i in range(4)]
        sbank = [self.ps[0], self.ps[1], self.ps[2], self.ps[5]]
        rcs = [k.sb(f"rc{i}", [128, 512], F32) for i in range(2)]
        daccs = [k.sb(f"dacc{i}", [128, 512], F32) for i in range(2)]
        obs = [k.sb(f"ob{i}", [128, 512], BF16) for i in range(2)]
        nfull = T // 128
        rem = T - nfull * 128

        def load_head(h):
            kn = kns[h % 2]
            v = vs_[h % 2]
            self.load(kn, kn.h[:, :], s["knT"][h * 128:(h + 1) * 128, :])
            k.dma("sp", v.h[:, 0:nfull, :],
                  s["vm"][0:nfull * 128, h * cfg.VD:(h + 1) * cfg.VD].rearrange("(a p) d -> p a d", p=128),
                  owner=v, writes=[v])
            if rem:
                k.dma("sp", v.h[0:rem, nfull, :], s["vm"][nfull * 128:T, h * cfg.VD:(h + 1) * cfg.VD], owner=v,
                      writes=[])
                v.b.w = {v.b.sem: k.dtotal[v.b.sem]}

        steps = [(h, c0, n) for h in range(cfg.MH) for (c0, n) in cfg.chunks]

        def load_q(si):
            h, c0, n = steps[si]
            qn, qr = qns[si % 2], qrs[si % 2]
            self.load(qn, qn.h[:, 0:n], s["qnT"][h * 128:(h + 1) * 128, c0:c0 + n])
            self.load(qr, qr.h[0:64, 0:n], s["qrT"][h * 64:(h + 1) * 64, c0:c0 + n])

        load_head(0)
        load_q(0)
        for si, (h, c0, n) in enumerate(steps):
            kn = kns[h % 2]
            v = vs_[h % 2]
            if c0 == 0 and h + 1 < cfg.MH:
                load_head(h + 1)
            if si + 1 < len(steps):
                load_q(si + 1)
            qn, qr, rc, ob = qns[si % 2], qrs[si % 2], rcs[si % 2], obs[si % 2]
            dacc = daccs[si % 2]
            po, pd = self.ps[3], self.ps[4]

            def smm(kt):
                kk = 128 if kt < nfull else rem
                S = sbank[kt % 4]
                self.mm(S, S.h[0:kk, 0:n], kn.h[:, kt * 128:kt * 128 + kk], qn.h[:, 0:n], [kn, qn], True, False)
                self.mm(S, S.h[0:kk, 0:n], kr.h[:, kt * 128:kt * 128 + kk], qr.h[:, 0:n], [kr, qr], False, True)

            smm(0)
            if ntile > 1:
                smm(1)
            for kt in range(ntile):
                kk = 128 if kt < nfull else rem
                if kt + 2 < ntile:
                    smm(kt + 2)
                S = sbank[kt % 4]
                pt = pts[kt % 4]
                self.act(pt, pt.h[0:kk, 0:n], S, S.h[0:kk, 0:n], AF.Exp, scale=scale)
                self.mm(po, po.h[:, 0:n], v.h[0:kk, kt, :], pt.h[0:kk, 0:n], [v, pt], kt == 0, kt == ntile - 1)
                if kt == 0:
                    self.cp(dacc, dacc.h[0:kk, 0:n], pt, pt.h[0:kk, 0:n])
                else:
                    self.tt(dacc, dacc.h[0:kk, 0:n], dacc, dacc.h[0:kk, 0:n], pt, pt.h[0:kk, 0:n], ALU.add)
            self.mm(pd, pd.h[:, 0:n], P["ones"].h[:, :], dacc.h[:, 0:n], [P["ones"], dacc], True, True)
            k.op("dve", lambda e, rc=rc, pd=pd, n=n: e.reciprocal(rc.h[:, 0:n], pd.h[:, 0:n]), reads=[pd],
                 writes=[rc])
            self.tt(ob, ob.h[:, 0:n], po, po.h[:, 0:n], rc, rc.h[:, 0:n], ALU.mult)
            self.store(s["oaT"][h * 128:(h + 1) * 128, c0:c0 + n], ob, ob.h[:, 0:n])

    def phase_swa(self, l, biasd, biasmk, biasqm, sinkb):
        cfg, k, P, s = self.cfg, self.k, self.P, self.s
        T, NB, G = cfg.T, cfg.NB, cfg.GRP
        ntile = (T + 127) // 128
        scale = cfg.HD ** -0.5
        inv = 1.0 / scale
        GW = G * 128
        sk = k.sb("sk", [128, cfg.SH], F32)
        self.load(sk, sk.h[:, :], sinkb[:, :])
        self.act(sk, sk.h[:, :], sk, sk.h[:, :], AF.Exp)
        ks_ = [k.sb(f"ks{i}", [128, T], BF16) for i in range(2)]
        vs_ = [k.sb(f"v{i}", [128, ntile, cfg.HD], BF16) for i in range(2)]
        qs_ = [k.sb(f"q{i}", [128, G, T], BF16) for i in range(2)]
        bd = [k.sb(f"bd{i}", [128, 3, GW], BF16) for i in range(2)]
        bmk = [k.sb(f"bmk{i}", [16, 2, GW], BF16) for i in range(2)]
        bqm = [k.sb(f"bqm{i}", [128, 2, G * 16], BF16) for i in range(2)]
        ske = [k.sb(f"ske{i}", [128, G, 128], F32) for i in range(2)]
        pts = [k.sb(f"pt{i}", [128, GW], BF16) for i in range(4)]
        rcs = [k.sb(f"rc{i}", [128, GW], F32) for i in range(2)]
        obs = [k.sb(f"ob{i}", [128, G, 128], BF16) for i in range(2)]
        si = 0
        bi = 0
        def load_hk(hk):
            ks, v, q = ks_[hk % 2], vs_[hk % 2], qs_[hk % 2]
            b_d, b_mk, b_qm = bd[hk % 2], bmk[hk % 2], bqm[hk % 2]
            self.load(ks, ks.h[:, :], s["ksT"][hk * 128:(hk + 1) * 128, :])
            k.dma("sp", v.h[:, 0:NB, :],
                  s["vs"][0:NB * 128, hk * cfg.HD:(hk + 1) * cfg.HD].rearrange("(a p) d -> p a d", p=128),
                  owner=v, writes=[v])
            k.dma("sp", v.h[0:16, NB, :], s["vs"][NB * 128:T, hk * cfg.HD:(hk + 1) * cfg.HD], owner=v, writes=[])
            v.b.w = {v.b.sem: k.dtotal[v.b.sem]}
            self.load(q, q.h[:, :, :],
                      s["qsT"][hk * G * 128:(hk + 1) * G * 128, :].rearrange("(g p) t -> p g t", p=128))
            self.load(b_d, b_d.h[:, :, :], biasd[hk].rearrange("d p w -> p d w"), eng="pool")
            self.load(b_mk, b_mk.h[:, :, :], biasmk[hk].rearrange("d p w -> p d w"), eng="pool")
            k.dma("pool", b_qm.h[0:16, 0, :], biasqm[hk, 0:16, :], owner=b_qm, writes=[b_qm])
            k.dma("pool", b_qm.h[:, 1, :], biasqm[hk, 16:144, :], owner=b_qm, writes=[])
            b_qm.b.w = {b_qm.b.sem: k.dtotal[b_qm.b.sem]}

        load_hk(0)
        for hk in range(cfg.SKV):
            ks, v, q = ks_[hk % 2], vs_[hk % 2], qs_[hk % 2]
            b_d, b_mk, b_qm, sk_e = bd[hk % 2], bmk[hk % 2], bqm[hk % 2], ske[hk % 2]
            if hk + 1 < cfg.SKV:
                load_hk(hk + 1)
            self.ts(b_d, b_d.h[:, :, :], b_d, b_d.h[:, :, :], inv, None, ALU.mult)
            self.ts(b_mk, b_mk.h[:, :, :], b_mk, b_mk.h[:, :, :], inv, None, ALU.mult)
            self.ts(b_qm, b_qm.h[0:16, 0, :], b_qm, b_qm.h[0:16, 0, :], inv, None, ALU.mult)
            self.ts(b_qm, b_qm.h[:, 1, :], b_qm, b_qm.h[:, 1, :], inv, None, ALU.mult)
            for g in range(G):
                hh = hk * G + g
                self.cp(sk_e, sk_e.h[:, g, :], sk, sk.h[:, hh:hh + 1].to_broadcast([128, 128]))
            for i in range(NB + 1):
                if i < NB:
                    nq = 128
                    W_ = GW
                    qap = q.h[:, :, i * 128:(i + 1) * 128]
                    keys = [(NB, 16, b_mk.h[0:16, 0 if i == 0 else 1, :])]
                    for dlt in (-1, 0, 1):
                        j = i + dlt
                        if 0 <= j < NB:
                            keys.append((j, 128, b_d.h[:, dlt + 1, :]))
                else:
                    nq = 16
                    W_ = G * 16
                    qap = q.h[:, :, NB * 128:NB * 128 + 16]
                    keys = [(NB, 16, b_qm.h[0:16, 0, :]), (0, 128, b_qm.h[:, 1, :])]
                po, pd = self.ps[3], self.ps[4]
                rc, ob = rcs[bi % 2], obs[bi % 2]
                bi += 1
                nk = len(keys)
                sb4 = [self.ps[0], self.ps[1], self.ps[2], self.ps[5]]
                slots = []
                for ki, (j, kk, bap) in enumerate(keys):
                    S = sb4[si % 4]
                    pt = pts[si % 4]
                    si += 1
                    slots.append((S, pt))
                    braw = b_mk if (i < NB and ki == 0) else (b_d if i < NB else b_qm)
                    self.mm(S, S.h[0:kk, 0:W_], ks.h[:, j * 128:j * 128 + kk], qap, [ks, q], True, False)
                    self.mm(S, S.h[0:kk, 0:W_], P["identb"].h[0:kk, 0:kk], bap, [P["identb"], braw], False, True)
                for ki, (j, kk, bap) in enumerate(keys):
                    S, pt = slots[ki]
                    self.act(pt, pt.h[0:kk, 0:W_], S, S.h[0:kk, 0:W_], AF.Exp, scale=scale)
                    self.mm(po, po.h[:, 0:W_], v.h[0:kk, j, :], pt.h[0:kk, 0:W_], [v, pt], ki == 0, ki == nk - 1)
                    self.mm(pd, pd.h[:, 0:W_], P["onesb"].h[0:kk, :], pt.h[0:kk, 0:W_], [P["onesb"], pt], ki == 0,
                            ki == nk - 1)
                pdv = pd.h[:, 0:W_].rearrange("p (g q) -> p g q", g=G)
                rcv = rc.h[:, 0:W_].rearrange("p (g q) -> p g q", g=G)
                pov = po.h[:, 0:W_].rearrange("p (g q) -> p g q", g=G)
                self.tt(rc, rcv, pd, pdv, sk_e, sk_e.h[:, :, 0:nq], ALU.add)
                k.op("dve", lambda e, rcv=rcv: e.reciprocal(rcv, rcv), reads=[rc], writes=[rc])
                self.tt(ob, ob.h[:, :, 0:nq], po, pov, rc, rcv, ALU.mult)
                t0 = i * 128
                dst = s["obT"][hk * G * 128:(hk + 1) * G * 128, t0:t0 + nq].rearrange("(g p) t -> p g t", p=128)
                self.store(dst, ob, ob.h[:, :, 0:nq])

    def phase_proj(self, l, Wa, Wb, Wo):
        cfg, k, P, s = self.cfg, self.k, self.P, self.s
        KC = cfg.KC

        def run(Xd, W, epi, nm, pre):
            Kx = Xd.shape[0]
            X = self.load_x(nm, Xd, Kx)
            wb = [k.sb(f"w{i}", [128, Kx // 128, 512], BF16) for i in range(2)]
            self.linear(X, Kx // 128, W, [(i * 128, 128) for i in range(KC)], epi, wb, pre=pre)

        NBUF = 4

        def alloc_common():
            return ([k.sb(f"g{i}", [128, 512], BF16) for i in range(NBUF)],
                    [k.sb(f"a{i}", [128, 512], F32) for i in range(NBUF)],
                    [k.sb(f"o{i}", [128, 512], F32) for i in range(NBUF)],
                    [k.sb(f"ob{i}", [128, 512], BF16) for i in range(NBUF)])

        pc = [0]
        ec = [0]
        gt, at, ot, obt = alloc_common()

        def preA(fi, ci, c0, n):
            i = pc[0] % NBUF
            pc[0] += 1
            self.load(gt[i], gt[i].h[:, 0:n], s["gaT"][fi * 128:(fi + 1) * 128, c0:c0 + n])

        def epiA(fi, ci, c0, n, ps):
            i = ec[0] % NBUF
            ec[0] += 1
            self.tt(ot[i], ot[i].h[:, 0:n], ps, ps.h[:, 0:n], gt[i], gt[i].h[:, 0:n], ALU.mult)
            self.store(s["tAT"][fi * 128:(fi + 1) * 128, c0:c0 + n], ot[i], ot[i].h[:, 0:n])

        run(s["oaT"], Wa, epiA, "Xa", preA)
        k.barrier()
        gt, at, ot, obt = alloc_common()
        pc[0] = ec[0] = 0

        def preB(fi, ci, c0, n):
            i = pc[0] % NBUF
            pc[0] += 1
            self.load(gt[i], gt[i].h[:, 0:n], s["gbT"][fi * 128:(fi + 1) * 128, c0:c0 + n])
            self.load(at[i], at[i].h[:, 0:n], s["tAT"][fi * 128:(fi + 1) * 128, c0:c0 + n])

        def epiB(fi, ci, c0, n, ps):
            i = ec[0] % NBUF
            ec[0] += 1
            self.tt(ot[i], ot[i].h[:, 0:n], ps, ps.h[:, 0:n], gt[i], gt[i].h[:, 0:n], ALU.mult)
            self.tt(obt[i], obt[i].h[:, 0:n], ot[i], ot[i].h[:, 0:n], at[i], at[i].h[:, 0:n], ALU.add)
            self.store(s["mgT"][fi * 128:(fi + 1) * 128, c0:c0 + n], obt[i], obt[i].h[:, 0:n])

        run(s["obT"], Wb, epiB, "Xb", preB)
        k.barrier()
        gt, at, ot, obt = alloc_common()
        pc[0] = ec[0] = 0

        def preO(fi, ci, c0, n):
            i = pc[0] % NBUF
            pc[0] += 1
            self.load(at[i], at[i].h[:, 0:n], s["hres"][fi * 128:(fi + 1) * 128, c0:c0 + n])

        def epiO(fi, ci, c0, n, ps):
            i = ec[0] % NBUF
            ec[0] += 1
            k.op("dve", lambda e, i=i, n=n, ps=ps: e.scalar_tensor_tensor(ot[i].h[:, 0:n], at[i].h[:, 0:n], cfg.ALPHA,
                                                                          ps.h[:, 0:n], ALU.mult, ALU.add),
                 reads=[at[i], ps], writes=[ot[i]])
            self.store(s["yT"][fi * 128:(fi + 1) * 128, c0:c0 + n], ot[i], ot[i].h[:, 0:n])

        run(s["mgT"], Wo, epiO, "Xm", preO)

    def phase_router(self, Wr, rbb):
        cfg, k, P, s = self.cfg, self.k, self.P, self.s
        KC, NE, T = cfg.KC, cfg.NE, cfg.T
        wr = k.sb("wr", [128, KC, NE], F32)
        self.load(wr, wr.h[:, :, :], Wr.rearrange("(kc p) e -> p kc e", p=128))
        rb = k.sb("rb", [128, NE], F32)
        self.load(rb, rb.h[:, :], rbb[:, :])
        hs = [k.sb(f"h{i}", [128, KC, 512], F32) for i in range(2)]
        lg = [k.sb(f"lg{i}", [128, NE], F32) for i in range(2)]
        mx = [k.sb(f"mx{i}", [128, 8], F32) for i in range(2)]
        ex = [k.sb(f"ex{i}", [128, NE], F32) for i in range(2)]
        mk = [k.sb(f"mk{i}", [128, NE], F32) for i in range(2)]
        dn = [k.sb(f"dn{i}", [128, 2], F32) for i in range(2)]
        gtt = [k.sb(f"gtt{i}", [NE, 128], F32) for i in range(2)]
        pmt = [k.sb(f"pmt{i}", [NE, 128], F32) for i in range(2)]
        pm = [k.sb(f"pm{i}", [128, NE], F32) for i in range(2)]
        R = k.sb("R", [128, NE], F32)
        k.op("dve", lambda e: e.memset(R.h[:, :], 0.0), writes=[R])
        obf = [k.sb(f"obf{i}", [128, cfg.D], BF16) for i in range(2)]
        ti = 0
        def ldh(ci):
            c0, n = cfg.chunks[ci]
            h = hs[ci % 2]
            self.load(h, h.h[:, :, 0:n], self.fm(s["hres"])[:, :, c0:c0 + n])

        ldh(0)
        for ci, (c0, n) in enumerate(cfg.chunks):
            h = hs[ci % 2]
            if ci + 1 < len(cfg.chunks):
                ldh(ci + 1)
            for a in range((n + 127) // 128):
                rows = min(128, n - a * 128)
                t0 = c0 + a * 128
                i = ti % 2
                ti += 1
                ps = self.ps[i]
                for kc in range(KC):
                    self.mm(ps, ps.h[0:rows, 0:NE], h.h[:, kc, a * 128:a * 128 + rows], wr.h[:, kc, :], [h, wr],
                            kc == 0, kc == KC - 1)
                L_, M_, E_, K_, D_ = lg[i], mx[i], ex[i], mk[i], dn[i]
                self.tt(L_, L_.h[0:rows, :], ps, ps.h[0:rows, 0:NE], rb, rb.h[0:rows, :], ALU.add)
                k.op("dve", lambda e, M_=M_, L_=L_, rows=rows: e.max(M_.h[0:rows, :], L_.h[0:rows, :]), reads=[L_],
                     writes=[M_])
                self.ts(K_, K_.h[0:rows, :], L_, L_.h[0:rows, :], M_.h[0:rows, 1:2], None, ALU.is_ge, extra_reads=[M_])
                self.ts(D_, D_.h[0:rows, 0:1], M_, M_.h[0:rows, 0:1], -1.0, None, ALU.mult)
                self.act(E_, E_.h[0:rows, :], L_, L_.h[0:rows, :], AF.Exp, extra_reads=[D_], bias=D_.h[0:rows, 0:1])
                self.tt(E_, E_.h[0:rows, :], E_, E_.h[0:rows, :], K_, K_.h[0:rows, :], ALU.mult)
                k.op("dve", lambda e, D_=D_, E_=E_, rows=rows: e.tensor_reduce(D_.h[0:rows, 1:2], E_.h[0:rows, :],
                                                                              AX.X, ALU.add),
                     reads=[E_], writes=[D_])
                k.op("dve", lambda e, D_=D_, rows=rows: e.reciprocal(D_.h[0:rows, 1:2], D_.h[0:rows, 1:2]), reads=[D_],
                     writes=[D_])
                self.ts(E_, E_.h[0:rows, :], E_, E_.h[0:rows, :], D_.h[0:rows, 1:2], None, ALU.mult, extra_reads=[D_])
                pt = self.ps[2 + i]
                self.mm(pt, pt.h[0:NE, 0:rows], E_.h[0:rows, :], P["ident"].h[0:rows, 0:rows], [E_, P["ident"]], True,
                        True)
                G_ = gtt[i]
                self.cp(G_, G_.h[:, 0:rows], pt, pt.h[0:NE, 0:rows], eng="act")
                self.store(s["gtT"][:, t0:t0 + rows], G_, G_.h[:, 0:rows])
                pp = self.ps[4 + i]
                self.mm(pp, pp.h[0:rows, 0:NE], P["upper"].h[0:rows, 0:rows], K_.h[0:rows, :], [P["upper"], K_], True,
                        ti == 1)
                if ti > 1:
                    self.mm(pp, pp.h[0:rows, 0:NE], P["ones"].h[:, 0:rows], R.h[:, :], [P["ones"], R], False, True)
                PM = pm[i]
                k.op("dve", lambda e, PM=PM, pp=pp, K_=K_, rows=rows: e.scalar_tensor_tensor(
                    PM.h[0:rows, :], pp.h[0:rows, 0:NE], 1.0, K_.h[0:rows, :], ALU.add, ALU.mult),
                    reads=[pp, K_], writes=[PM])
                self.ts(PM, PM.h[0:rows, :], PM, PM.h[0:rows, :], -1.0, None, ALU.add)
                self.tt(R, R.h[0:rows, :], R, R.h[0:rows, :], K_, K_.h[0:rows, :], ALU.add)
                self.store(s["pmtm"][t0:t0 + rows, :], PM, PM.h[0:rows, :])
                pt2 = self.ps[6 + i]
                self.mm(pt2, pt2.h[0:NE, 0:rows], PM.h[0:rows, :], P["ident"].h[0:rows, 0:rows], [PM, P["ident"]],
                        True, True)
                PT = pmt[i]
                self.cp(PT, PT.h[:, 0:rows], pt2, pt2.h[0:NE, 0:rows], eng="act")
                self.store(s["pmT"][:, t0:t0 + rows], PT, PT.h[:, 0:rows])
                ob = obf[i]
                for g in range(0, KC, 4):
                    ph = self.ps[2 + (g // 4) % 2]
                    gn = min(4, KC - g)
                    for j in range(gn):
                        self.mm(ph, ph.h[0:rows, j * 128:(j + 1) * 128], h.h[:, g + j, a * 128:a * 128 + rows],
                                P["ident"].h[:, :], [h, P["ident"]], True, True)
                    self.cp(ob, ob.h[0:rows, g * 128:(g + gn) * 128], ph, ph.h[0:rows, 0:gn * 128],
                            eng=("act" if (g // 4) % 2 else "dve"))
                self.store(s["htm"][t0:t0 + rows, :], ob, ob.h[0:rows, :])

    def slot_chunks(self, s0, s1):
        out = []
        c = s0
        while c < s1:
            n = min(512, s1 - c)
            out.append((c, n))
            c += n
        return out

    def phase_gather(self):
        cfg, k, P, s = self.cfg, self.k, self.P, self.s
        KC, NE, T, CAP = cfg.KC, cfg.NE, cfg.T, cfg.CAP
        nfull = T // 128
        rem = T - nfull * 128
        ntile = nfull + (1 if rem else 0)
        iota = k.sb("iota", [128, CAP], F32)
        self.load(iota, iota.h[:, :], self.iotac[:, :])
        pma = k.sb("pma", [128, ntile, NE], F32)
        k.dma("sp", pma.h[:, 0:nfull, :], s["pmtm"][0:nfull * 128, :].rearrange("(a p) e -> p a e", p=128), owner=pma,
              writes=[pma])
        if rem:
            k.dma("sp", pma.h[0:rem, nfull, :], s["pmtm"][nfull * 128:T, :], owner=pma, writes=[])
            pma.b.w = {pma.b.sem: k.dtotal[pma.b.sem]}
        sel = k.sb("sel", [128, ntile, CAP], BF16)
        hts = [k.sb(f"htk{i}", [128, ntile, 256], BF16) for i in range(2)]
        st = [k.sb(f"st{i}", [128, 512], BF16) for i in range(4)]
        sti = 0
        hi = 0
        def ld_ht(idx):
            kc0 = (idx % ((KC + 1) // 2)) * 2
            kn_ = min(2, KC - kc0)
            ht = hts[idx % 2]
            k.dma("sp", ht.h[:, 0:nfull, 0:kn_ * 128],
                  s["htm"][0:nfull * 128, kc0 * 128:(kc0 + kn_) * 128].rearrange("(a p) d -> p a d", p=128),
                  owner=ht, writes=[ht])
            if rem:
                k.dma("sp", ht.h[0:rem, nfull, 0:kn_ * 128], s["htm"][nfull * 128:T, kc0 * 128:(kc0 + kn_) * 128],
                      owner=ht, writes=[])
                ht.b.w = {ht.b.sem: k.dtotal[ht.b.sem]}

        nk2 = (KC + 1) // 2
        total = NE * nk2
        ld_ht(0)
        for e in range(NE):
            for i in range(ntile):
                rows = 128 if i < nfull else rem
                self.ts(sel, sel.h[0:rows, i, :], iota, iota.h[0:rows, :], pma.h[0:rows, i, e:e + 1], None, ALU.is_equal,
                        extra_reads=[pma])
            for kq in range(nk2):
                idx = e * nk2 + kq
                kc0 = kq * 2
                kn_ = min(2, KC - kc0)
                ht = hts[idx % 2]
                if idx + 1 < total:
                    ld_ht(idx + 1)
                for kk_ in range(kn_):
                    kc = kc0 + kk_
                    for (c0, n) in self.slot_chunks(0, CAP):
                        ps = self.ps[sti % 4]
                        for i in range(ntile):
                            rows = 128 if i < nfull else rem
                            self.mm(ps, ps.h[:, 0:n], ht.h[0:rows, i, kk_ * 128:(kk_ + 1) * 128],
                                    sel.h[0:rows, i, c0:c0 + n], [ht, sel], i == 0, i == ntile - 1)
                        t = st[sti % 4]
                        self.cp(t, t.h[:, 0:n], ps, ps.h[:, 0:n], eng=("act" if sti % 2 else "dve"))
                        sti += 1
                        self.store(s["XeT"][e * cfg.D + kc * 128:e * cfg.D + (kc + 1) * 128, c0:c0 + n], t,
                                   t.h[:, 0:n])

    def phase_moe_ffn(self, experts):
        cfg, k, P, s = self.cfg, self.k, self.P, self.s
        KC, DFF, CAP, D = cfg.KC, cfg.DFF, cfg.CAP, cfg.D
        FK = DFF // 128
        HALF = max(e1 - e0 for e0, e1 in cfg.HALVES)
        nst = HALF // 128
        X = k.sb("Xe", [128, KC, HALF], BF16)
        G_ = k.sb("G", [128, FK, HALF], BF16)
        w1b = [k.sb(f"w1_{i}", [128, KC, 256], BF16) for i in range(2)]
        w3b = [k.sb(f"w3_{i}", [128, KC, 256], BF16) for i in range(2)]
        w2b = [k.sb(f"w2_{i}", [128, FK, 256], BF16) for i in range(2)]
        Yb = k.sb("Yb", [128, nst, D], BF16)
        sa = [k.sb(f"sa{i}", [128, 512], F32) for i in range(2)]
        ctr = 0
        wctr = 0
        for ei, (W1, W3, W2) in enumerate(experts):
            W1v = W1.rearrange("(kc p) n -> p kc n", p=128)
            W3v = W3.rearrange("(kc p) n -> p kc n", p=128)
            W2v = W2.rearrange("(kc p) n -> p kc n", p=128)
            for (s0, s1) in cfg.HALVES:
                hw = s1 - s0
                hst = hw // 128
                chs = self.slot_chunks(0, hw)
                self.load(X, X.h[:, :, 0:hw], self.fm(s["XeT"][ei * D:(ei + 1) * D, :])[:, :, s0:s1])
                for j0 in range(0, FK, 2):
                    jn = min(2, FK - j0)
                    w1, w3 = w1b[wctr % 2], w3b[wctr % 2]
                    wctr += 1
                    self.load(w1, w1.h[:, :, 0:jn * 128], W1v[:, :, j0 * 128:(j0 + jn) * 128], eng="pool")
                    self.load(w3, w3.h[:, :, 0:jn * 128], W3v[:, :, j0 * 128:(j0 + jn) * 128], eng="pool")
                    for jj in range(jn):
                        j = j0 + jj
                        for (c0, n) in chs:
                            i = ctr % 2
                            ctr += 1
                            pa, pb = self.ps[2 * i], self.ps[2 * i + 1]
                            for kc in range(KC):
                                self.mm(pa, pa.h[:, 0:n], w1.h[:, kc, jj * 128:(jj + 1) * 128], X.h[:, kc, c0:c0 + n],
                                        [w1, X], kc == 0, kc == KC - 1)
                            for kc in range(KC):
                                self.mm(pb, pb.h[:, 0:n], w3.h[:, kc, jj * 128:(jj + 1) * 128], X.h[:, kc, c0:c0 + n],
                                        [w3, X], kc == 0, kc == KC - 1)
                            self.act(sa[i], sa[i].h[:, 0:n], pa, pa.h[:, 0:n], AF.Silu)
                            self.tt(G_, G_.h[:, j, c0:c0 + n], sa[i], sa[i].h[:, 0:n], pb, pb.h[:, 0:n], ALU.mult)
                for cg in range(0, D, 256):
                    cn = min(256, D - cg)
                    w2 = w2b[wctr % 2]
                    wctr += 1
                    self.load(w2, w2.h[:, :, 0:cn], W2v[:, :, cg:cg + cn], eng="pool")
                    for st_ in range(hst):
                        ps = self.ps[4 + ctr % 4]
                        ctr += 1
                        for j in range(FK):
                            self.mm(ps, ps.h[:, 0:cn], G_.h[:, j, st_ * 128:(st_ + 1) * 128], w2.h[:, j, 0:cn], [G_, w2],
                                    j == 0, j == FK - 1)
                        self.cp(Yb, Yb.h[:, st_, cg:cg + cn], ps, ps.h[:, 0:cn], eng=("act" if ctr % 2 else "dve"))
                r0 = ei * CAP + s0
                self.store(s["Yd"][r0:r0 + hw, :].rearrange("(a p) d -> p a d", p=128), Yb, Yb.h[:, 0:hst, :])

    def phase_combine(self):
        cfg, k, P, s = self.cfg, self.k, self.P, self.s
        KC, NE, CAP, D = cfg.KC, cfg.NE, cfg.CAP, cfg.D
        NST = CAP // 128
        sid = k.sb("sid", [128, NST], F32)
        self.load(sid, sid.h[:, :], self.slotid[:, :])
        selT = k.sb("selT", [128, NE * NST, 512], BF16)
        pmb = [k.sb(f"pmb{i}", [128, 512], F32) for i in range(2)]
        gtb = [k.sb(f"gtb{i}", [128, 512], F32) for i in range(2)]
        tmpq = [k.sb(f"tmpq{i}", [128, 512], F32) for i in range(2)]
        FH = min(8, KC)
        yts = [k.sb(f"yt{i}", [128, FH * 128], BF16) for i in range(4)]
        hr = [k.sb(f"hr{i}", [128, 512], F32) for i in range(FH)]
        ot = [k.sb(f"ot{i}", [128, 512], F32) for i in range(2)]
        yi = 0
        oi = 0
        for (c0, n) in cfg.chunks:
            for e in range(NE):
                pb_, gb_ = pmb[e % 2], gtb[e % 2]
                self.load(pb_, pb_.h[:, 0:n], s["pmT"][e, c0:c0 + n].partition_broadcast(128))
                self.load(gb_, gb_.h[:, 0:n], s["gtT"][e, c0:c0 + n].partition_broadcast(128))
                for st_ in range(NST):
                    k.op("dve", lambda en, e=e, st_=st_, n=n, pb_=pb_, gb_=gb_: en.scalar_tensor_tensor(
                        selT.h[:, e * NST + st_, 0:n], pb_.h[:, 0:n], sid.h[:, st_:st_ + 1], gb_.h[:, 0:n],
                        ALU.is_equal, ALU.mult), reads=[pb_, gb_, sid], writes=[selT])
            for f0 in range(0, KC, FH):
                fn_ = min(FH, KC - f0)
                tot = NE * NST
                for f in range(fn_):
                    fr = (f0 + f) * 128
                    self.load(hr[f], hr[f].h[:, 0:n], s["hres"][fr:fr + 128, c0:c0 + n])
                for q in range(tot):
                    e, st_ = divmod(q, NST)
                    yt = yts[yi % 4]
                    yi += 1
                    r0 = e * CAP + st_ * 128
                    self.load(yt, yt.h[:, 0:fn_ * 128], s["Yd"][r0:r0 + 128, f0 * 128:(f0 + fn_) * 128])
                    for f in range(fn_):
                        ps = self.ps[f]
                        self.mm(ps, ps.h[:, 0:n], yt.h[:, f * 128:(f + 1) * 128], selT.h[:, q, 0:n], [yt, selT], q == 0,
                                q == tot - 1, inc=(q == tot - 1 or f == fn_ - 1))
                for f in range(fn_):
                    ps = self.ps[f]
                    hrt, o_ = hr[f], ot[oi % 2]
                    oi += 1
                    fr = (f0 + f) * 128
                    k.op("dve", lambda en, o_=o_, hrt=hrt, ps=ps, n=n: en.scalar_tensor_tensor(
                        o_.h[:, 0:n], hrt.h[:, 0:n], cfg.ALPHA, ps.h[:, 0:n], ALU.mult, ALU.add), reads=[hrt, ps],
                        writes=[o_])
                    self.store(s["yT"][fr:fr + 128, c0:c0 + n], o_, o_.h[:, 0:n])

    def phase_ffn(self, experts, gated):
        cfg, k, P, s = self.cfg, self.k, self.P, self.s
        KC, DFF, T = cfg.KC, cfg.DFF, cfg.T
        FK = DFF // 128
        PART = cfg.FFPART
        supers = []
        cur = []
        for (c0, n) in cfg.chunks:
            if cur and sum(x[1] for x in cur) + n > cfg.SUPER + 16:
                supers.append(cur)
                cur = []
            cur.append((c0, n))
        if cur:
            supers.append(cur)
        SW = max(sum(x[1] for x in sc) for sc in supers)
        hb = k.sb("hbS", [128, KC, SW], BF16)
        G_ = k.sb("G", [128, PART, SW], BF16)
        acc = k.sb("acc", [128, KC, SW], F32)
        w1b = [k.sb(f"w1_{i}", [128, KC, 256], BF16) for i in range(2)]
        w3b = [k.sb(f"w3_{i}", [128, KC, 256], BF16) for i in range(2)]
        w2b = [k.sb(f"w2_{i}", [128, PART, 256], BF16) for i in range(2)]
        sa = [k.sb(f"sa{i}", [128, 512], F32) for i in range(2)]
        gbs = [k.sb(f"gb{i}", [128, 512], F32) for i in range(2)]
        hr = [k.sb(f"hr{i}", [128, 512], F32) for i in range(2)]
        ctr = [0]
        wctr = [0]
        for sc in supers:
            s0 = sc[0][0]
            sw = sum(x[1] for x in sc)
            self.load(hb, hb.h[:, :, 0:sw], self.fm(s["hb"])[:, :, s0:s0 + sw])
            first_acc = True
            for ei, (W1, W3, W2) in enumerate(experts):
                W1v = W1.rearrange("(kc p) n -> p kc n", p=128)
                W3v = W3.rearrange("(kc p) n -> p kc n", p=128)
                W2v = W2.rearrange("(kc p) n -> p kc n", p=128)
                for p0 in range(0, FK, PART):
                    pn = min(PART, FK - p0)
                    for j0 in range(0, pn, 2):
                        jn = min(2, pn - j0)
                        wi = wctr[0] % 2
                        wctr[0] += 1
                        w1, w3 = w1b[wi], w3b[wi]
                        col = (p0 + j0) * 128
                        self.load(w1, w1.h[:, :, 0:jn * 128], W1v[:, :, col:col + jn * 128], eng="pool")
                        self.load(w3, w3.h[:, :, 0:jn * 128], W3v[:, :, col:col + jn * 128], eng="pool")
                        for jj in range(jn):
                            j = j0 + jj
                            for (c0, n) in sc:
                                lo = c0 - s0
                                i = ctr[0] % 2
                                ctr[0] += 1
                                pa, pb = self.ps[2 * i], self.ps[2 * i + 1]
                                for kc in range(KC):
                                    self.mm(pa, pa.h[:, 0:n], w1.h[:, kc, jj * 128:(jj + 1) * 128],
                                            hb.h[:, kc, lo:lo + n], [w1, hb], kc == 0, kc == KC - 1)
                                for kc in range(KC):
                                    self.mm(pb, pb.h[:, 0:n], w3.h[:, kc, jj * 128:(jj + 1) * 128],
                                            hb.h[:, kc, lo:lo + n], [w3, hb], kc == 0, kc == KC - 1)
                                self.act(sa[i], sa[i].h[:, 0:n], pa, pa.h[:, 0:n], AF.Silu)
                                if gated:
                                    gbt = gbs[i]
                                    self.load(gbt, gbt.h[:, 0:n], s["gtT"][ei, c0:c0 + n].partition_broadcast(128))
                                    self.tt(sa[i], sa[i].h[:, 0:n], sa[i], sa[i].h[:, 0:n], gbt, gbt.h[:, 0:n],
                                            ALU.mult)
                                self.tt(G_, G_.h[:, j, lo:lo + n], sa[i], sa[i].h[:, 0:n], pb, pb.h[:, 0:n], ALU.mult)
                    for f0 in range(0, KC, 2):
                        fn_ = min(2, KC - f0)
                        wi = wctr[0] % 2
                        wctr[0] += 1
                        w2 = w2b[wi]
                        self.load(w2, w2.h[:, 0:pn, 0:fn_ * 128], W2v[:, p0:p0 + pn, f0 * 128:(f0 + fn_) * 128],
                                  eng="pool")
                        for ff in range(fn_):
                            f = f0 + ff
                            for (c0, n) in sc:
                                lo = c0 - s0
                                ps = self.ps[4 + ctr[0] % 4]
                                ctr[0] += 1
                                for j in range(pn):
                                    self.mm(ps, ps.h[:, 0:n], w2.h[:, j, ff * 128:(ff + 1) * 128], G_.h[:, j, lo:lo + n],
                                            [w2, G_], j == 0, j == pn - 1)
                                if first_acc and p0 == 0:
                                    hi = ctr[0] % 2
                                    hrt = hr[hi]
                                    self.load(hrt, hrt.h[:, 0:n], s["hres"][f * 128:(f + 1) * 128, c0:c0 + n])
                                    k.op("dve", lambda e, f=f, lo=lo, n=n, ps=ps, hrt=hrt: e.scalar_tensor_tensor(
                                        acc.h[:, f, lo:lo + n], hrt.h[:, 0:n], cfg.ALPHA, ps.h[:, 0:n], ALU.mult,
                                        ALU.add), reads=[hrt, ps], writes=[acc])
                                else:
                                    self.tt(acc, acc.h[:, f, lo:lo + n], acc, acc.h[:, f, lo:lo + n], ps, ps.h[:, 0:n],
                                            ALU.add)
                first_acc = False
            self.store(self.fm(s["yT"])[:, :, s0:s0 + sw], acc, acc.h[:, :, 0:sw])


_BUCKET_STARTS = [8, 12, 16, 23, 32, 46, 64, 91]


def _bucket(rel):
    rel = np.asarray(rel)
    n = np.abs(rel)
    b = np.where(n < 8, n, 8 + sum((n >= t).astype(np.int64) for t in _BUCKET_STARTS[1:]))
    return np.where(rel > 0, 16, 0) + b


def _host_tables(cfg, rel_bias):
    T, G = cfg.T, cfg.GRP
    pos = np.concatenate([cfg.NMETA + np.arange(cfg.SEQ), np.arange(cfg.NMETA)]).astype(np.float32)
    inv = (np.float32(10000.0) ** (-np.arange(0, cfg.ROPE, 2, dtype=np.float32) / np.float32(cfg.ROPE))).astype(np.float32)
    ang = pos[None, :] * inv[:, None]
    cos = np.cos(ang).astype(np.float32)
    sin = np.sin(ang).astype(np.float32)
    ropet = np.stack([np.concatenate([cos, cos], 0), np.concatenate([-sin, sin], 0)]).astype(np.float32)
    rb = np.asarray(rel_bias, np.float32)

    def tab(rel, masked, heads):
        bk = _bucket(rel)
        outs = []
        for h in heads:
            t = rb[bk, h]
            t = np.where(masked, np.float32(NEGV), t)
            outs.append(t)
        return np.concatenate(outs, axis=1).astype(np.float32)

    kk = np.arange(128)[:, None]
    qq = np.arange(128)[None, :]
    biasd = np.zeros((cfg.SKV, 3, 128, G * 128), np.float32)
    biasmk = np.zeros((cfg.SKV, 2, 16, G * 128), np.float32)
    biasqm = np.zeros((cfg.SKV, 144, G * 16), np.float32)
    mj = np.arange(16)[:, None]
    for hk in range(cfg.SKV):
        heads = [hk * G + g for g in range(G)]
        for d in (-1, 0, 1):
            rel = d * 128 + kk - qq
            biasd[hk, d + 1] = tab(rel, np.abs(rel) > 128, heads)
        rel0 = mj - (16 + qq)
        biasmk[hk, 0] = tab(rel0, np.zeros_like(rel0, bool), heads)
        rel1 = mj - (16 + 128 + qq)
        biasmk[hk, 1] = tab(rel1, np.zeros_like(rel1, bool), heads)
        q16 = np.arange(16)[None, :]
        relm = mj - q16
        biasqm[hk, 0:16] = tab(relm, np.zeros_like(relm, bool), heads)
        relb = (16 + kk) - q16
        biasqm[hk, 16:144] = tab(relb, relb > 128, heads)
    return ropet, biasd, biasmk, biasqm


def _fmcol(v):
    v = np.asarray(v, np.float32)
    return np.ascontiguousarray(v.reshape(-1, 128).T)


def prepare_inputs(cfg, inp):
    L = cfg.DEPTH
    ropet, biasd, biasmk, biasqm = _host_tables(cfg, inp["rel_bias"])
    consts = np.zeros((128, 384), np.float32)
    consts[:, 0:128] = np.eye(128, dtype=np.float32)
    consts[:, 128:256] = 1.0
    consts[:, 256:384] = np.triu(np.ones((128, 128), np.float32), 1)
    iotac = np.ascontiguousarray(np.broadcast_to(np.arange(cfg.CAP, dtype=np.float32)[None, :], (128, cfg.CAP)))
    slotid = (np.arange(cfg.CAP // 128, dtype=np.float32)[None, :] * 128 + np.arange(128, dtype=np.float32)[:, None])
    slotid = np.ascontiguousarray(slotid.astype(np.float32))
    cols = [_fmcol(inp["emb_ln_g"]), _fmcol(inp["emb_ln_b"])]
    for l in range(L):
        cols += [_fmcol(inp["ln_mix_g"][l]), _fmcol(inp["ln_mix_b"][l]), _fmcol(inp["ln_ffn_g"][l]),
                 _fmcol(inp["ln_ffn_b"][l]), _fmcol(inp["q_norm_g"][l]), _fmcol(inp["kv_norm_g"][l])]
    pvec = np.ascontiguousarray(np.concatenate(cols, axis=1))
    w_in = np.asarray(inp["w_in"], np.float32)
    ro = cfg.QR + cfg.KVR
    half = cfg.ROPE // 2
    rot = np.concatenate([w_in[:, :, ro + half:ro + cfg.ROPE], w_in[:, :, ro:ro + half]], axis=2)
    w_in_e = np.ascontiguousarray(np.concatenate([w_in, rot], axis=2))
    w_uq = np.asarray(inp["w_uq"], np.float32).reshape(L, cfg.QR, cfg.MH, cfg.NOPE + cfg.ROPE)
    qn = w_uq[..., :cfg.NOPE].reshape(L, cfg.QR, -1)
    qr = w_uq[..., cfg.NOPE:]
    qrot = np.concatenate([qr[..., half:], qr[..., :half]], axis=-1)
    qrr = np.concatenate([qr, qrot], axis=-1).reshape(L, cfg.QR, -1)
    w_uq_e = np.ascontiguousarray(np.concatenate([qn, qrr], axis=2))
    w_ukv = np.asarray(inp["w_ukv"], np.float32).reshape(L, cfg.KVR, cfg.MH, cfg.NOPE + cfg.VD)
    w_ukv_e = np.ascontiguousarray(np.concatenate([w_ukv[..., :cfg.NOPE].reshape(L, cfg.KVR, -1),
                                                   w_ukv[..., cfg.NOPE:].reshape(L, cfg.KVR, -1)], axis=2))
    sinkb = np.ascontiguousarray(np.broadcast_to(np.asarray(inp["sink_logits"], np.float32)[:, None, :],
                                                 (L, 128, cfg.SH)))
    shared = dict(meta=np.asarray(inp["meta_tokens"], np.float32), consts=consts, iotac=iotac, slotid=slotid,
                  pvec=pvec, ropet=ropet,
                  biasd=biasd, biasmk=biasmk, biasqm=biasqm, sinkb=sinkb, w_in=w_in_e, w_uq=w_uq_e, w_ukv=w_ukv_e,
                  w_pa=np.asarray(inp["w_proj_a"], np.float32), w_pb=np.asarray(inp["w_proj_b"], np.float32),
                  w_out=np.asarray(inp["w_out"], np.float32), ffn_w1=np.asarray(inp["ffn_w1"], np.float32),
                  ffn_w3=np.asarray(inp["ffn_w3"], np.float32), ffn_w2=np.asarray(inp["ffn_w2"], np.float32))
    if L // 2:
        nm = L // 2
        shared["router_w"] = np.asarray(inp["router_w"], np.float32)
        shared["router_bb"] = np.ascontiguousarray(np.broadcast_to(np.asarray(inp["router_b"], np.float32)[:, None, :],
                                                                   (nm, 128, cfg.NE)))
        shared["moe_w1"] = np.asarray(inp["moe_w1"], np.float32).reshape(nm * cfg.NE, cfg.D, cfg.DFF)
        shared["moe_w3"] = np.asarray(inp["moe_w3"], np.float32).reshape(nm * cfg.NE, cfg.D, cfg.DFF)
        shared["moe_w2"] = np.asarray(inp["moe_w2"], np.float32).reshape(nm * cfg.NE, cfg.DFF, cfg.D)
    return shared


def run(cfg, inp, debug=None, stop_after=None, trace=False):
    prog = Prog(cfg, debug=debug, stop_after=stop_after)
    nc = prog.build()
    shared = prepare_inputs(cfg, inp)
    x = np.asarray(inp["x"], np.float32)
    in_maps = []
    for c in range(cfg.NCORES):
        m = dict(shared)
        m["x"] = np.ascontiguousarray(x[c])
        in_maps.append(m)
    res = run_bass_kernel_spmd(nc, in_maps, core_ids=list(range(cfg.NCORES)), trace=trace)
    return res, prog


def kernel(**inputs):
    cfg = Cfg()
    res, _ = run(cfg, inputs)
    return np.stack([np.asarray(r["out"], np.float32) for r in res.results], axis=0)
```
